# Optimizing a Trainium2 kernel written in Bass

```python
import math
import jax, jax.numpy as jnp
from jax import lax
import numpy as np

D_MODEL = 2048
BATCH = 2
SEQ = 8192
DEPTH = 1
DEC_BATCH = 8
DEC_SEQ = 64
PAST_LEN = 4096

CHUNK = 64
Q_BLOCK = 128
EPS = 1e-6
NEG = -1e30
D_CONV = D_MODEL // 2
CONV_W = 31
N_HEADS = 16
HEAD_DIM = 64
D_ATTN = N_HEADS * 2 * HEAD_DIM
MEM_TOKENS = 256
MEM_HEADS = 4
MEM_HEAD_DIM = 256
D_MEM = MEM_HEADS * MEM_HEAD_DIM
N_BRANCH = 3
N_IN = 2 * D_CONV + 3 * D_ATTN + D_MEM + N_BRANCH * D_MODEL
N_GROUPS = 4
EXPERTS_PER_GROUP = 8
N_EXPERTS = N_GROUPS * EXPERTS_PER_GROUP
TOP_K = 2
D_EXPERT = 256

kernel_name = 'hybrid_streaming_encoder_step'


def rmsnorm(x, g, eps=EPS):
    xf = x.astype(jnp.float32)
    y = xf * lax.rsqrt(jnp.mean(xf * xf, axis=-1, keepdims=True) + eps)
    return (y * g.astype(jnp.float32)).astype(x.dtype)


def layernorm(x, g, b, eps=EPS):
    xf = x.astype(jnp.float32)
    mu = jnp.mean(xf, axis=-1, keepdims=True)
    var = jnp.mean(jnp.square(xf - mu), axis=-1, keepdims=True)
    y = (xf - mu) * lax.rsqrt(var + eps) * g.astype(jnp.float32) + b.astype(jnp.float32)
    return y.astype(x.dtype)


def conv_branch(u_in, conv_buf, w_dw, b_dw, ln_g, ln_b, w_conv_out):
    a, gt = jnp.split(u_in, 2, axis=-1)
    u = a * jax.nn.sigmoid(gt)
    ext = jnp.concatenate([conv_buf.astype(u.dtype), u], axis=1)
    y = lax.conv_general_dilated(ext, w_dw[:, None, :].astype(u.dtype), window_strides=(1,), padding='VALID',
                                 dimension_numbers=('NWC', 'WIO', 'NWC'), feature_group_count=D_CONV)
    y = jax.nn.silu(layernorm(y + b_dw, ln_g, ln_b))
    return y @ w_conv_out, ext[:, -(CONV_W - 1):]


def diff_attention(q, k, v, past_len, lam):
    B, T = q.shape[0], q.shape[1]
    bs = min(Q_BLOCK, T)
    nb = T // bs
    k_pos = jnp.arange(k.shape[1])
    kf = k.astype(jnp.float32)
    qb = jnp.moveaxis(q.reshape(B, nb, bs, N_HEADS, 2, HEAD_DIM), 1, 0)
    scale = HEAD_DIM ** -0.5

    def one_block(args):
        q_blk, blk = args
        q_pos = past_len + blk * bs + jnp.arange(bs)
        s = jnp.einsum('bqhcd,bkhcd->bhcqk', q_blk.astype(jnp.float32), kf) * scale
        mask = (k_pos[None, :] // CHUNK) <= (q_pos[:, None] // CHUNK)
        p = jax.nn.softmax(jnp.where(mask, s, NEG), axis=-1)
        a = p[:, :, 0] - lam * p[:, :, 1]
        return jnp.einsum('bhqk,bkhe->bqhe', a.astype(v.dtype), v)

    out = lax.map(one_block, (qb, jnp.arange(nb)))
    return jnp.moveaxis(out, 0, 1).reshape(B, T, N_HEADS, 2 * HEAD_DIM)


def memory_kv(mem, norm_mem, w_mem_k, w_mem_v):
    B, M = mem.shape[0], mem.shape[1]
    hm = rmsnorm(mem, norm_mem)
    mk = (hm @ w_mem_k).reshape(B, M, MEM_HEADS, MEM_HEAD_DIM)
    mv = (hm @ w_mem_v).reshape(B, M, MEM_HEADS, MEM_HEAD_DIM)
    return mk, mv


def mem_attention(mq, mk, mv):
    B, T = mq.shape[0], mq.shape[1]
    s = jnp.einsum('bqhd,bmhd->bhqm', mq.astype(jnp.float32), mk.astype(jnp.float32)) * (MEM_HEAD_DIM ** -0.5)
    p = jax.nn.softmax(s, axis=-1)
    return jnp.einsum('bhqm,bmhd->bqhd', p.astype(mv.dtype), mv).reshape(B, T, D_MEM)


def mixer_sublayer(x, conv_buf, k_past, v_past, mk, mv, norm_mix, w_in, w_dw, b_dw, conv_ln_g, conv_ln_b,
                   w_conv_out, lam, lam_init, subln_g, w_attn_out, w_mem_out, w_out):
    B, T = x.shape[0], x.shape[1]
    h = rmsnorm(x, norm_mix)
    proj = h @ w_in
    c0 = 2 * D_CONV
    c1 = c0 + D_ATTN
    c2 = c1 + D_ATTN
    c3 = c2 + D_ATTN
    c4 = c3 + D_MEM
    conv_y, new_conv_buf = conv_branch(proj[..., :c0], conv_buf, w_dw, b_dw, conv_ln_g, conv_ln_b, w_conv_out)
    q = proj[..., c0:c1].reshape(B, T, N_HEADS, 2, HEAD_DIM)
    k_rows = proj[..., c1:c2].reshape(B, T, N_HEADS, 2 * HEAD_DIM)
    v_rows = proj[..., c2:c3].reshape(B, T, N_HEADS, 2 * HEAD_DIM)
    mq = proj[..., c3:c4].reshape(B, T, MEM_HEADS, MEM_HEAD_DIM)
    gates = jax.nn.sigmoid(proj[..., c4:].reshape(B, T, N_BRANCH, D_MODEL))
    past_len = k_past.shape[1]
    k_all = jnp.concatenate([k_past.astype(k_rows.dtype), k_rows], axis=1).reshape(B, past_len + T, N_HEADS, 2, HEAD_DIM)
    v_all = jnp.concatenate([v_past.astype(v_rows.dtype), v_rows], axis=1)
    o = diff_attention(q, k_all, v_all, past_len, lam)
    o = rmsnorm(o, subln_g, 1e-5) * (1.0 - lam_init)
    attn_y = o.reshape(B, T, D_ATTN) @ w_attn_out
    mem_y = mem_attention(mq, mk, mv) @ w_mem_out
    merged = gates[:, :, 0] * conv_y + gates[:, :, 1] * attn_y + gates[:, :, 2] * mem_y
    return x + merged @ w_out, new_conv_buf, k_rows, v_rows


def hier_moe(h, w_rg, b_rg, w_re, b_re, w_g, w_u, w_d):
    shp = h.shape
    hf = h.reshape(-1, D_MODEL)
    ntok = hf.shape[0]
    grp_logits = (hf @ w_rg + b_rg).astype(jnp.float32)
    p_grp = jax.nn.softmax(grp_logits, axis=-1)
    g_idx = jnp.argmax(grp_logits, axis=-1)
    p_g = jnp.take_along_axis(p_grp, g_idx[:, None], axis=1)[:, 0]
    exp_logits = (hf @ w_re + b_re).astype(jnp.float32).reshape(ntok, N_GROUPS, EXPERTS_PER_GROUP)
    sel = jnp.take_along_axis(exp_logits, g_idx[:, None, None], axis=1)[:, 0]
    top_v, top_i = lax.top_k(jax.nn.softmax(sel, axis=-1), TOP_K)
    top_v = top_v / jnp.sum(top_v, axis=-1, keepdims=True)
    expert_id = g_idx[:, None] * EXPERTS_PER_GROUP + top_i
    combine = jnp.einsum('nk,nke->ne', p_g[:, None] * top_v,
                         jax.nn.one_hot(expert_id, N_EXPERTS, dtype=jnp.float32)).astype(h.dtype)
    y = jnp.zeros_like(hf)
    for e in range(N_EXPERTS):
        ye = (jax.nn.silu(hf @ w_g[e]) * (hf @ w_u[e])) @ w_d[e]
        y = y + combine[:, e:e + 1] * ye
    return y.reshape(shp)


def setup_inputs(seed: int = 0) -> dict:
    key = jax.random.key(seed)
    ks = iter(jax.random.split(key, 48))

    def nrm(shape, scale):
        return jax.random.normal(next(ks), shape, jnp.float32) * scale

    def gain(shape):
        return 1.0 + nrm(shape, 0.01)

    L = DEPTH
    return {
        'x_prompt': nrm((BATCH, SEQ, D_MODEL), 1.0),
        'x_sample': nrm((DEC_BATCH, DEC_SEQ, D_MODEL), 1.0),
        'mem_prompt': nrm((BATCH, MEM_TOKENS, D_MODEL), 1.0),
        'cache_conv': nrm((L, DEC_BATCH, CONV_W - 1, D_CONV), 1.0),
        'cache_diff_k': nrm((L, DEC_BATCH, PAST_LEN, N_HEADS, 2 * HEAD_DIM), 1.0),
        'cache_diff_v': nrm((L, DEC_BATCH, PAST_LEN, N_HEADS, 2 * HEAD_DIM), 1.0),
        'cache_mem_k': nrm((L, DEC_BATCH, MEM_TOKENS, MEM_HEADS, MEM_HEAD_DIM), 1.0),
        'cache_mem_v': nrm((L, DEC_BATCH, MEM_TOKENS, MEM_HEADS, MEM_HEAD_DIM), 1.0),
        'norm_mix': gain((L, D_MODEL)),
        'w_in': nrm((L, D_MODEL, N_IN), D_MODEL ** -0.5),
        'w_dw': nrm((L, CONV_W, D_CONV), CONV_W ** -0.5),
        'b_dw': nrm((L, D_CONV), 0.01),
        'conv_ln_g': gain((L, D_CONV)),
        'conv_ln_b': nrm((L, D_CONV), 0.01),
        'w_conv_out': nrm((L, D_CONV, D_MODEL), D_CONV ** -0.5),
        'lambda_q1': nrm((L, HEAD_DIM), 0.1),
        'lambda_k1': nrm((L, HEAD_DIM), 0.1),
        'lambda_q2': nrm((L, HEAD_DIM), 0.1),
        'lambda_k2': nrm((L, HEAD_DIM), 0.1),
        'subln_g': gain((L, 2 * HEAD_DIM)),
        'w_attn_out': nrm((L, D_ATTN, D_MODEL), D_ATTN ** -0.5),
        'norm_mem': gain((L, D_MODEL)),
        'w_mem_k': nrm((L, D_MODEL, D_MEM), D_MODEL ** -0.5),
        'w_mem_v': nrm((L, D_MODEL, D_MEM), D_MODEL ** -0.5),
        'w_mem_out': nrm((L, D_MEM, D_MODEL), D_MEM ** -0.5),
        'w_out': nrm((L, D_MODEL, D_MODEL), D_MODEL ** -0.5),
        'norm_ffn': gain((L, D_MODEL)),
        'w_router_grp': nrm((L, D_MODEL, N_GROUPS), D_MODEL ** -0.5),
        'b_router_grp': nrm((L, N_GROUPS), 0.01),
        'w_router_exp': nrm((L, D_MODEL, N_EXPERTS), D_MODEL ** -0.5),
        'b_router_exp': nrm((L, N_EXPERTS), 0.01),
        'w_exp_gate': nrm((L, N_EXPERTS, D_MODEL, D_EXPERT), D_MODEL ** -0.5),
        'w_exp_up': nrm((L, N_EXPERTS, D_MODEL, D_EXPERT), D_MODEL ** -0.5),
        'w_exp_down': nrm((L, N_EXPERTS, D_EXPERT, D_MODEL), D_EXPERT ** -0.5),
        'norm_final': gain((D_MODEL,)),
    }


def reference(x_prompt, x_sample, mem_prompt, cache_conv, cache_diff_k, cache_diff_v, cache_mem_k, cache_mem_v,
              norm_mix, w_in, w_dw, b_dw, conv_ln_g, conv_ln_b, w_conv_out, lambda_q1, lambda_k1, lambda_q2,
              lambda_k2, subln_g, w_attn_out, norm_mem, w_mem_k, w_mem_v, w_mem_out, w_out, norm_ffn,
              w_router_grp, b_router_grp, w_router_exp, b_router_exp, w_exp_gate, w_exp_up, w_exp_down, norm_final):
    xp, xs = x_prompt, x_sample
    bp = x_prompt.shape[0]
    conv_p, k_p, v_p, mk_p, mv_p, conv_s, k_s, v_s = [], [], [], [], [], [], [], []
    for l in range(DEPTH):
        lam_init = 0.8 - 0.6 * math.exp(-0.3 * l)
        lam = (jnp.exp(jnp.sum(lambda_q1[l].astype(jnp.float32) * lambda_k1[l].astype(jnp.float32)))
               - jnp.exp(jnp.sum(lambda_q2[l].astype(jnp.float32) * lambda_k2[l].astype(jnp.float32))) + lam_init)
        shared = (norm_mix[l], w_in[l], w_dw[l], b_dw[l], conv_ln_g[l], conv_ln_b[l], w_conv_out[l], lam, lam_init,
                  subln_g[l], w_attn_out[l], w_mem_out[l], w_out[l])
        moe_w = (w_router_grp[l], b_router_grp[l], w_router_exp[l], b_router_exp[l], w_exp_gate[l], w_exp_up[l], w_exp_down[l])
        mk, mv = memory_kv(mem_prompt, norm_mem[l], w_mem_k[l], w_mem_v[l])
        conv0 = jnp.zeros((bp, CONV_W - 1, D_CONV), xp.dtype)
        kv0 = jnp.zeros((bp, 0, N_HEADS, 2 * HEAD_DIM), xp.dtype)
        xp, cb_p, kr_p, vr_p = mixer_sublayer(xp, conv0, kv0, kv0, mk, mv, *shared)
        xp = xp + hier_moe(rmsnorm(xp, norm_ffn[l]), *moe_w)
        xs, cb_s, kr_s, vr_s = mixer_sublayer(xs, cache_conv[l], cache_diff_k[l], cache_diff_v[l],
                                              cache_mem_k[l], cache_mem_v[l], *shared)
        xs = xs + hier_moe(rmsnorm(xs, norm_ffn[l]), *moe_w)
        conv_p.append(cb_p); k_p.append(kr_p); v_p.append(vr_p); mk_p.append(mk); mv_p.append(mv)
        conv_s.append(cb_s); k_s.append(kr_s); v_s.append(vr_s)
    y_prompt = rmsnorm(xp, norm_final)
    y_sample = rmsnorm(xs, norm_final)
    return (y_prompt, y_sample, jnp.stack(conv_p), jnp.stack(k_p), jnp.stack(v_p), jnp.stack(mk_p), jnp.stack(mv_p),
            jnp.stack(conv_s), jnp.stack(k_s), jnp.stack(v_s))
```

```python
import math
from contextlib import ExitStack

import numpy as np
import concourse.bass as bass
import concourse.mybir as mybir
from concourse.bass_utils import run_bass_kernel_spmd

F32 = mybir.dt.float32
BF16 = mybir.dt.bfloat16
ACT = mybir.ActivationFunctionType
ALU = mybir.AluOpType
AX = mybir.AxisListType

D = 2048
KC = 16
SEQ = 8192
NGRP = 4
DBG_STOP = None
GT = 512
DCONV = 1024
NSAMP = 64
PAST = 4096
NIN = 15360
NEXP = 32
DEXP = 256
EPS = 1e-6
NEG = -30000.0
C_AGT, C_Q, C_K, C_V, C_MQ, C_G = 0, 2048, 4096, 6144, 8192, 9216
STAGE = 2


class Buf:
    def __init__(self, name):
        self.name = name
        self.w = {}
        self.r = {}
        self.dsem = None
        self.dcount = 0


class Eng:
    def __init__(self, kb, e, name, is_pe=False):
        self.e = e
        self.name = name
        self.sem = kb.newsem("e_" + name)
        self.n = 0
        self.seen = {}
        self.is_pe = is_pe

    def wait(self, sem, val):
        if val <= 0:
            return
        if self.is_pe and sem is self.sem:
            return
        if self.seen.get(sem, 0) >= val:
            return
        self.e.wait_ge(sem, val)
        self.seen[sem] = val


class KB:
    def __init__(self, nc, es):
        self.nc = nc
        self.es = es
        self.sem_es = es
        self.nsem = 0
        self.pe = Eng(self, nc.tensor, "pe", True)
        self.act = Eng(self, nc.scalar, "act")
        self.dve = Eng(self, nc.vector, "dve")
        self.pool = Eng(self, nc.gpsimd, "pool")
        self.sp = Eng(self, nc.sync, "sp")
        self.engs = [self.pe, self.act, self.dve, self.pool, self.sp]
        self.dma_bufs = []

    def newsem(self, name):
        self.nsem += 1
        return self.sem_es.enter_context(self.nc.semaphore(name + "_%d" % self.nsem))

    def sb(self, name, shape, dt):
        self.nsb = getattr(self, "nsb", 0) + 1
        name = "%s_%d" % (name, self.nsb)
        t = self.es.enter_context(self.nc.sbuf_tensor(name, shape, dt))
        return t, Buf(name)

    def _deps(self, eng, reads, writes):
        for b in reads:
            for s, v in b.w.items():
                eng.wait(s, v)
        for b in writes:
            for s, v in b.w.items():
                eng.wait(s, v)
            for s, v in b.r.items():
                eng.wait(s, v)

    def _mark(self, sem, val, reads, writes):
        for b in reads:
            if b.r.get(sem, 0) < val:
                b.r[sem] = val
        for b in writes:
            if b.w.get(sem, 0) < val:
                b.w[sem] = val

    def op(self, eng, fn, reads=(), writes=()):
        self._deps(eng, reads, writes)
        ins = fn(eng.e)
        eng.n += 1
        ins.then_inc(eng.sem, 1)
        self._mark(eng.sem, eng.n, reads, writes)

    def dma(self, q, out, in_, reads, writes, owner, throttle=None):
        self._deps(q, reads, writes)
        if throttle is not None:
            hist, depth = throttle
            if len(hist) >= depth:
                ps_, pv_ = hist[len(hist) - depth]
                q.wait(ps_, pv_)
        if owner.dsem is None:
            owner.dsem = self.newsem("d_" + owner.name)
            self.dma_bufs.append(owner)
        ins = q.e.dma_start(out=out, in_=in_)
        owner.dcount += 16
        ins.then_inc(owner.dsem, 16)
        if throttle is not None:
            throttle[0].append((owner.dsem, owner.dcount))
        self._mark(owner.dsem, owner.dcount, reads, writes)

    def barrier(self):
        for e in self.engs:
            for f in self.engs:
                if f is not e:
                    e.wait(f.sem, f.n)
            for b in self.dma_bufs:
                e.wait(b.dsem, b.dcount)


def build_program():
    nc = bass.Bass("TRN2", target_bir_lowering=False)

    def din(name, shape):
        return nc.dram_tensor(name, list(shape), F32, kind="ExternalInput").ap()

    def dout(name, shape):
        return nc.dram_tensor(name, list(shape), F32, kind="ExternalOutput").ap()

    def dscr(name, shape, dt=BF16):
        return nc.dram_tensor(name, list(shape), dt, kind="Internal").ap()

    xfull = din("xfull", [SEQ, D])
    xhalo = din("xhalo", [NGRP, 32, D])
    gpos = din("gpos", [128, 4])
    xsamp = din("xsamp", [NSAMP, D])
    cconv = din("cconv", [30, DCONV])
    ck = din("ck", [PAST, D])
    cv = din("cv", [PAST, D])
    cmk = din("cmk", [256, 1024])
    cmv = din("cmv", [256, 1024])
    memx = din("memx", [256, D])
    norm_mix = din("norm_mix", [D])
    w_in = din("w_in", [D, NIN])
    w_dw = din("w_dw", [31, DCONV])
    b_dw = din("b_dw", [DCONV])
    ln_g = din("conv_ln_g", [DCONV])
    ln_b = din("conv_ln_b", [DCONV])
    w_co = din("w_conv_out", [DCONV, D])
    lq1 = din("lambda_q1", [64])
    lk1 = din("lambda_k1", [64])
    lq2 = din("lambda_q2", [64])
    lk2 = din("lambda_k2", [64])
    subg = din("subln_g", [128])
    w_ao = din("w_attn_out", [D, D])
    norm_mem = din("norm_mem", [D])
    w_mk = din("w_mem_k", [D, 1024])
    w_mv = din("w_mem_v", [D, 1024])
    w_mo = din("w_mem_out", [1024, D])
    w_o = din("w_out", [D, D])
    norm_ffn = din("norm_ffn", [D])
    w_rg = din("w_router_grp", [D, 4])
    b_rg = din("b_router_grp", [4])
    w_re = din("w_router_exp", [D, 32])
    b_re = din("b_router_exp", [32])
    w_eg = din("w_exp_gate", [NEXP, D, DEXP])
    w_eu = din("w_exp_up", [NEXP, D, DEXP])
    w_ed = din("w_exp_down", [NEXP, DEXP, D])
    norm_final = din("norm_final", [D])

    NTOK = (SEQ // 2048) * GT + NSAMP
    y_o = dout("y", [NTOK, D])
    k_o = dout("kout", [NTOK, D])
    v_o = dout("vout", [NTOK, D])
    convp_o = dout("convp", [30, DCONV])
    convs_o = dout("convs", [30, DCONV])
    memk_o = dout("memk", [256, 1024])
    memv_o = dout("memv", [256, 1024])

    s_win = dscr("s_win", [D, NIN])
    s_wco = dscr("s_wco", [DCONV, D])
    s_wao = dscr("s_wao", [D, D])
    s_wmk = dscr("s_wmk", [D, 1024])
    s_wmv = dscr("s_wmv", [D, 1024])
    s_wmo = dscr("s_wmo", [1024, D])
    s_wo = dscr("s_wo", [D, D])
    s_wr = dscr("s_wr", [D, 36])
    s_eg = dscr("s_eg", [NEXP, D, DEXP])
    s_eu = dscr("s_eu", [NEXP, D, DEXP])
    s_ed = dscr("s_ed", [NEXP, DEXP, D])
    NBP = SEQ // 128
    NBS = PAST // 128 + 1
    NGRP_ = SEQ // 2048
    s_ktp = dscr("s_ktp", [16, 128, SEQ])
    s_vp = dscr("s_vp", [16, 128, NBP, 128])
    s_dg = dscr("s_dg", [8, 128, 31 * 128])
    s_kts = dscr("s_kts", [16, 128, NBS * 128])
    s_vs = dscr("s_vs", [16, 128, NBS, 128])

    with ExitStack() as es:
        es.enter_context(nc.allow_non_contiguous_dma(reason="small parameter loads"))
        kb = KB(nc, es)
        pe, act, dve, pool, sp = kb.pe, kb.act, kb.dve, kb.pool, kb.sp

        ps = []
        psb = []
        for i in range(8):
            t = es.enter_context(nc.psum_tensor("ps%d" % i, [128, 512], F32))
            ps.append(t)
            psb.append(Buf("ps%d" % i))
        rr = {"a": 0, "s": 0}

        def next_ps(pool_name="a"):
            if pool_name == "a":
                i = rr["a"] % 8
                rr["a"] += 1
            else:
                i = rr["s"] % 4
                rr["s"] += 1
            return i

        if DBG_STOP == "pm1":
            kb.dma(sp, y_o[0:128, :], xfull[0:128, :], [], [], Buf("t"))
            kb.barrier()
            return nc
        if DBG_STOP == "p0a":
            kb.barrier()
            return nc
        ident, B_c = kb.sb("ident", [128, 128], BF16)
        ones, _ = kb.sb("ones", [128, 128], BF16)
        kb.op(pool, lambda e: e.memset(ident[:], 0.0), [], [B_c])
        kb.op(pool, lambda e: e.affine_select(out=ident[:], in_=ident[:], pattern=[[-1, 128]],
                                              compare_op=ALU.not_equal, fill=1.0, base=0,
                                              channel_multiplier=1), [B_c], [B_c])
        kb.op(pool, lambda e: e.memset(ones[:], 1.0), [], [B_c])

        cols, B_cols = kb.sb("cols", [128, 64], F32)
        kb.op(dve, lambda e: e.memset(cols[:], 0.0), [], [B_cols])
        pfm, B_pfm = kb.sb("pfm", [128, 3, 16], F32)
        for i, src in enumerate((norm_mix, norm_mem, norm_ffn)):
            kb.dma(sp, pfm[:, i, :], src.rearrange("(k p) -> p k", p=128), [], [B_pfm], B_pfm)
        pcv, B_pcv = kb.sb("pcv", [128, 34 + 3, 8], F32)
        kb.dma(sp, pcv[:, 0:31, :], w_dw.rearrange("t (k p) -> p t k", p=128), [], [B_pcv], B_pcv)
        kb.dma(sp, pcv[:, 31, :], b_dw.rearrange("(k p) -> p k", p=128), [], [B_pcv], B_pcv)
        kb.dma(sp, pcv[:, 32, :], ln_g.rearrange("(k p) -> p k", p=128), [], [B_pcv], B_pcv)
        kb.dma(sp, pcv[:, 33, :], ln_b.rearrange("(k p) -> p k", p=128), [], [B_pcv], B_pcv)
        lamt, B_lam = kb.sb("lamt", [128, 4, 64], F32)
        for i, src in enumerate((lq1, lk1, lq2, lk2)):
            kb.dma(sp, lamt[:, i, :], src.partition_broadcast(128), [], [B_lam], B_lam)
        subgc, B_subg = kb.sb("subgc", [128, 1], F32)
        kb.dma(sp, subgc[:, :], subg.rearrange("(p o) -> p o", o=1), [], [B_subg], B_subg)
        gpt, B_gp = kb.sb("gpt", [128, 4], F32)
        kb.dma(sp, gpt[:, :], gpos[:, :], [], [B_gp], B_gp)
        rbias, B_rb = kb.sb("rbias", [128, 36], F32)
        kb.dma(sp, rbias[:, 0:4], b_rg.partition_broadcast(128), [], [B_rb], B_rb)
        kb.dma(sp, rbias[:, 4:36], b_re.partition_broadcast(128), [], [B_rb], B_rb)
        wr_f, B_wrf = kb.sb("wr_f", [128, KC, 36], F32)
        wr_sb, B_wrs = kb.sb("wr_sb", [128, KC, 36], BF16)
        kb.dma(sp, wr_f[:, :, 0:4], w_rg.rearrange("(k p) c -> p k c", p=128), [], [B_wrf], B_wrf)
        kb.dma(sp, wr_f[:, :, 4:36], w_re.rearrange("(k p) c -> p k c", p=128), [], [B_wrf], B_wrf)
        kb.op(dve, lambda e: e.tensor_copy(out=wr_sb[:, :, :], in_=wr_f[:, :, :]), [B_wrf], [B_wrs])

        gbc, B_gbc = kb.sb("gbc", [128, 2, KC, 128], BF16)

        def fill_gbc(dst, src_i):
            for k in range(KC):
                kb.op(dve, lambda e, k=k: e.tensor_copy(out=dst[:, k, :], in_=pfm[:, src_i, k:k + 1].to_broadcast([128, 128])),
                      [B_pfm], [B_gbc])
        fill_gbc(gbc[:, 0, :, :], 0)
        fill_gbc(gbc[:, 1, :, :], 2)
        G_MIX, G_FFN = gbc[:, 0, :, :], gbc[:, 1, :, :]
        lam_init = 0.8 - 0.6 * math.exp(-0.3 * 0)
        ltmp, B_lt = kb.sb("ltmp", [128, 2, 64], F32)
        kb.op(dve, lambda e: e.tensor_tensor(out=ltmp[:, 0, :], in0=lamt[:, 0, :], in1=lamt[:, 1, :], op=ALU.mult),
              [B_lam], [B_lt])
        kb.op(dve, lambda e: e.tensor_tensor(out=ltmp[:, 1, :], in0=lamt[:, 2, :], in1=lamt[:, 3, :], op=ALU.mult),
              [B_lam], [B_lt])
        kb.op(dve, lambda e: e.reduce_sum(out=cols[:, 4:6], in_=ltmp[:, :, :], axis=AX.X), [B_lt], [B_cols])
        kb.op(act, lambda e: e.activation(out=cols[:, 6:8], in_=cols[:, 4:6], func=ACT.Exp), [B_cols], [B_cols])
        kb.op(dve, lambda e: e.tensor_tensor(out=cols[:, 0:1], in0=cols[:, 6:7], in1=cols[:, 7:8], op=ALU.subtract),
              [B_cols], [B_cols])
        kb.op(dve, lambda e: e.tensor_scalar(out=cols[:, 0:1], in0=cols[:, 0:1], scalar1=lam_init, scalar2=None,
                                             op0=ALU.add), [B_cols], [B_cols])
        kb.op(dve, lambda e: e.tensor_scalar(out=cols[:, 1:2], in0=cols[:, 0:1], scalar1=-1.0, scalar2=None,
                                             op0=ALU.mult), [B_cols], [B_cols])
        kb.op(dve, lambda e: e.tensor_scalar(out=cols[:, 2:3], in0=subgc[:, 0:1], scalar1=(1.0 - lam_init),
                                             scalar2=None, op0=ALU.mult), [B_subg, B_cols], [B_cols])
        for m in range(1, 4):
            kb.op(dve, lambda e, m=m: e.tensor_scalar(out=cols[:, 8 + m:9 + m], in0=gpt[:, m:m + 1],
                                                      scalar1=gpt[:, 0:1], scalar2=NEG, op0=ALU.is_gt,
                                                      op1=ALU.mult), [B_gp, B_cols], [B_cols])

        B_win = {}
        cth = ([], 2)
        for nm, c0, c1 in (("kv", C_K, C_MQ), ("agt", C_AGT, C_Q), ("g0", C_G, C_G + D), ("mq", C_MQ, C_G),
                           ("g2", C_G + 2 * D, NIN), ("q", C_Q, C_K), ("g1", C_G + D, C_G + 2 * D)):
            b = Buf("cw_" + nm)
            B_win[nm] = b
            for r in range(0, D, 512):
                kb.dma(pool, s_win[r:r + 512, c0:c1], w_in[r:r + 512, c0:c1], [], [b], b, cth)

        def cast_simple(name, dst, src, rows):
            b = Buf("cw_" + name)
            step = 512
            for r in range(0, rows, step):
                kb.dma(pool, dst[r:r + step, :], src[r:r + step, :], [], [b], b, cth)
            return b

        B_wmk = cast_simple("wmk", s_wmk, w_mk, D)
        B_wmv = cast_simple("wmv", s_wmv, w_mv, D)
        B_wco = cast_simple("wco", s_wco, w_co, DCONV)
        B_wao = cast_simple("wao", s_wao, w_ao, D)
        B_wmo = cast_simple("wmo", s_wmo, w_mo, 1024)
        B_wo = cast_simple("wo", s_wo, w_o, D)
        B_exp = []
        for g8 in range(NEXP // 4):
            b = Buf("cw_e%d" % g8)
            B_exp.append(b)
            for e in range(g8 * 4, g8 * 4 + 4):
                kb.dma(pool, s_eg[e], w_eg[e], [], [b], b, cth)
                kb.dma(pool, s_eu[e], w_eu[e], [], [b], b, cth)
                kb.dma(pool, s_ed[e], w_ed[e], [], [b], b, cth)

        if DBG_STOP == "p0":
            kb.barrier()
            return nc
        def rmsnorm_T(xt, Bx, nt, gi, hT, BhT, t0, xn, Bxn, sq, Bsq, col0):
            c_ss = cols[:nt, col0:col0 + 1]
            c_r = cols[:nt, col0 + 1:col0 + 2]
            kb.op(dve, lambda e: e.memset(c_ss, 0.0), [], [B_cols])
            kb.op(act, lambda e: e.activation(out=sq[:nt, :], in_=xt, func=ACT.Square, accum_out=c_ss),
                  [Bx, B_cols], [Bsq, B_cols])
            kb.op(act, lambda e: e.activation(out=c_r, in_=c_ss, func=ACT.Sqrt, scale=1.0 / D, bias=EPS),
                  [B_cols], [B_cols])
            kb.op(dve, lambda e: e.reciprocal(out=c_r, in_=c_r), [B_cols], [B_cols])
            kb.op(act, lambda e: e.activation(out=xn[:nt, :], in_=xt, func=ACT.Copy, scale=c_r),
                  [Bx, B_cols], [Bxn])
            for half in range(2):
                b = next_ps()
                pv = ps[b][:].bitcast(BF16).rearrange("p (j c) -> p j c", c=128)
                for j in range(8):
                    k = half * 8 + j
                    kb.op(pe, lambda e, j=j, k=k: e.transpose(pv[:, j, :nt], xn[:nt, k * 128:(k + 1) * 128],
                                                              ident[:nt, :nt]), [Bxn, B_c], [psb[b]])
                kb.op(dve, lambda e, half=half, pv=pv: e.tensor_tensor(
                    out=hT[:, half * 8:half * 8 + 8, t0:t0 + nt], in0=pv[:, :, :nt],
                    in1=gi[:, half * 8:half * 8 + 8, :nt], op=ALU.mult), [psb[b], B_gbc], [BhT])

        def evac(i, out_ap, in_ap, reads, writes, scale=None):
            if i % 2 == 0:
                if scale is None:
                    kb.op(act, lambda e: e.activation(out=out_ap, in_=in_ap, func=ACT.Copy), reads, writes)
                else:
                    kb.op(act, lambda e: e.activation(out=out_ap, in_=in_ap, func=ACT.Copy, scale=scale), reads, writes)
            else:
                if scale is None:
                    kb.op(dve, lambda e: e.tensor_copy(out=out_ap, in_=in_ap), reads, writes)
                else:
                    kb.op(dve, lambda e: e.tensor_scalar(out=out_ap, in0=in_ap, scalar1=scale, scalar2=None,
                                                         op0=ALU.mult), reads, writes)

        def mm_acc(b, out_ap, pairs, reads):
            n = len(pairs)
            for i, (l, r) in enumerate(pairs):
                kb.op(pe, lambda e, l=l, r=r, i=i: e.matmul(out_ap, l, r, start=(i == 0), stop=(i == n - 1)),
                      reads, [psb[b]])

        B_ktp, B_vp, B_kts, B_vs = Buf("ktp"), Buf("vp"), Buf("kts"), Buf("vs")
        dgB = Buf("s_dg")
        with ExitStack() as es1:
            kb_es = kb.es
            kb.es = es1
            B_dg = dgB
            dgs = [kb.sb("dgs%d" % i, [128, 31, 128], BF16) for i in range(2)]
            for cc in range(8):
                dg_, Bdg_ = dgs[cc % 2]
                for k in range(31):
                    kb.op(dve, lambda e, cc=cc, k=k, dg_=dg_: e.tensor_scalar(out=dg_[:, k, :], in0=ident[:, :],
                                                                            scalar1=pcv[:, k, cc:cc + 1], scalar2=None,
                                                                            op0=ALU.mult), [B_c, B_pcv], [Bdg_])
                kb.dma(sp, s_dg[cc], dg_[:, :, :].rearrange("p k c -> p (k c)"), [Bdg_], [B_dg], Bdg_)
            p1w = [kb.sb("p1w%d" % i, [128, KC, 512], BF16) for i in range(3)]
            xs = [kb.sb("p1x%d" % i, [128, D], F32) for i in range(2)]
            xn1, B_xn1 = kb.sb("p1xn", [128, D], BF16)
            sq1, B_sq1 = xn1, B_xn1
            hTs = [kb.sb("p1hT%d" % i, [128, KC, GT], BF16) for i in range(2)]
            kts = [kb.sb("p1kt%d" % i, [128, 4, GT], BF16) for i in range(2)]
            vss = [kb.sb("p1v%d" % i, [128, 4, 512], BF16) for i in range(2)]
            ckb = [kb.sb("p1ck%d" % i, [128, D], BF16) for i in range(2)]
            xi = [0]
            wq = {"n": 0, "loaded": 0}
            NCH = SEQ // GT + 1

            def p1_wload(upto):
                while wq["loaded"] < min(upto, NCH * 8):
                    i = wq["loaded"]
                    blk = i % 8
                    w, Bw = p1w[i % 3]
                    kb.dma(sp, w[:, :, :], s_win[:, C_K + blk * 512:C_K + (blk + 1) * 512].rearrange(
                        "(k p) c -> p k c", p=128), [B_win["kv"]], [Bw], Bw)
                    wq["loaded"] += 1

            def kv_norm(ci, src_rows, ntok):
                hT, BhT = hTs[ci % 2]
                ntile = (ntok + 127) // 128
                for t in range(ntile):
                    nt = min(128, ntok - t * 128)
                    xt, Bx = xs[xi[0] % 2]
                    xi[0] += 1
                    kb.dma(sp, xt[:nt, :], src_rows[t * 128:t * 128 + nt, :], [], [Bx], Bx)
                    rmsnorm_T(xt[:nt, :], Bx, nt, G_MIX, hT, BhT, t * 128, xn1, B_xn1, sq1, B_sq1, 16)

            def kv_chunk(ci, ntok, kt_dst_fn, v_dst_fn, Bkt, Bv, mid_fn=None):
                hT, BhT = hTs[ci % 2]
                ntile = (ntok + 127) // 128
                for blk in range(8):
                    if blk == 3 and mid_fn is not None:
                        mid_fn()
                    i = wq["n"]
                    wq["n"] += 1
                    p1_wload(i + 2)
                    w, Bw = p1w[i % 3]
                    if blk < 4:
                        kt, Bk = kts[blk % 2]
                        for hh in range(4):
                            b = next_ps()
                            mm_acc(b, ps[b][:, :ntok], [(w[:, k, hh * 128:(hh + 1) * 128], hT[:, k, :ntok])
                                                        for k in range(KC)], [Bw, BhT])
                            evac(hh, kt[:, hh, :ntok], ps[b][:, :ntok], [psb[b]], [Bk])
                        kb.dma(sp, kt_dst_fn(blk), kt[:, :, :ntok], [Bk], [Bkt], Bk)
                    else:
                        cb = blk - 4
                        vs_, Bvs = vss[blk % 2]
                        for t in range(ntile):
                            nt = min(128, ntok - t * 128)
                            b = next_ps()
                            mm_acc(b, ps[b][:nt, :], [(hT[:, k, t * 128:t * 128 + nt], w[:, k, :])
                                                      for k in range(KC)], [Bw, BhT])
                            evac(t, vs_[:nt, t, :], ps[b][:nt, :], [psb[b]], [Bvs])
                            kb.dma(sp, v_dst_fn(t, nt, cb), vs_[:nt, t, :].rearrange("p (h d) -> p h d", d=128),
                                   [Bvs], [Bv], Bvs)

            vp_v = s_vp.rearrange("h p nb d -> p h nb d")
            vs_v = s_vs.rearrange("h p nb d -> p h nb d")
            NCHK = SEQ // GT
            kv_norm(0, xfull[0:GT, :], GT)
            for ci in range(NCHK):
                if ci + 1 < NCHK:
                    mid = lambda ci=ci: kv_norm(ci + 1, xfull[(ci + 1) * GT:(ci + 2) * GT, :], GT)
                else:
                    mid = lambda: kv_norm(NCHK, xsamp, NSAMP)
                kv_chunk(ci, GT,
                         lambda blk, ci=ci: s_ktp.rearrange("h p n -> p h n")[:, blk * 4:blk * 4 + 4, ci * GT:(ci + 1) * GT],
                         lambda t, nt, cb, ci=ci: vp_v[:nt, cb * 4:cb * 4 + 4, ci * 4 + t, :], B_ktp, B_vp, mid)
            kv_chunk(NCHK, NSAMP, lambda blk: s_kts.rearrange("h p n -> p h n")[:, blk * 4:blk * 4 + 4, PAST:PAST + NSAMP],
                     lambda t, nt, cb: vs_v[:nt, cb * 4:cb * 4 + 4, NBS - 1, :], B_kts, B_vs)
            for nb0 in range(PAST // 128):
                kb.dma(pool, vs_v[:, :, nb0, :],
                       cv[nb0 * 128:(nb0 + 1) * 128, :].rearrange("p (h d) -> p h d", d=128),
                       [], [B_vs], B_vs)
            ktc = [kb.sb("p1ktc%d" % i, [128, 16, GT], BF16) for i in range(2)]
            for nb in range(PAST // 128):
                ct, Bct = ckb[nb % 2]
                kt, Bk = ktc[(nb // 4) % 2]
                kb.dma(pool, ct[:, :], ck[nb * 128:(nb + 1) * 128, :], [], [Bct], Bct)
                for half in range(2):
                    b = next_ps()
                    pv = ps[b][:].bitcast(BF16).rearrange("p (j c) -> p j c", c=128)
                    for j in range(8):
                        h = half * 8 + j
                        kb.op(pe, lambda e, j=j, h=h, pv=pv, ct=ct: e.transpose(pv[:, j, :], ct[:, h * 128:(h + 1) * 128],
                                                                              ident[:, :]), [Bct, B_c], [psb[b]])
                    evac(half, kt[:, half * 8:half * 8 + 8, (nb % 4) * 128:(nb % 4 + 1) * 128], pv[:, :, :],
                         [psb[b]], [Bk])
                if nb % 4 == 3:
                    kb.dma(pool, s_kts.rearrange("h p n -> p h n")[:, :, (nb - 3) * 128:(nb + 1) * 128], kt[:, :, :],
                           [Bk], [B_kts], Bk)
            kb.barrier()
            kb.es = kb_es

        if DBG_STOP == "p1":
            return nc
        mkT_p, B_mkp = kb.sb("mkT_p", [128, 8, 256], BF16)
        mv_p, B_mvp = kb.sb("mv_p", [128, 2, 1024], BF16)

        xn, B_xn = kb.sb("xn", [128, D], BF16)
        sq, B_sq = xn, B_xn
        ostage, B_ost = kb.sb("ostage", [128, 2, 512], F32)
        with ExitStack() as es2:
            kb_es = kb.es
            kb.es = es2
            mx, B_mx = kb.sb("mx", [128, 2, D], F32)
            gmem, _ = kb.sb("gmem", [128, KC, 128], BF16)
            fill_gbc(gmem, 1)
            hmT, B_hmT = kb.sb("hmT", [128, KC, 256], BF16)
            wm, B_wm = kb.sb("wm", [128, KC, 1024], BF16)
            for t in range(2):
                kb.dma(sp, mx[:, t, :], memx[t * 128:(t + 1) * 128, :], [], [B_mx], B_mx)
            for t in range(2):
                rmsnorm_T(mx[:, t, :], B_mx, 128, gmem[:, :, :], hmT, B_hmT, t * 128, xn, B_xn, sq, B_sq, 16)
            if DBG_STOP == "p2a":
                kb.barrier()
                return nc
            for which, (sw, Bsw, outd) in enumerate(((s_wmk, B_wmk, memk_o), (s_wmv, B_wmv, memv_o))):
                if DBG_STOP == "p2b" and which == 1:
                    kb.barrier()
                    return nc
                kb.dma(sp, wm[:, :, :], sw.rearrange("(k p) c -> p k c", p=128), [Bsw], [B_wm], B_wm)
                if which == 0:
                    for cc in range(8):
                        b = next_ps()
                        mm_acc(b, ps[b][:, :256], [(wm[:, k, cc * 128:(cc + 1) * 128], hmT[:, k, :]) for k in range(KC)],
                               [B_wm, B_hmT])
                        evac(cc, mkT_p[:, cc, :], ps[b][:, :256], [psb[b]], [B_mkp])
                for t in range(2):
                    for cb in range(2):
                        b = next_ps()
                        mm_acc(b, ps[b][:, :], [(hmT[:, k, t * 128:(t + 1) * 128], wm[:, k, cb * 512:(cb + 1) * 512])
                                                for k in range(KC)], [B_wm, B_hmT])
                        evac(0, ostage[:, cb, :], ps[b][:, :], [psb[b]], [B_ost])
                        if which == 1:
                            kb.op(dve, lambda e, t=t, cb=cb: e.tensor_copy(out=mv_p[:, t, cb * 512:(cb + 1) * 512],
                                                                           in_=ostage[:, cb, :]), [B_ost], [B_mvp])
                        kb.dma(pool, outd[t * 128:(t + 1) * 128, cb * 512:(cb + 1) * 512], ostage[:, cb, :], [B_ost], [], B_ost)
            if DBG_STOP == "p2c":
                kb.barrier()
                return nc
            kb.barrier()
            kb.es = kb_es

        xg, B_xg = kb.sb("xg", [128, 4, D], F32)
        hT, B_hT = kb.sb("hT", [128, KC, GT], BF16)
        NSLOT = 2
        wsl = [kb.sb("ws%d" % i, [128, 8192], BF16) for i in range(NSLOT)]
        merged, B_mg = kb.sb("merged", [128, KC, GT], BF16)
        sig, B_sig = kb.sb("sig", [128, 4, GT], F32)
        tmpf, B_tmpf = kb.sb("tmpf", [128, 2, GT], F32)


        if DBG_STOP == "p2":
            return nc
        steps = []

        def run_steps():
            emitted = [0]

            def emit_load(i):
                loads = steps[i][0]
                if not loads:
                    return
                sl, Bsl = wsl[steps[i][2] % NSLOT]
                for off, src, nk, c, Bsrc in loads:
                    dst = sl[:, off:off + nk * c].rearrange("p (k c) -> p k c", c=c)
                    kb.dma(sp, dst, src.rearrange("(k p) c -> p k c", p=128), [Bsrc], [Bsl], Bsl)

            widx = 0
            for i, st in enumerate(steps):
                if st[0]:
                    steps[i] = (st[0], st[1], widx)
                    widx += 1
                else:
                    steps[i] = (st[0], st[1], -1)
            n = len(steps)
            nxt = 0
            for i in range(n):
                ahead = 0
                j = i
                while j < n and ahead < NSLOT - 1:
                    if steps[j][0]:
                        ahead += 1
                        if j >= nxt:
                            emit_load(j)
                            nxt = j + 1
                    j += 1
                nxt = max(nxt, i + 1) if not steps[i][0] else nxt
                if steps[i][0]:
                    sl, Bsl = wsl[steps[i][2] % NSLOT]
                    steps[i][1](sl, Bsl)
                else:
                    steps[i][1](None, None)

        def group(gi, NQ, x_src, row0, halo_kind, mkT, B_mk, mv, B_mv, kt_scr, B_kt, v_scr, B_v, blocks, conv_out):
            ntile = (NQ + 127) // 128
            tl = [(t, min(128, NQ - t * 128)) for t in range(ntile)]
            st = {}

            def s_load(_, __):
                for t, nt in tl:
                    kb.dma(sp, xg[:nt, t, :], x_src[t * 128:t * 128 + nt, :], [], [B_xg], B_xg)
                for t, nt in tl:
                    rmsnorm_T(xg[:nt, t, :], B_xg, nt, G_MIX, hT, B_hT, t * 128, xn, B_xn, sq, B_sq, 16)
            steps.append(([], s_load))

            for which, c0, outd in ((0, C_K, k_o), (1, C_V, v_o)):
                for cb in range(4):
                    def s_kvout(sl, Bsl, cb=cb, outd=outd):
                        w = sl[:, 0:KC * 512].rearrange("p (k c) -> p k c", c=512)
                        for t, nt in tl:
                            b = next_ps()
                            mm_acc(b, ps[b][:nt, :], [(hT[:, k, t * 128:t * 128 + nt], w[:, k, :]) for k in range(KC)],
                                   [Bsl, B_hT])
                            evac(t, ostage[:nt, t % 2, 0:512], ps[b][:nt, :], [psb[b]], [B_ost])
                            kb.dma(pool, outd[row0 + t * 128:row0 + t * 128 + nt, cb * 512:(cb + 1) * 512],
                                   ostage[:nt, t % 2, 0:512], [B_ost], [], B_ost)
                    steps.append(([(0, s_win[:, c0 + cb * 512:c0 + (cb + 1) * 512], KC, 512, B_win["kv"])], s_kvout))

            if STAGE < 2:
                return
            is_s = (halo_kind == "cache")
            sc = {}

            def open_scope():
                kb.barrier()
                sc["es"] = ExitStack()
                sc["old"] = kb.es
                kb.es = sc["es"]

            def close_scope(_=None, __=None):
                kb.barrier()
                kb.es = sc["old"]
                sc["es"].close()

            def wstep(src, nk, Bsrc, fn, c=512):
                steps.append(([(0, src, nk, c, Bsrc)], fn))

            def branch_out(bidx, gname, wsrc_fn, wk_n, Bw, rhs_key):
                for cb in range(4):
                    def s_gate(sl, Bsl, cb=cb):
                        w = sl[:, 0:KC * 512].rearrange("p (k c) -> p k c", c=512)
                        for j in range(4):
                            b = next_ps()
                            mm_acc(b, ps[b][:, :NQ], [(w[:, k, j * 128:(j + 1) * 128], hT[:, k, :NQ]) for k in range(KC)],
                                   [Bsl, B_hT])
                            kb.op(act, lambda e, j=j, b=b: e.activation(out=sig[:, j, :NQ], in_=ps[b][:, :NQ],
                                                                        func=ACT.Sigmoid), [psb[b]], [B_sig])
                    wstep(s_win[:, C_G + bidx * D + cb * 512:C_G + bidx * D + (cb + 1) * 512], KC, B_win[gname], s_gate)

                    def s_y(sl, Bsl, cb=cb):
                        rhsT, BrhsT = sc[rhs_key]
                        w = sl[:, 0:wk_n * 512].rearrange("p (k c) -> p k c", c=512)
                        for j in range(4):
                            c = cb * 4 + j
                            b = next_ps()
                            mm_acc(b, ps[b][:, :NQ], [(w[:, k, j * 128:(j + 1) * 128], rhsT[:, k, :NQ]) for k in range(wk_n)],
                                   [Bsl, BrhsT])
                            if bidx == 0:
                                kb.op(dve, lambda e, j=j, b=b, c=c: e.tensor_tensor(out=merged[:, c, :NQ], in0=ps[b][:, :NQ],
                                                                                  in1=sig[:, j, :NQ], op=ALU.mult),
                                      [psb[b], B_sig], [B_mg])
                            else:
                                kb.op(dve, lambda e, j=j, b=b: e.tensor_tensor(out=tmpf[:, j % 2, :NQ], in0=ps[b][:, :NQ],
                                                                               in1=sig[:, j, :NQ], op=ALU.mult),
                                      [psb[b], B_sig], [B_tmpf])
                                kb.op(dve, lambda e, j=j, c=c: e.tensor_tensor(out=merged[:, c, :NQ], in0=merged[:, c, :NQ],
                                                                               in1=tmpf[:, j % 2, :NQ], op=ALU.add),
                                      [B_tmpf, B_mg], [B_mg])
                    wstep(wsrc_fn(cb), wk_n, Bw, s_y)

            def s_conv_open(_, __):
                open_scope()
                sc["uext"] = kb.sb("uext", [128, 8, 32 + GT], F32)
                sc["cacc"] = kb.sb("cacc", [128, 8, GT], F32)
                sc["ub"] = kb.sb("ub", [128, 8, 32 + GT], BF16)
                sc["zT"] = kb.sb("zT", [128, 8, GT], BF16)
                sc["hh"] = kb.sb("hh", [128, KC, 32], BF16)
                sc["st"] = kb.sb("cst", [128, 3, GT], F32)
                sc["identf"] = kb.sb("identf", [128, 128], F32)
                uext, B_u = sc["uext"]
                hh, B_hh = sc["hh"]
                cacc_, B_xh = sc["cacc"]
                xh = cacc_[:, 0:4, :].rearrange("p a c -> p (a c)")
                identf, B_if = sc["identf"]
                kb.op(dve, lambda e: e.tensor_copy(out=identf[:, :], in_=ident[:, :]), [B_c], [B_if])
                if not is_s:
                    kb.dma(sp, xh[:32, :], xhalo[gi], [], [B_xh], B_xh)
                    rmsnorm_T(xh[:32, :], B_xh, 32, G_MIX, hh, B_hh, 0, xn, B_xn, sq, B_sq, 16)
                else:
                    kb.op(dve, lambda e: e.memset(xh[:32, 0:DCONV], 0.0), [], [B_xh])
                    kb.dma(sp, xh[2:32, 0:DCONV], cconv[:, :], [], [B_xh], B_xh)
                    kb.op(act, lambda e: e.activation(out=xn[:32, 0:DCONV], in_=xh[:32, 0:DCONV], func=ACT.Copy),
                          [B_xh], [B_xn])
                    b = next_ps()
                    pv = ps[b][:].bitcast(BF16).rearrange("p (j c) -> p j c", c=128)
                    for cc in range(8):
                        kb.op(pe, lambda e, cc=cc, pv=pv: e.transpose(pv[:, cc, :32], xn[:32, cc * 128:(cc + 1) * 128],
                                                                      ident[:32, :32]), [B_xn, B_c], [psb[b]])
                    kb.op(dve, lambda e, pv=pv: e.tensor_copy(out=uext[:, :, 0:32], in_=pv[:, :, :32]), [psb[b]], [B_u])
            steps.append(([], s_conv_open))

            for half_i, cbase in ((0, C_AGT), (1, C_AGT + DCONV)):
                for cb in range(2):
                    def s_agt(sl, Bsl, half_i=half_i, cb=cb):
                        uext, B_u = sc["uext"]
                        hh, B_hh = sc["hh"]
                        w = sl[:, 0:KC * 512].rearrange("p (k c) -> p k c", c=512)
                        for j in range(4):
                            cc = cb * 4 + j
                            parts = [(32, NQ, hT, B_hT)]
                            if not is_s:
                                parts.append((0, 32, hh, B_hh))
                            for (o0, n, src, Bs) in parts:
                                b = next_ps()
                                mm_acc(b, ps[b][:, :n], [(w[:, k, j * 128:(j + 1) * 128], src[:, k, :n]) for k in range(KC)],
                                       [Bsl, Bs])
                                if half_i == 0:
                                    evac(j, uext[:, cc, o0:o0 + n], ps[b][:, :n], [psb[b]], [B_u])
                                else:
                                    kb.op(act, lambda e, b=b, n=n: e.activation(out=tmpf[:, 0, :n], in_=ps[b][:, :n],
                                                                                func=ACT.Sigmoid), [psb[b]], [B_tmpf])
                                    kb.op(dve, lambda e, cc=cc, o0=o0, n=n: e.tensor_tensor(
                                        out=uext[:, cc, o0:o0 + n], in0=uext[:, cc, o0:o0 + n], in1=tmpf[:, 0, :n],
                                        op=ALU.mult), [B_tmpf, B_u], [B_u])
                    wstep(s_win[:, cbase + cb * 512:cbase + (cb + 1) * 512], KC, B_win["agt"], s_agt)

            def s_conv_pre(_, __):
                uext, B_u = sc["uext"]
                identf, B_if = sc["identf"]
                ub, B_ub = sc["ub"]
                xh, B_xh = ostage[:, :, :].rearrange("p a c -> p (a c)"), B_ost
                kb.op(act, lambda e: e.activation(out=ub[:, :, 0:32 + NQ], in_=uext[:, :, 0:32 + NQ], func=ACT.Copy),
                      [B_u], [B_ub])
                if conv_out is not None:
                    for half in range(2):
                        b = next_ps()
                        for j in range(4):
                            cc = half * 4 + j
                            kb.op(pe, lambda e, cc=cc, b=b, j=j: e.transpose(ps[b][:32, j * 128:(j + 1) * 128],
                                                                            uext[:, cc, NQ:NQ + 32], identf[:, :]),
                                  [B_u, B_if], [psb[b]])
                        evac(half, xh[:32, half * 512:(half + 1) * 512], ps[b][:32, :], [psb[b]], [B_xh])
                    kb.dma(pool, conv_out[:, :], xh[2:32, 0:DCONV], [B_xh], [], B_xh)
            steps.append(([], s_conv_pre))
            for cc in range(8):
                def s_dw(sl, Bsl, cc=cc):
                    ub, B_ub = sc["ub"]
                    cacc, B_ca = sc["cacc"]
                    b = next_ps()
                    mm_acc(b, ps[b][:, :NQ], [(sl[:, k * 128:(k + 1) * 128], ub[:, cc, 2 + k:2 + k + NQ]) for k in range(31)],
                           [Bsl, B_ub])
                    kb.op(act, lambda e, b=b: e.activation(out=cacc[:, cc, :NQ], in_=ps[b][:, :NQ], func=ACT.Identity,
                                                           bias=pcv[:, 31, cc:cc + 1]), [psb[b], B_pcv], [B_ca])
                steps.append(([(0, s_dg[cc], 1, 31 * 128, dgB)], s_dw))

            def s_conv(_, __):
                uext, B_u = sc["uext"]
                cacc, B_ca = sc["cacc"]
                zT, B_z = sc["zT"]
                cst, B_st = sc["st"]
                ysq, B_ysq = sc["ub"]
                kb.op(act, lambda e: e.activation(out=zT[:, :, :NQ], in_=cacc[:, :, :NQ], func=ACT.Copy), [B_ca], [B_z])
                kb.op(act, lambda e: e.activation(out=ysq[:, :, :NQ], in_=cacc[:, :, :NQ], func=ACT.Square), [B_ca], [B_ysq])
                b1 = next_ps()
                mm_acc(b1, ps[b1][:, :NQ], [(ones[:, :], zT[:, k, :NQ]) for k in range(8)], [B_z, B_c])
                b2 = next_ps()
                mm_acc(b2, ps[b2][:, :NQ], [(ones[:, :], ysq[:, k, :NQ]) for k in range(8)], [B_ysq, B_c])
                mean, msq, var = cst[:, 0, :NQ], cst[:, 1, :NQ], cst[:, 2, :NQ]
                kb.op(dve, lambda e: e.tensor_scalar(out=mean, in0=ps[b1][:, :NQ], scalar1=1.0 / DCONV, scalar2=None,
                                                     op0=ALU.mult), [psb[b1]], [B_st])
                kb.op(dve, lambda e: e.tensor_tensor(out=msq, in0=mean, in1=mean, op=ALU.mult), [B_st], [B_st])
                kb.op(dve, lambda e: e.scalar_tensor_tensor(out=var, in0=ps[b2][:, :NQ], scalar=1.0 / DCONV, in1=msq,
                                                            op0=ALU.mult, op1=ALU.subtract), [psb[b2], B_st], [B_st])
                kb.op(act, lambda e: e.activation(out=var, in_=var, func=ACT.Sqrt, bias=EPS), [B_st], [B_st])
                kb.op(dve, lambda e: e.reciprocal(out=var, in_=var), [B_st], [B_st])
                for cc in range(8):
                    kb.op(dve, lambda e, cc=cc: e.tensor_tensor(out=cacc[:, cc, :NQ], in0=cacc[:, cc, :NQ], in1=mean,
                                                                op=ALU.subtract), [B_st, B_ca], [B_ca])
                    kb.op(dve, lambda e, cc=cc: e.tensor_tensor(out=cacc[:, cc, :NQ], in0=cacc[:, cc, :NQ], in1=var,
                                                                op=ALU.mult), [B_st, B_ca], [B_ca])
                    kb.op(act, lambda e, cc=cc: e.activation(out=zT[:, cc, :NQ], in_=cacc[:, cc, :NQ], func=ACT.Silu,
                                                             scale=pcv[:, 32, cc:cc + 1], bias=pcv[:, 33, cc:cc + 1]),
                          [B_ca, B_pcv, B_z], [B_z])
            steps.append(([], s_conv))
            branch_out(0, "g0", lambda cb: s_wco[:, cb * 512:(cb + 1) * 512], 8, B_wco, "zT")
            steps.append(([], close_scope))

            def s_mem_open(_, __):
                open_scope()
                sc["mqT"] = kb.sb("mqT", [128, 8, GT], BF16)
                sc["moT"] = kb.sb("moT", [128, 8, GT], BF16)
                sc["mP"] = kb.sb("mP", [128, 2, GT], BF16)
                sc["mr"] = kb.sb("mr", [128, GT], F32)
            steps.append(([], s_mem_open))
            for cb in range(2):
                def s_mq(sl, Bsl, cb=cb):
                    mqT, B_mq = sc["mqT"]
                    w = sl[:, 0:KC * 512].rearrange("p (k c) -> p k c", c=512)
                    for j in range(4):
                        b = next_ps()
                        mm_acc(b, ps[b][:, :NQ], [(w[:, k, j * 128:(j + 1) * 128], hT[:, k, :NQ]) for k in range(KC)],
                               [Bsl, B_hT])
                        evac(j, mqT[:, cb * 4 + j, :NQ], ps[b][:, :NQ], [psb[b]], [B_mq], scale=1.0 / 16.0)
                wstep(s_win[:, C_MQ + cb * 512:C_MQ + (cb + 1) * 512], KC, B_win["mq"], s_mq)

            def s_mem(_, __):
                mqT, B_mq = sc["mqT"]
                moT, B_mo = sc["moT"]
                mP, B_mP = sc["mP"]
                mr, B_mr = sc["mr"]
                for hd in range(4):
                    for mt in range(2):
                        b = next_ps()
                        mm_acc(b, ps[b][:, :NQ], [(mkT[:, hd * 2 + hf, mt * 128:(mt + 1) * 128], mqT[:, hd * 2 + hf, :NQ])
                                                  for hf in range(2)], [B_mk, B_mq])
                        kb.op(act, lambda e, b=b, mt=mt: e.activation(out=mP[:, mt, :NQ], in_=ps[b][:, :NQ], func=ACT.Exp),
                              [psb[b]], [B_mP])
                    bl = next_ps()
                    mm_acc(bl, ps[bl][:, :NQ], [(ones[:, :], mP[:, mt, :NQ]) for mt in range(2)], [B_mP, B_c])
                    kb.op(dve, lambda e, bl=bl: e.reciprocal(out=mr[:, :NQ], in_=ps[bl][:, :NQ]), [psb[bl]], [B_mr])
                    for dh in range(2):
                        b = next_ps()
                        mm_acc(b, ps[b][:, :NQ], [(mv[:, mt, hd * 256 + dh * 128:hd * 256 + (dh + 1) * 128], mP[:, mt, :NQ])
                                                  for mt in range(2)], [B_mv, B_mP])
                        kb.op(dve, lambda e, b=b, hd=hd, dh=dh: e.tensor_tensor(out=moT[:, hd * 2 + dh, :NQ], in0=ps[b][:, :NQ],
                                                                               in1=mr[:, :NQ], op=ALU.mult),
                              [psb[b], B_mr], [B_mo])
            steps.append(([], s_mem))
            branch_out(2, "g2", lambda cb: s_wmo[:, cb * 512:(cb + 1) * 512], 8, B_wmo, "moT")
            steps.append(([], close_scope))

            def s_att_open(_, __):
                open_scope()
                sc["qT"] = kb.sb("qT", [128, 16, GT], BF16)
                sc["onT"] = kb.sb("onT", [128, 16, GT], BF16)
                sc["kt"] = [kb.sb("akt%d" % i, [128, 2048], BF16) for i in range(2)]
                sc["vv"] = [kb.sb("avv%d" % i, [128, 16, 128], BF16) for i in range(2)]
                sc["P"] = [kb.sb("aP%d" % i, [128, GT], BF16) for i in range(4)]
                sc["fin"] = kb.sb("afin", [128, 4, GT], F32)
                sc["osq"] = kb.sb("aosq", [128, GT], BF16)
            steps.append(([], s_att_open))
            for cb in range(4):
                def s_q(sl, Bsl, cb=cb):
                    qT, B_q = sc["qT"]
                    w = sl[:, 0:KC * 512].rearrange("p (k c) -> p k c", c=512)
                    for j in range(4):
                        b = next_ps()
                        mm_acc(b, ps[b][:, :NQ], [(w[:, k, j * 128:(j + 1) * 128], hT[:, k, :NQ]) for k in range(KC)],
                               [Bsl, B_hT])
                        evac(j, qT[:, cb * 4 + j, :NQ], ps[b][:, :NQ], [psb[b]], [B_q], scale=0.125)
                wstep(s_win[:, C_Q + cb * 512:C_Q + (cb + 1) * 512], KC, B_win["q"], s_q)

            segs = []
            if not is_s:
                for s_ in range(gi):
                    segs.append((s_ * 2048, [(128, "full", None)] * 16))
                dblk = [(128, "diag", kb_) for kb_ in range(4)] + [(128, "bias", 1 + (bk - 4) // 4) for bk in range(4, 16)]
                segs.append((gi * 2048, dblk))
            else:
                nb_tot = PAST // 128
                k0 = 0
                while nb_tot > 0:
                    n = min(16, nb_tot)
                    segs.append((k0, [(128, "full", None)] * n))
                    k0 += n * 128
                    nb_tot -= n
                if len(segs[-1][1]) < 16:
                    segs[-1][1].append((NSAMP, "full", None))
                else:
                    segs.append((k0, [(NSAMP, "full", None)]))

            def s_att(_, __):
                qT, B_q = sc["qT"]
                onT, B_on = sc["onT"]
                fin, B_fin = sc["fin"]
                osq, B_osq = sc["osq"]
                ktv = kt_scr
                vvv = v_scr
                li = [0]
                pi = [0]
                nblk_all = sum(len(s_[1]) for s_ in segs)
                bO = [4, 5, 6, 7]
                r1, r2, t1, o = fin[:, 0, :NQ], fin[:, 1, :NQ], fin[:, 2, :NQ], fin[:, 3, :NQ]

                items = []
                for h in range(16):
                    bi = 0
                    for (k0, blks) in segs:
                        for bl, (kn, kind, arg) in enumerate(blks):
                            items.append(dict(h=h, k0=k0, blks=blks, bl=bl, kn=kn, kind=kind, arg=arg,
                                              first=(bi == 0), last=(bi == nblk_all - 1), bi=bi))
                            bi += 1

                def emit_S(it):
                    h = it["h"]
                    if it["bl"] == 0:
                        ktt, B_ktt = sc["kt"][li[0] % 2]
                        vvt, B_vvt = sc["vv"][li[0] % 2]
                        li[0] += 1
                        blks, k0 = it["blks"], it["k0"]
                        nk = sum(b_[0] for b_ in blks)
                        nfull = sum(1 for b_ in blks if b_[0] == 128)
                        kb.dma(sp, ktt[:, :nk], ktv[h, :, k0:k0 + nk], [B_kt], [B_ktt], B_ktt)
                        if nfull:
                            kb.dma(sp, vvt[:, :nfull, :], vvv[h, :, k0 // 128:k0 // 128 + nfull, :], [B_v], [B_vvt], B_vvt)
                        if nfull < len(blks):
                            kn_ = blks[-1][0]
                            kb.dma(sp, vvt[:kn_, nfull, :], vvv[h, :kn_, k0 // 128 + nfull, :], [B_v], [B_vvt], B_vvt)
                        cur["kt"] = (ktt, B_ktt)
                        cur["vv"] = (vvt, B_vvt)
                    ktt, B_ktt = cur["kt"]
                    it["vv"] = cur["vv"]
                    kn, kind, arg, bl = it["kn"], it["kind"], it["arg"], it["bl"]
                    c0 = 128 * arg if kind == "diag" else 0
                    it["c0"] = c0
                    it["P"] = []
                    for mp in range(2):
                        bS = next_ps("s")
                        p0 = mp * 64
                        kb.op(pe, lambda e, bS=bS, p0=p0: e.matmul(
                            ps[bS][:kn, c0:NQ], ktt[p0:p0 + 64, bl * 128:bl * 128 + kn], qT[p0:p0 + 64, h, c0:NQ],
                            start=True, stop=True), [B_ktt, B_q], [psb[bS]])
                        Pt, B_P = sc["P"][pi[0] % 4]
                        pi[0] += 1
                        it["P"].append((Pt, B_P))
                        if kind == "bias":
                            kb.op(act, lambda e, bS=bS, Pt=Pt: e.activation(
                                out=Pt[:kn, c0:NQ], in_=ps[bS][:kn, c0:NQ], func=ACT.Exp,
                                bias=cols[:kn, 8 + arg:9 + arg]), [psb[bS], B_cols], [B_P])
                        else:
                            kb.op(act, lambda e, bS=bS, Pt=Pt: e.activation(
                                out=Pt[:kn, c0:NQ], in_=ps[bS][:kn, c0:NQ], func=ACT.Exp), [psb[bS]], [B_P])
                        if kind == "diag":
                            kb.op(pool, lambda e, Pt=Pt: e.memset(Pt[64:128, c0:c0 + 64], 0.0), [], [B_P])

                def emit_PV(it):
                    kn, bl, c0, first, last = it["kn"], it["bl"], it["c0"], it["first"], it["last"]
                    vvt, B_vvt = it["vv"]
                    for mp in range(2):
                        Pt, B_P = it["P"][mp]
                        kb.op(pe, lambda e, mp=mp, Pt=Pt: e.matmul(
                            ps[bO[2 * mp]][:, c0:NQ], vvt[:kn, bl, :], Pt[:kn, c0:NQ], start=first, stop=last),
                            [B_vvt, B_P], [psb[bO[2 * mp]]])
                        kb.op(pe, lambda e, mp=mp, Pt=Pt: e.matmul(
                            ps[bO[2 * mp + 1]][:, c0:NQ], ones[:kn, :], Pt[:kn, c0:NQ], start=first, stop=last),
                            [B_c, B_P], [psb[bO[2 * mp + 1]]])

                def fin1():
                    kb.op(dve, lambda e: e.reciprocal(out=r1, in_=ps[5][:, :NQ]), [psb[5]], [B_fin])
                    kb.op(dve, lambda e: e.reciprocal(out=r2, in_=ps[7][:, :NQ]), [psb[7]], [B_fin])
                    kb.op(dve, lambda e: e.tensor_tensor(out=t1, in0=ps[4][:, :NQ], in1=r1, op=ALU.mult), [psb[4], B_fin], [B_fin])
                    kb.op(dve, lambda e: e.tensor_tensor(out=r2, in0=ps[6][:, :NQ], in1=r2, op=ALU.mult), [psb[6], B_fin], [B_fin])
                    kb.op(dve, lambda e: e.scalar_tensor_tensor(out=o, in0=r2, scalar=cols[:, 1:2], in1=t1, op0=ALU.mult,
                                                                op1=ALU.add), [B_fin, B_cols], [B_fin])
                    kb.op(act, lambda e: e.activation(out=osq[:, :NQ], in_=o, func=ACT.Square), [B_fin], [B_osq])

                def fin2(h):
                    bs_ = next_ps("s")
                    kb.op(pe, lambda e, bs_=bs_: e.matmul(ps[bs_][:, :NQ], ones[:, :], osq[:, :NQ], start=True, stop=True),
                          [B_osq, B_c], [psb[bs_]])
                    kb.op(act, lambda e, bs_=bs_: e.activation(out=r1, in_=ps[bs_][:, :NQ], func=ACT.Sqrt, scale=1.0 / 128,
                                                               bias=1e-5), [psb[bs_]], [B_fin])
                    kb.op(dve, lambda e: e.reciprocal(out=r1, in_=r1), [B_fin], [B_fin])
                    kb.op(dve, lambda e, h=h: e.scalar_tensor_tensor(out=onT[:, h, :NQ], in0=o, scalar=cols[:, 2:3], in1=r1,
                                                                     op0=ALU.mult, op1=ALU.mult), [B_fin, B_cols], [B_on])

                cur = {}
                n_it = len(items)
                pend_fin2 = None
                emit_S(items[0])
                for i in range(n_it):
                    it = items[i]
                    if i + 1 < n_it:
                        emit_S(items[i + 1])
                    emit_PV(it)
                    if pend_fin2 is not None and (it["bi"] >= min(1, nblk_all - 1)):
                        fin2(pend_fin2)
                        pend_fin2 = None
                    if it["last"]:
                        fin1()
                        pend_fin2 = it["h"]
                if pend_fin2 is not None:
                    fin2(pend_fin2)
            steps.append(([], s_att))
            branch_out(1, "g1", lambda cb: s_wao[:, cb * 512:(cb + 1) * 512], KC, B_wao, "onT")
            steps.append(([], close_scope))

            sc["merged"] = (merged, B_mg)
            for cb in range(4):
                def s_wout(sl, Bsl, cb=cb):
                    w = sl[:, 0:KC * 512].rearrange("p (k c) -> p k c", c=512)
                    for t, nt in tl:
                        b = next_ps()
                        mm_acc(b, ps[b][:nt, :], [(merged[:, k, t * 128:t * 128 + nt], w[:, k, :]) for k in range(KC)],
                               [Bsl, B_mg])
                        kb.op(dve, lambda e, b=b, t=t, nt=nt: e.tensor_tensor(
                            out=xg[:nt, t, cb * 512:(cb + 1) * 512], in0=xg[:nt, t, cb * 512:(cb + 1) * 512],
                            in1=ps[b][:nt, :], op=ALU.add), [psb[b], B_xg], [B_xg])
                wstep(s_wo[:, cb * 512:(cb + 1) * 512], KC, B_wo, s_wout)

            def s_moe_open(_, __):
                open_scope()
                sc["acc"] = kb.sb("macc", [128, 4, D], F32)
                sc["aT"] = [kb.sb("maT%d" % i, [128, 2, GT], BF16) for i in range(2)]
                sc["sg"] = [kb.sb("msg%d" % i, [128, GT], F32) for i in range(2)]
                sc["comb"] = kb.sb("mcomb", [128, 4, 32], F32)
                sc["rt"] = kb.sb("mrt", [128, 96], F32)
                sc["gfin"] = kb.sb("gfin", [128, D], F32)
                acc, B_acc = sc["acc"]
                comb, B_comb = sc["comb"]
                rt, B_rt = sc["rt"]
                gfin, B_gfin = sc["gfin"]
                kb.dma(sp, gfin[:, :], norm_final.partition_broadcast(128), [], [B_gfin], B_gfin)
                kb.op(pool, lambda e: e.memset(acc[:, :, :], 0.0), [], [B_acc])
                for t, nt in tl:
                    rmsnorm_T(xg[:nt, t, :], B_xg, nt, G_FFN, hT, B_hT, t * 128, xn, B_xn, sq, B_sq, 16)
                for t, nt in tl:
                    b = next_ps()
                    mm_acc(b, ps[b][:nt, :36], [(hT[:, k, t * 128:t * 128 + nt], wr_sb[:, k, :]) for k in range(KC)],
                           [B_hT, B_wrs])
                    lg = rt[:nt, 0:36]
                    gmax, ngmax, gsum, emax, nemax, m2, den = (rt[:nt, 36 + i:37 + i] for i in range(7))
                    gmask = rt[:nt, 44:48]
                    pen = rt[:nt, 48:52]
                    ge = rt[:nt, 52:56]
                    ee = rt[:nt, 56:88]
                    kb.op(dve, lambda e, b=b, nt=nt, lg=lg: e.tensor_tensor(out=lg, in0=ps[b][:nt, :36], in1=rbias[:nt, :],
                                                                         op=ALU.add), [psb[b], B_rb], [B_rt])
                    kb.op(dve, lambda e, lg=lg, gmax=gmax: e.reduce_max(out=gmax, in_=lg[:, 0:4], axis=AX.X), [B_rt], [B_rt])
                    kb.op(dve, lambda e, gmax=gmax, ngmax=ngmax: e.tensor_scalar(out=ngmax, in0=gmax, scalar1=-1.0, scalar2=None,
                                                                                 op0=ALU.mult), [B_rt], [B_rt])
                    kb.op(dve, lambda e, lg=lg, gmax=gmax, gmask=gmask: e.tensor_scalar(out=gmask, in0=lg[:, 0:4], scalar1=gmax,
                                                                                      scalar2=None, op0=ALU.is_ge),
                          [B_rt], [B_rt])
                    kb.op(dve, lambda e, gsum=gsum: e.memset(gsum, 0.0), [], [B_rt])
                    kb.op(act, lambda e, lg=lg, ge=ge, ngmax=ngmax, gsum=gsum: e.activation(
                        out=ge, in_=lg[:, 0:4], func=ACT.Exp, bias=ngmax, accum_out=gsum), [B_rt], [B_rt])
                    kb.op(dve, lambda e, gmask=gmask, pen=pen: e.tensor_scalar(out=pen, in0=gmask, scalar1=-1.0, scalar2=1e30,
                                                                              op0=ALU.add, op1=ALU.mult), [B_rt], [B_rt])
                    for g in range(4):
                        kb.op(dve, lambda e, g=g, lg=lg, pen=pen, ee=ee: e.tensor_scalar(
                            out=ee[:, g * 8:(g + 1) * 8], in0=lg[:, 4 + g * 8:12 + g * 8], scalar1=pen[:, g:g + 1],
                            scalar2=None, op0=ALU.add), [B_rt], [B_rt])
                    kb.op(dve, lambda e, ee=ee, emax=emax: e.reduce_max(out=emax, in_=ee, axis=AX.X), [B_rt], [B_rt])
                    kb.op(dve, lambda e, emax=emax, nemax=nemax: e.tensor_scalar(out=nemax, in0=emax, scalar1=-1.0, scalar2=None,
                                                                                 op0=ALU.mult), [B_rt], [B_rt])
                    kb.op(act, lambda e, ee=ee, nemax=nemax: e.activation(out=ee, in_=ee, func=ACT.Exp, bias=nemax),
                          [B_rt], [B_rt])
                    e2 = rt[:nt, 0:32]
                    kb.op(dve, lambda e, ee=ee, e2=e2: e.scalar_tensor_tensor(out=e2, in0=ee, scalar=1.0, in1=ee, op0=ALU.is_lt,
                                                                             op1=ALU.mult), [B_rt], [B_rt])
                    kb.op(dve, lambda e, e2=e2, m2=m2: e.reduce_max(out=m2, in_=e2, axis=AX.X), [B_rt], [B_rt])
                    kb.op(dve, lambda e, m2=m2, den=den, gsum=gsum: e.scalar_tensor_tensor(
                        out=den, in0=m2, scalar=1.0, in1=gsum, op0=ALU.add, op1=ALU.mult), [B_rt], [B_rt])
                    kb.op(dve, lambda e, den=den: e.reciprocal(out=den, in_=den), [B_rt], [B_rt])
                    kb.op(dve, lambda e, ee=ee, e2=e2, m2=m2: e.scalar_tensor_tensor(out=e2, in0=ee, scalar=m2, in1=ee,
                                                                                   op0=ALU.is_ge, op1=ALU.mult), [B_rt], [B_rt])
                    kb.op(dve, lambda e, e2=e2, den=den, t=t, nt=nt: e.tensor_scalar(out=comb[:nt, t, :], in0=e2, scalar1=den,
                                                                                    scalar2=None, op0=ALU.mult),
                          [B_rt], [B_comb])
            steps.append(([], s_moe_open))

            for ex in range(NEXP):
                def s_gu(sl, Bsl, ex=ex):
                    aT, B_aT = sc["aT"][ex % 2]
                    wg = sl[:, 0:KC * 256].rearrange("p (k c) -> p k c", c=256)
                    wu = sl[:, KC * 256:2 * KC * 256].rearrange("p (k c) -> p k c", c=256)
                    for hc in range(2):
                        sg, B_sg = sc["sg"][hc]
                        bg = next_ps()
                        mm_acc(bg, ps[bg][:, :NQ], [(wg[:, k, hc * 128:(hc + 1) * 128], hT[:, k, :NQ]) for k in range(KC)],
                               [Bsl, B_hT])
                        bu = next_ps()
                        mm_acc(bu, ps[bu][:, :NQ], [(wu[:, k, hc * 128:(hc + 1) * 128], hT[:, k, :NQ]) for k in range(KC)],
                               [Bsl, B_hT])
                        kb.op(act, lambda e, bg=bg, sg=sg: e.activation(out=sg[:, :NQ], in_=ps[bg][:, :NQ], func=ACT.Silu),
                              [psb[bg]], [B_sg])
                        kb.op(dve, lambda e, bu=bu, sg=sg, hc=hc, aT=aT: e.tensor_tensor(out=aT[:, hc, :NQ], in0=ps[bu][:, :NQ],
                                                                                        in1=sg[:, :NQ], op=ALU.mult),
                              [psb[bu], B_sg], [B_aT])
                steps.append(([(0, s_eg[ex], KC, 256, B_exp[ex // 4]), (KC * 256, s_eu[ex], KC, 256, B_exp[ex // 4])], s_gu))

                def s_dn(sl, Bsl, ex=ex):
                    aT, B_aT = sc["aT"][ex % 2]
                    acc, B_acc = sc["acc"]
                    comb, B_comb = sc["comb"]
                    wd = sl[:, 0:2 * D].rearrange("p (k c) -> p k c", c=D)
                    for t, nt in tl:
                        for cb in range(4):
                            b = next_ps()
                            mm_acc(b, ps[b][:nt, :], [(aT[:, hc, t * 128:t * 128 + nt], wd[:, hc, cb * 512:(cb + 1) * 512])
                                                      for hc in range(2)], [Bsl, B_aT])
                            kb.op(dve, lambda e, b=b, t=t, nt=nt, cb=cb: e.scalar_tensor_tensor(
                                out=acc[:nt, t, cb * 512:(cb + 1) * 512], in0=ps[b][:nt, :], scalar=comb[:nt, t, ex:ex + 1],
                                in1=acc[:nt, t, cb * 512:(cb + 1) * 512], op0=ALU.mult, op1=ALU.add),
                                [psb[b], B_comb, B_acc], [B_acc])
                steps.append(([(0, s_ed[ex], 2, D, B_exp[ex // 4])], s_dn))

            def s_final(_, __):
                acc, B_acc = sc["acc"]
                gfin, B_gfin = sc["gfin"]
                for t, nt in tl:
                    c_ss = cols[:nt, 20:21]
                    c_r = cols[:nt, 21:22]
                    kb.op(dve, lambda e, t=t, nt=nt: e.tensor_tensor(out=acc[:nt, t, :], in0=acc[:nt, t, :], in1=xg[:nt, t, :],
                                                                    op=ALU.add), [B_xg, B_acc], [B_acc])
                    kb.op(dve, lambda e, c_ss=c_ss: e.memset(c_ss, 0.0), [], [B_cols])
                    kb.op(act, lambda e, t=t, nt=nt, c_ss=c_ss: e.activation(out=sq[:nt, :], in_=acc[:nt, t, :], func=ACT.Square,
                                                                            accum_out=c_ss), [B_acc, B_cols], [B_sq, B_cols])
                    kb.op(act, lambda e, c_ss=c_ss, c_r=c_r: e.activation(out=c_r, in_=c_ss, func=ACT.Sqrt, scale=1.0 / D,
                                                                          bias=EPS), [B_cols], [B_cols])
                    kb.op(dve, lambda e, c_r=c_r: e.reciprocal(out=c_r, in_=c_r), [B_cols], [B_cols])
                    kb.op(dve, lambda e, t=t, nt=nt, c_r=c_r: e.scalar_tensor_tensor(
                        out=acc[:nt, t, :], in0=acc[:nt, t, :], scalar=c_r, in1=gfin[:nt, :], op0=ALU.mult, op1=ALU.mult),
                        [B_acc, B_cols, B_gfin], [B_acc])
                    kb.dma(pool, y_o[row0 + t * 128:row0 + t * 128 + nt, :], acc[:nt, t, :], [B_acc], [], B_acc)
            steps.append(([], s_final))
            steps.append(([], close_scope))

        for gi in range(NGRP_):
            blocks = None
            group(gi, GT, xfull[gi * 4 * GT:(gi * 4 + 1) * GT, :], gi * GT, "x", mkT_p, B_mkp, mv_p, B_mvp,
                  s_ktp, B_ktp, s_vp, B_vp, blocks, convp_o if gi == NGRP_ - 1 else None)
        def s_mem_sample(_, __):
            cmb = xn[:, :].rearrange("p (t c) -> p t c", c=1024)
            for t in range(2):
                kb.dma(pool, cmb[:, t, :], cmk[t * 128:(t + 1) * 128, :], [], [B_xn], B_xn)
                kb.dma(pool, mv_p[:, t, :], cmv[t * 128:(t + 1) * 128, :], [], [B_mvp], B_mvp)
            for t in range(2):
                b = next_ps()
                pv = ps[b][:].bitcast(BF16).rearrange("p (j c) -> p j c", c=128)
                for cc in range(8):
                    kb.op(pe, lambda e, cc=cc, pv=pv, t=t: e.transpose(pv[:, cc, :], cmb[:, t, cc * 128:(cc + 1) * 128],
                                                                      ident[:, :]), [B_xn, B_c], [psb[b]])
                evac(t, mkT_p[:, :, t * 128:(t + 1) * 128], pv[:, :, :], [psb[b]], [B_mkp])
        steps.append(([], s_mem_sample))
        group(NGRP_, NSAMP, xsamp, NGRP_ * GT, "cache", mkT_p, B_mkp, mv_p, B_mvp, s_kts, B_kts, s_vs, B_vs, None, convs_o)
        run_steps()

        kb.barrier()
    return nc


_NC_CACHE = {}


def kernel(**inp):
    inp = {k: np.asarray(v) for k, v in inp.items()}
    if "nc" not in _NC_CACHE:
        _NC_CACHE["nc"] = build_program()
    nc = _NC_CACHE["nc"]
    xp = inp["x_prompt"]
    in_maps = []
    wnames = ["norm_mix", "w_in", "w_dw", "b_dw", "conv_ln_g", "conv_ln_b", "w_conv_out", "lambda_q1", "lambda_k1",
              "lambda_q2", "lambda_k2", "subln_g", "w_attn_out", "norm_mem", "w_mem_k", "w_mem_v", "w_mem_out",
              "w_out", "norm_ffn", "w_router_grp", "b_router_grp", "w_router_exp", "b_router_exp", "w_exp_gate",
              "w_exp_up", "w_exp_down"]
    wts = {n: np.ascontiguousarray(inp[n][0]) for n in wnames}
    wts["norm_final"] = np.ascontiguousarray(inp["norm_final"])
    for c in range(8):
        b, j = c // 4, c % 4
        xb = xp[b].reshape(16, GT, D)
        order = []
        for i in range(4):
            order.append(4 * i + j)
            order += [4 * i + m for m in range(4) if m != j]
        xfull = np.ascontiguousarray(xb[order].reshape(SEQ, D))
        xhalo = np.zeros((NGRP, 32, D), np.float32)
        for i in range(4):
            g = 4 * i + j
            if g > 0:
                xhalo[i] = xp[b, g * GT - 32:g * GT]
        gp = np.array([j] + [m for m in range(4) if m != j], np.float32)
        m = dict(wts)
        m.update({
            "xfull": xfull, "xhalo": xhalo, "gpos": np.ascontiguousarray(np.broadcast_to(gp, (128, 4))),
            "xsamp": np.ascontiguousarray(inp["x_sample"][c]),
            "cconv": np.ascontiguousarray(inp["cache_conv"][0, c]),
            "ck": np.ascontiguousarray(inp["cache_diff_k"][0, c].reshape(PAST, D)),
            "cv": np.ascontiguousarray(inp["cache_diff_v"][0, c].reshape(PAST, D)),
            "cmk": np.ascontiguousarray(inp["cache_mem_k"][0, c].reshape(256, 1024)),
            "cmv": np.ascontiguousarray(inp["cache_mem_v"][0, c].reshape(256, 1024)),
            "memx": np.ascontiguousarray(inp["mem_prompt"][b]),
        })
        in_maps.append(m)
    res = run_bass_kernel_spmd(nc, in_maps, core_ids=list(range(8)))
    R = res.results
    y_p = np.zeros((2, SEQ, D), np.float32)
    k_p = np.zeros((1, 2, SEQ, 16, 128), np.float32)
    v_p = np.zeros((1, 2, SEQ, 16, 128), np.float32)
    y_s = np.zeros((8, NSAMP, D), np.float32)
    k_s = np.zeros((1, 8, NSAMP, 16, 128), np.float32)
    v_s = np.zeros((1, 8, NSAMP, 16, 128), np.float32)
    conv_p = np.zeros((1, 2, 30, DCONV), np.float32)
    conv_s = np.zeros((1, 8, 30, DCONV), np.float32)
    mk_p = np.zeros((1, 2, 256, 4, 256), np.float32)
    mv_p = np.zeros((1, 2, 256, 4, 256), np.float32)
    for c in range(8):
        b, j = c // 4, c % 4
        r = R[c]
        for i in range(4):
            g = 4 * i + j
            y_p[b, g * GT:(g + 1) * GT] = r["y"][i * GT:(i + 1) * GT]
            k_p[0, b, g * GT:(g + 1) * GT] = r["kout"][i * GT:(i + 1) * GT].reshape(GT, 16, 128)
            v_p[0, b, g * GT:(g + 1) * GT] = r["vout"][i * GT:(i + 1) * GT].reshape(GT, 16, 128)
        y_s[c] = r["y"][NGRP * GT:]
        k_s[0, c] = r["kout"][NGRP * GT:].reshape(NSAMP, 16, 128)
        v_s[0, c] = r["vout"][NGRP * GT:].reshape(NSAMP, 16, 128)
        conv_s[0, c] = r["convs"]
        if j == 3:
            conv_p[0, b] = r["convp"]
        if j == 0:
            mk_p[0, b] = r["memk"].reshape(256, 4, 256)
            mv_p[0, b] = r["memv"].reshape(256, 4, 256)
    return (y_p, y_s, conv_p, k_p, v_p, mk_p, mv_p, conv_s, k_s, v_s)
```

```python
import math
from contextlib import ExitStack

import numpy as np
import concourse.bass as bass
import concourse.mybir as mybir
from concourse.bass_utils import run_bass_kernel_spmd

F32 = mybir.dt.float32
BF16 = mybir.dt.bfloat16
ACT = mybir.ActivationFunctionType
ALU = mybir.AluOpType
AX = mybir.AxisListType

D = 2048
KC = 16
SEQ = 8192
NGRP = 4
DBG_STOP = None
GT = 512
DCONV = 1024
NSAMP = 64
PAST = 4096
NIN = 15360
NEXP = 32
DEXP = 256
EPS = 1e-6
NEG = -30000.0
C_AGT, C_Q, C_K, C_V, C_MQ, C_G = 0, 2048, 4096, 6144, 8192, 9216
STAGE = 2
MOE_SPLIT = True


class Buf:
    def __init__(self, name):
        self.name = name
        self.w = {}
        self.r = {}
        self.dsem = None
        self.dcount = 0


class Eng:
    def __init__(self, kb, e, name, is_pe=False):
        self.e = e
        self.name = name
        self.sem = kb.newsem("e_" + name)
        self.n = 0
        self.seen = {}
        self.is_pe = is_pe

    def wait(self, sem, val):
        if val <= 0:
            return
        if self.is_pe and sem is self.sem:
            return
        if self.seen.get(sem, 0) >= val:
            return
        self.e.wait_ge(sem, val)
        self.seen[sem] = val


class KB:
    def __init__(self, nc, es):
        self.nc = nc
        self.es = es
        self.sem_es = es
        self.nsem = 0
        self.pe = Eng(self, nc.tensor, "pe", True)
        self.act = Eng(self, nc.scalar, "act")
        self.dve = Eng(self, nc.vector, "dve")
        self.pool = Eng(self, nc.gpsimd, "pool")
        self.sp = Eng(self, nc.sync, "sp")
        self.engs = [self.pe, self.act, self.dve, self.pool, self.sp]
        self.dma_bufs = []

    def newsem(self, name):
        self.nsem += 1
        return self.sem_es.enter_context(self.nc.semaphore(name + "_%d" % self.nsem))

    def sb(self, name, shape, dt):
        self.nsb = getattr(self, "nsb", 0) + 1
        name = "%s_%d" % (name, self.nsb)
        t = self.es.enter_context(self.nc.sbuf_tensor(name, shape, dt))
        return t, Buf(name)

    def _deps(self, eng, reads, writes):
        for b in reads:
            for s, v in b.w.items():
                eng.wait(s, v)
        for b in writes:
            for s, v in b.w.items():
                eng.wait(s, v)
            for s, v in b.r.items():
                eng.wait(s, v)

    def _mark(self, sem, val, reads, writes):
        for b in reads:
            if b.r.get(sem, 0) < val:
                b.r[sem] = val
        for b in writes:
            if b.w.get(sem, 0) < val:
                b.w[sem] = val

    def op(self, eng, fn, reads=(), writes=()):
        self._deps(eng, reads, writes)
        ins = fn(eng.e)
        eng.n += 1
        ins.then_inc(eng.sem, 1)
        self._mark(eng.sem, eng.n, reads, writes)

    def dma(self, q, out, in_, reads, writes, owner, throttle=None):
        self._deps(q, reads, writes)
        if throttle is not None:
            hist, depth = throttle
            if len(hist) >= depth:
                ps_, pv_ = hist[len(hist) - depth]
                q.wait(ps_, pv_)
        if owner.dsem is None:
            owner.dsem = self.newsem("d_" + owner.name)
            self.dma_bufs.append(owner)
        ins = q.e.dma_start(out=out, in_=in_)
        owner.dcount += 16
        ins.then_inc(owner.dsem, 16)
        if throttle is not None:
            throttle[0].append((owner.dsem, owner.dcount))
        self._mark(owner.dsem, owner.dcount, reads, writes)

    def sync_bufs(self, bufs):
        for e in self.engs:
            for b in bufs:
                for sm, v in list(b.w.items()) + list(b.r.items()):
                    e.wait(sm, v)

    def barrier(self):
        for e in self.engs:
            for f in self.engs:
                if f is not e:
                    e.wait(f.sem, f.n)
            for b in self.dma_bufs:
                e.wait(b.dsem, b.dcount)


def build_program():
    nc = bass.Bass("TRN2", target_bir_lowering=False)

    def din(name, shape):
        return nc.dram_tensor(name, list(shape), F32, kind="ExternalInput").ap()

    def dout(name, shape):
        return nc.dram_tensor(name, list(shape), F32, kind="ExternalOutput").ap()

    def dscr(name, shape, dt=BF16):
        return nc.dram_tensor(name, list(shape), dt, kind="Internal").ap()

    xfull = din("xfull", [SEQ, D])
    xhalo = din("xhalo", [NGRP, 32, D])
    gpos = din("gpos", [128, 4])
    xsamp = din("xsamp", [NSAMP, D])
    cconv = din("cconv", [30, DCONV])
    ck = din("ck", [PAST, D])
    cv = din("cv", [PAST, D])
    cmk = din("cmk", [256, 1024])
    cmv = din("cmv", [256, 1024])
    memx = din("memx", [256, D])
    norm_mix = din("norm_mix", [D])
    w_in = din("w_in", [D, NIN])
    w_dw = din("w_dw", [31, DCONV])
    b_dw = din("b_dw", [DCONV])
    ln_g = din("conv_ln_g", [DCONV])
    ln_b = din("conv_ln_b", [DCONV])
    w_co = din("w_conv_out", [DCONV, D])
    lq1 = din("lambda_q1", [64])
    lk1 = din("lambda_k1", [64])
    lq2 = din("lambda_q2", [64])
    lk2 = din("lambda_k2", [64])
    subg = din("subln_g", [128])
    w_ao = din("w_attn_out", [D, D])
    norm_mem = din("norm_mem", [D])
    w_mk = din("w_mem_k", [D, 1024])
    w_mv = din("w_mem_v", [D, 1024])
    w_mo = din("w_mem_out", [1024, D])
    w_o = din("w_out", [D, D])
    norm_ffn = din("norm_ffn", [D])
    w_rg = din("w_router_grp", [D, 4])
    b_rg = din("b_router_grp", [4])
    w_re = din("w_router_exp", [D, 32])
    b_re = din("b_router_exp", [32])
    w_eg = din("w_exp_gate", [NEXP, D, DEXP])
    w_eu = din("w_exp_up", [NEXP, D, DEXP])
    w_ed = din("w_exp_down", [NEXP, DEXP, D])
    norm_final = din("norm_final", [D])

    NTOK = (SEQ // 2048) * GT + NSAMP
    y_o = dout("y", [NTOK, D])
    k_o = dout("kout", [NTOK, D])
    v_o = dout("vout", [NTOK, D])
    convp_o = dout("convp", [30, DCONV])
    convs_o = dout("convs", [30, DCONV])
    memk_o = dout("memk", [256, 1024])
    memv_o = dout("memv", [256, 1024])

    s_win = dscr("s_win", [D, NIN])
    s_wco = dscr("s_wco", [DCONV, D])
    s_wao = dscr("s_wao", [D, D])
    s_wmk = dscr("s_wmk", [D, 1024])
    s_wmv = dscr("s_wmv", [D, 1024])
    s_wmo = dscr("s_wmo", [1024, D])
    s_wo = dscr("s_wo", [D, D])
    s_wr = dscr("s_wr", [D, 36])
    s_eg = dscr("s_eg", [NEXP, D, DEXP])
    s_eu = dscr("s_eu", [NEXP, D, DEXP])
    s_ed = dscr("s_ed", [NEXP, DEXP, D])
    NBP = SEQ // 128
    NBS = PAST // 128 + 1
    NGRP_ = SEQ // 2048
    s_ktp = dscr("s_ktp", [16, 128, SEQ])
    s_vp = dscr("s_vp", [16, 128, NBP, 128])
    s_dg = dscr("s_dg", [8, 128, 31 * 128])
    s_kts = dscr("s_kts", [16, 128, NBS * 128])
    s_vs = dscr("s_vs", [16, 128, NBS, 128])

    with ExitStack() as es:
        es.enter_context(nc.allow_non_contiguous_dma(reason="small parameter loads"))
        kb = KB(nc, es)
        pe, act, dve, pool, sp = kb.pe, kb.act, kb.dve, kb.pool, kb.sp

        ps = []
        psb = []
        for i in range(8):
            t = es.enter_context(nc.psum_tensor("ps%d" % i, [128, 512], F32))
            ps.append(t)
            psb.append(Buf("ps%d" % i))
        rr = {"a": 0, "s": 0}

        def next_ps(pool_name="a"):
            if pool_name == "a":
                i = rr["a"] % 8
                rr["a"] += 1
            else:
                i = rr["s"] % 4
                rr["s"] += 1
            return i

        if DBG_STOP == "pm1":
            kb.dma(sp, y_o[0:128, :], xfull[0:128, :], [], [], Buf("t"))
            kb.barrier()
            return nc
        if DBG_STOP == "p0a":
            kb.barrier()
            return nc
        ident, B_c = kb.sb("ident", [128, 128], BF16)
        ones, _ = kb.sb("ones", [128, 128], BF16)
        kb.op(pool, lambda e: e.memset(ident[:], 0.0), [], [B_c])
        kb.op(pool, lambda e: e.affine_select(out=ident[:], in_=ident[:], pattern=[[-1, 128]],
                                              compare_op=ALU.not_equal, fill=1.0, base=0,
                                              channel_multiplier=1), [B_c], [B_c])
        kb.op(pool, lambda e: e.memset(ones[:], 1.0), [], [B_c])

        cols, B_cols = kb.sb("cols", [128, 64], F32)
        kb.op(dve, lambda e: e.memset(cols[:], 0.0), [], [B_cols])
        pfm, B_pfm = kb.sb("pfm", [128, 3, 16], F32)
        for i, src in enumerate((norm_mix, norm_mem, norm_ffn)):
            kb.dma(sp, pfm[:, i, :], src.rearrange("(k p) -> p k", p=128), [], [B_pfm], B_pfm)
        pcv, B_pcv = kb.sb("pcv", [128, 34 + 3, 8], F32)
        kb.dma(sp, pcv[:, 0:31, :], w_dw.rearrange("t (k p) -> p t k", p=128), [], [B_pcv], B_pcv)
        kb.dma(sp, pcv[:, 31, :], b_dw.rearrange("(k p) -> p k", p=128), [], [B_pcv], B_pcv)
        kb.dma(sp, pcv[:, 32, :], ln_g.rearrange("(k p) -> p k", p=128), [], [B_pcv], B_pcv)
        kb.dma(sp, pcv[:, 33, :], ln_b.rearrange("(k p) -> p k", p=128), [], [B_pcv], B_pcv)
        lamt, B_lam = kb.sb("lamt", [128, 4, 64], F32)
        for i, src in enumerate((lq1, lk1, lq2, lk2)):
            kb.dma(sp, lamt[:, i, :], src.partition_broadcast(128), [], [B_lam], B_lam)
        subgc, B_subg = kb.sb("subgc", [128, 1], F32)
        kb.dma(sp, subgc[:, :], subg.rearrange("(p o) -> p o", o=1), [], [B_subg], B_subg)
        gpt, B_gp = kb.sb("gpt", [128, 4], F32)
        kb.dma(sp, gpt[:, :], gpos[:, :], [], [B_gp], B_gp)
        rbias, B_rb = kb.sb("rbias", [128, 36], F32)
        kb.dma(sp, rbias[:, 0:4], b_rg.partition_broadcast(128), [], [B_rb], B_rb)
        kb.dma(sp, rbias[:, 4:36], b_re.partition_broadcast(128), [], [B_rb], B_rb)
        wr_f, B_wrf = kb.sb("wr_f", [128, KC, 36], F32)
        wr_sb, B_wrs = kb.sb("wr_sb", [128, KC, 36], BF16)
        kb.dma(sp, wr_f[:, :, 0:4], w_rg.rearrange("(k p) c -> p k c", p=128), [], [B_wrf], B_wrf)
        kb.dma(sp, wr_f[:, :, 4:36], w_re.rearrange("(k p) c -> p k c", p=128), [], [B_wrf], B_wrf)
        kb.op(dve, lambda e: e.tensor_copy(out=wr_sb[:, :, :], in_=wr_f[:, :, :]), [B_wrf], [B_wrs])

        gbc, B_gbc = kb.sb("gbc", [128, 2, KC, 128], BF16)

        def fill_gbc(dst, src_i):
            for k in range(KC):
                kb.op(dve, lambda e, k=k: e.tensor_copy(out=dst[:, k, :], in_=pfm[:, src_i, k:k + 1].to_broadcast([128, 128])),
                      [B_pfm], [B_gbc])
        fill_gbc(gbc[:, 0, :, :], 0)
        fill_gbc(gbc[:, 1, :, :], 2)
        G_MIX, G_FFN = gbc[:, 0, :, :], gbc[:, 1, :, :]
        lam_init = 0.8 - 0.6 * math.exp(-0.3 * 0)
        ltmp, B_lt = kb.sb("ltmp", [128, 2, 64], F32)
        kb.op(dve, lambda e: e.tensor_tensor(out=ltmp[:, 0, :], in0=lamt[:, 0, :], in1=lamt[:, 1, :], op=ALU.mult),
              [B_lam], [B_lt])
        kb.op(dve, lambda e: e.tensor_tensor(out=ltmp[:, 1, :], in0=lamt[:, 2, :], in1=lamt[:, 3, :], op=ALU.mult),
              [B_lam], [B_lt])
        kb.op(dve, lambda e: e.reduce_sum(out=cols[:, 4:6], in_=ltmp[:, :, :], axis=AX.X), [B_lt], [B_cols])
        kb.op(act, lambda e: e.activation(out=cols[:, 6:8], in_=cols[:, 4:6], func=ACT.Exp), [B_cols], [B_cols])
        kb.op(dve, lambda e: e.tensor_tensor(out=cols[:, 0:1], in0=cols[:, 6:7], in1=cols[:, 7:8], op=ALU.subtract),
              [B_cols], [B_cols])
        kb.op(dve, lambda e: e.tensor_scalar(out=cols[:, 0:1], in0=cols[:, 0:1], scalar1=lam_init, scalar2=None,
                                             op0=ALU.add), [B_cols], [B_cols])
        kb.op(dve, lambda e: e.tensor_scalar(out=cols[:, 1:2], in0=cols[:, 0:1], scalar1=-1.0, scalar2=None,
                                             op0=ALU.mult), [B_cols], [B_cols])
        kb.op(dve, lambda e: e.tensor_scalar(out=cols[:, 2:3], in0=subgc[:, 0:1], scalar1=(1.0 - lam_init),
                                             scalar2=None, op0=ALU.mult), [B_subg, B_cols], [B_cols])
        for m in range(1, 4):
            kb.op(dve, lambda e, m=m: e.tensor_scalar(out=cols[:, 8 + m:9 + m], in0=gpt[:, m:m + 1],
                                                      scalar1=gpt[:, 0:1], scalar2=NEG, op0=ALU.is_gt,
                                                      op1=ALU.mult), [B_gp, B_cols], [B_cols])

        B_win = {}
        cth = ([], 2)
        for nm, c0, c1 in (("kv", C_K, C_MQ), ("agt", C_AGT, C_Q), ("g0", C_G, C_G + D), ("mq", C_MQ, C_G),
                           ("g2", C_G + 2 * D, NIN), ("q", C_Q, C_K), ("g1", C_G + D, C_G + 2 * D)):
            b = Buf("cw_" + nm)
            B_win[nm] = b
            for r in range(0, D, 512):
                kb.dma(pool, s_win[r:r + 512, c0:c1], w_in[r:r + 512, c0:c1], [], [b], b, cth)

        def cast_simple(name, dst, src, rows):
            b = Buf("cw_" + name)
            step = 512
            for r in range(0, rows, step):
                kb.dma(pool, dst[r:r + step, :], src[r:r + step, :], [], [b], b, cth)
            return b

        B_wmk = cast_simple("wmk", s_wmk, w_mk, D)
        B_wmv = cast_simple("wmv", s_wmv, w_mv, D)
        B_wco = cast_simple("wco", s_wco, w_co, DCONV)
        B_wao = cast_simple("wao", s_wao, w_ao, D)
        B_wmo = cast_simple("wmo", s_wmo, w_mo, 1024)
        B_wo = cast_simple("wo", s_wo, w_o, D)
        B_exp = []
        for g8 in range(NEXP // 4):
            b = Buf("cw_e%d" % g8)
            B_exp.append(b)
            for e in range(g8 * 4, g8 * 4 + 4):
                kb.dma(pool, s_eg[e], w_eg[e], [], [b], b, cth)
                kb.dma(pool, s_eu[e], w_eu[e], [], [b], b, cth)
                kb.dma(pool, s_ed[e], w_ed[e], [], [b], b, cth)

        if DBG_STOP == "p0":
            kb.barrier()
            return nc
        def rmsnorm_T(xt, Bx, nt, gi, hT, BhT, t0, xn, Bxn, sq, Bsq, col0):
            c_ss = cols[:nt, col0:col0 + 1]
            c_r = cols[:nt, col0 + 1:col0 + 2]
            kb.op(dve, lambda e: e.memset(c_ss, 0.0), [], [B_cols])
            kb.op(act, lambda e: e.activation(out=sq[:nt, :], in_=xt, func=ACT.Square, accum_out=c_ss),
                  [Bx, B_cols], [Bsq, B_cols])
            kb.op(act, lambda e: e.activation(out=c_r, in_=c_ss, func=ACT.Sqrt, scale=1.0 / D, bias=EPS),
                  [B_cols], [B_cols])
            kb.op(dve, lambda e: e.reciprocal(out=c_r, in_=c_r), [B_cols], [B_cols])
            kb.op(act, lambda e: e.activation(out=xn[:nt, :], in_=xt, func=ACT.Copy, scale=c_r),
                  [Bx, B_cols], [Bxn])
            for half in range(2):
                b = next_ps()
                pv = ps[b][:].bitcast(BF16).rearrange("p (j c) -> p j c", c=128)
                for j in range(8):
                    k = half * 8 + j
                    kb.op(pe, lambda e, j=j, k=k: e.transpose(pv[:, j, :nt], xn[:nt, k * 128:(k + 1) * 128],
                                                              ident[:nt, :nt]), [Bxn, B_c], [psb[b]])
                kb.op(dve, lambda e, half=half, pv=pv: e.tensor_tensor(
                    out=hT[:, half * 8:half * 8 + 8, t0:t0 + nt], in0=pv[:, :, :nt],
                    in1=gi[:, half * 8:half * 8 + 8, :nt], op=ALU.mult), [psb[b], B_gbc], [BhT])

        def evac(i, out_ap, in_ap, reads, writes, scale=None):
            if i % 2 == 0:
                if scale is None:
                    kb.op(act, lambda e: e.activation(out=out_ap, in_=in_ap, func=ACT.Copy), reads, writes)
                else:
                    kb.op(act, lambda e: e.activation(out=out_ap, in_=in_ap, func=ACT.Copy, scale=scale), reads, writes)
            else:
                if scale is None:
                    kb.op(dve, lambda e: e.tensor_copy(out=out_ap, in_=in_ap), reads, writes)
                else:
                    kb.op(dve, lambda e: e.tensor_scalar(out=out_ap, in0=in_ap, scalar1=scale, scalar2=None,
                                                         op0=ALU.mult), reads, writes)

        def mm_acc(b, out_ap, pairs, reads):
            n = len(pairs)
            for i, (l, r) in enumerate(pairs):
                kb.op(pe, lambda e, l=l, r=r, i=i: e.matmul(out_ap, l, r, start=(i == 0), stop=(i == n - 1)),
                      reads, [psb[b]])

        B_ktp, B_vp, B_kts, B_vs = Buf("ktp"), Buf("vp"), Buf("kts"), Buf("vs")
        dgB = Buf("s_dg")
        with ExitStack() as es0:
            kb_es = kb.es
            kb.es = es0
            B_dg = dgB
            dgs = [kb.sb("dgs%d" % i, [128, 31, 128], BF16) for i in range(2)]
            for cc in range(8):
                dg_, Bdg_ = dgs[cc % 2]
                for k in range(31):
                    kb.op(dve, lambda e, cc=cc, k=k, dg_=dg_: e.tensor_scalar(out=dg_[:, k, :], in0=ident[:, :],
                                                                            scalar1=pcv[:, k, cc:cc + 1], scalar2=None,
                                                                            op0=ALU.mult), [B_c, B_pcv], [Bdg_])
                kb.dma(sp, s_dg[cc], dg_[:, :, :].rearrange("p k c -> p (k c)"), [Bdg_], [B_dg], Bdg_)
            kb.sync_bufs([d_[1] for d_ in dgs])
            kb.es = kb_es
        with ExitStack() as es1:
            kb_es = kb.es
            kb.es = es1
            p1w = [kb.sb("p1w%d" % i, [128, KC, 512], BF16) for i in range(3)]
            xs = [kb.sb("p1x%d" % i, [128, D], F32) for i in range(2)]
            xn1, B_xn1 = kb.sb("p1xn", [128, D], BF16)
            sq1, B_sq1 = xn1, B_xn1
            CH = 1024
            hTs = [kb.sb("p1hT%d" % i, [128, KC, CH], BF16) for i in range(2)]
            kts = [kb.sb("p1kt%d" % i, [128, 4, GT], BF16) for i in range(2)]
            vss = [kb.sb("p1v%d" % i, [128, 4, 512], BF16) for i in range(2)]
            ckb = [kb.sb("p1ck%d" % i, [128, D], BF16) for i in range(2)]
            ckf = [kb.sb("p1ckf%d" % i, [128, D], F32) for i in range(2)]
            ktc = [kb.sb("p1ktc%d" % i, [128, 16, 256], BF16) for i in range(2)]
            xi = [0]
            ki = [0]
            wq = {"n": 0, "loaded": 0}
            NCHK = SEQ // CH
            NCH = NCHK + 1
            vp_v = s_vp.rearrange("h p nb d -> p h nb d")
            vs_v = s_vs.rearrange("h p nb d -> p h nb d")
            ktp_v = s_ktp.rearrange("h p n -> p h n")
            kts_v = s_kts.rearrange("h p n -> p h n")

            def p1_wload(upto):
                while wq["loaded"] < min(upto, NCH * 8):
                    i = wq["loaded"]
                    blk = i % 8
                    w, Bw = p1w[i % 3]
                    kb.dma(sp, w[:, :, :], s_win[:, C_K + blk * 512:C_K + (blk + 1) * 512].rearrange(
                        "(k p) c -> p k c", p=128), [B_win["kv"]], [Bw], Bw)
                    wq["loaded"] += 1

            def kv_norm(ci, src_rows, ntok):
                hT, BhT = hTs[ci % 2]
                ntile = (ntok + 127) // 128
                for t in range(ntile):
                    nt = min(128, ntok - t * 128)
                    xt, Bx = xs[xi[0] % 2]
                    xi[0] += 1
                    kb.dma(sp, xt[:nt, :], src_rows[t * 128:t * 128 + nt, :], [], [Bx], Bx)
                    rmsnorm_T(xt[:nt, :], Bx, nt, G_MIX, hT, BhT, t * 128, xn1, B_xn1, sq1, B_sq1, 16)

            cache_nb = [0]

            def cache_blocks(n):
                for _ in range(n):
                    nb = cache_nb[0]
                    if nb >= PAST // 128:
                        return
                    cache_nb[0] += 1
                    ct, Bct = ckb[nb % 2]
                    kt, Bk = ktc[(nb // 2) % 2]
                    cf, Bcf = ckf[nb % 2]
                    kb.dma(sp, cf[:, :], ck[nb * 128:(nb + 1) * 128, :], [], [Bcf], Bcf)
                    kb.op(act, lambda e, ct=ct, cf=cf: e.activation(out=ct[:, :], in_=cf[:, :], func=ACT.Copy), [Bcf], [Bct])
                    for half in range(2):
                        b = next_ps()
                        pv = ps[b][:].bitcast(BF16).rearrange("p (j c) -> p j c", c=128)
                        for j in range(8):
                            h = half * 8 + j
                            kb.op(pe, lambda e, j=j, h=h, pv=pv, ct=ct: e.transpose(pv[:, j, :], ct[:, h * 128:(h + 1) * 128],
                                                                                  ident[:, :]), [Bct, B_c], [psb[b]])
                        evac(half, kt[:, half * 8:half * 8 + 8, (nb % 2) * 128:(nb % 2 + 1) * 128], pv[:, :, :],
                             [psb[b]], [Bk])
                    if nb % 2 == 1:
                        kb.dma(sp, kts_v[:, :, (nb - 1) * 128:(nb + 1) * 128], kt[:, :, :], [Bk], [B_kts], Bk)

            def kv_chunk(ci, ntok, kt_dst_fn, v_dst_fn, Bkt, Bv, mid_fn=None, ncache=0):
                hT, BhT = hTs[ci % 2]
                for blk in range(8):
                    if blk == 3 and mid_fn is not None:
                        mid_fn()
                    if ncache and blk % 2 == 1:
                        cache_blocks(ncache)
                    i = wq["n"]
                    wq["n"] += 1
                    p1_wload(i + 2)
                    w, Bw = p1w[i % 3]
                    for c0 in range(0, ntok, GT):
                        nq = min(GT, ntok - c0)
                        ntile = (nq + 127) // 128
                        if blk < 4:
                            kt, Bk = kts[ki[0] % 2]
                            ki[0] += 1
                            for hh in range(4):
                                b = next_ps()
                                mm_acc(b, ps[b][:, :nq], [(w[:, k, hh * 128:(hh + 1) * 128], hT[:, k, c0:c0 + nq])
                                                          for k in range(KC)], [Bw, BhT])
                                evac(hh, kt[:, hh, :nq], ps[b][:, :nq], [psb[b]], [Bk])
                            kb.dma(sp, kt_dst_fn(blk, c0, nq), kt[:, :, :nq], [Bk], [Bkt], Bk)
                        else:
                            cb = blk - 4
                            vs_, Bvs = vss[ki[0] % 2]
                            ki[0] += 1
                            for t in range(ntile):
                                nt = min(128, nq - t * 128)
                                b = next_ps()
                                mm_acc(b, ps[b][:nt, :], [(hT[:, k, c0 + t * 128:c0 + t * 128 + nt], w[:, k, :])
                                                          for k in range(KC)], [Bw, BhT])
                                evac(t, vs_[:nt, t, :], ps[b][:nt, :], [psb[b]], [Bvs])
                                kb.dma(sp, v_dst_fn(c0 // 128 + t, nt, cb), vs_[:nt, t, :].rearrange("p (h d) -> p h d", d=128),
                                       [Bvs], [Bv], Bvs)

            kv_norm(0, xfull[0:CH, :], CH)
            for ci in range(NCHK):
                if ci + 1 < NCHK:
                    mid = lambda ci=ci: kv_norm(ci + 1, xfull[(ci + 1) * CH:(ci + 2) * CH, :], CH)
                else:
                    mid = lambda: kv_norm(NCHK, xsamp, NSAMP)
                kv_chunk(ci, CH,
                         lambda blk, c0, nq, ci=ci: ktp_v[:, blk * 4:blk * 4 + 4, ci * CH + c0:ci * CH + c0 + nq],
                         lambda tt, nt, cb, ci=ci: vp_v[:nt, cb * 4:cb * 4 + 4, ci * (CH // 128) + tt, :], B_ktp, B_vp, mid,
                         ncache=1)
            kv_chunk(NCHK, NSAMP, lambda blk, c0, nq: kts_v[:, blk * 4:blk * 4 + 4, PAST:PAST + NSAMP],
                     lambda tt, nt, cb: vs_v[:nt, cb * 4:cb * 4 + 4, NBS - 1, :], B_kts, B_vs)
            for nb0 in range(PAST // 128):
                kb.dma(pool, vs_v[:, :, nb0, :],
                       cv[nb0 * 128:(nb0 + 1) * 128, :].rearrange("p (h d) -> p h d", d=128),
                       [], [B_vs], B_vs)
            cache_blocks(PAST // 128)
            kb.barrier()
            kb.es = kb_es

        if DBG_STOP == "p1":
            return nc
        mkT_p, B_mkp = kb.sb("mkT_p", [128, 8, 256], BF16)
        mv_p, B_mvp = kb.sb("mv_p", [128, 2, 1024], BF16)

        xn, B_xn = kb.sb("xn", [128, D], BF16)
        sq, B_sq = xn, B_xn
        ostage, B_ost = kb.sb("ostage", [128, 2, 512], F32)
        with ExitStack() as es2:
            kb_es = kb.es
            kb.es = es2
            mx, B_mx = kb.sb("mx", [128, 2, D], F32)
            gmem, _ = kb.sb("gmem", [128, KC, 128], BF16)
            fill_gbc(gmem, 1)
            hmT, B_hmT = kb.sb("hmT", [128, KC, 256], BF16)
            wm, B_wm = kb.sb("wm", [128, KC, 1024], BF16)
            for t in range(2):
                kb.dma(sp, mx[:, t, :], memx[t * 128:(t + 1) * 128, :], [], [B_mx], B_mx)
            for t in range(2):
                rmsnorm_T(mx[:, t, :], B_mx, 128, gmem[:, :, :], hmT, B_hmT, t * 128, xn, B_xn, sq, B_sq, 16)
            if DBG_STOP == "p2a":
                kb.barrier()
                return nc
            for which, (sw, Bsw, outd) in enumerate(((s_wmk, B_wmk, memk_o), (s_wmv, B_wmv, memv_o))):
                if DBG_STOP == "p2b" and which == 1:
                    kb.barrier()
                    return nc
                kb.dma(sp, wm[:, :, :], sw.rearrange("(k p) c -> p k c", p=128), [Bsw], [B_wm], B_wm)
                if which == 0:
                    for cc in range(8):
                        b = next_ps()
                        mm_acc(b, ps[b][:, :256], [(wm[:, k, cc * 128:(cc + 1) * 128], hmT[:, k, :]) for k in range(KC)],
                               [B_wm, B_hmT])
                        evac(cc, mkT_p[:, cc, :], ps[b][:, :256], [psb[b]], [B_mkp])
                for t in range(2):
                    for cb in range(2):
                        b = next_ps()
                        mm_acc(b, ps[b][:, :], [(hmT[:, k, t * 128:(t + 1) * 128], wm[:, k, cb * 512:(cb + 1) * 512])
                                                for k in range(KC)], [B_wm, B_hmT])
                        evac(0, ostage[:, cb, :], ps[b][:, :], [psb[b]], [B_ost])
                        if which == 1:
                            kb.op(dve, lambda e, t=t, cb=cb: e.tensor_copy(out=mv_p[:, t, cb * 512:(cb + 1) * 512],
                                                                           in_=ostage[:, cb, :]), [B_ost], [B_mvp])
                        kb.dma(pool, outd[t * 128:(t + 1) * 128, cb * 512:(cb + 1) * 512], ostage[:, cb, :], [B_ost], [], B_ost)
            if DBG_STOP == "p2c":
                kb.barrier()
                return nc
            kb.barrier()
            kb.es = kb_es

        xg, B_xg = kb.sb("xg", [128, 4, D], F32)
        hT, B_hT = kb.sb("hT", [128, KC, GT], BF16)
        NSLOT = 2
        wsl = [kb.sb("ws%d" % i, [128, 8192], BF16) for i in range(NSLOT)]
        merged, B_mg = kb.sb("merged", [128, KC, GT], BF16)
        sig, B_sig = kb.sb("sig", [128, 4, GT], F32)
        tmpf, B_tmpf = kb.sb("tmpf", [128, 2, GT], F32)


        if DBG_STOP == "p2":
            return nc
        steps = []

        def run_steps():
            emitted = [0]

            def emit_load(i):
                loads = steps[i][0]
                if not loads:
                    return
                sl, Bsl = wsl[steps[i][2] % NSLOT]
                for off, src, nk, c, Bsrc in loads:
                    dst = sl[:, off:off + nk * c].rearrange("p (k c) -> p k c", c=c)
                    kb.dma(sp, dst, src.rearrange("(k p) c -> p k c", p=128), [Bsrc], [Bsl], Bsl)

            widx = 0
            for i, st in enumerate(steps):
                if st[0]:
                    steps[i] = (st[0], st[1], widx)
                    widx += 1
                else:
                    steps[i] = (st[0], st[1], -1)
            n = len(steps)
            nxt = 0
            for i in range(n):
                ahead = 0
                j = i
                while j < n and ahead < NSLOT - 1:
                    if steps[j][0]:
                        ahead += 1
                        if j >= nxt:
                            emit_load(j)
                            nxt = j + 1
                    j += 1
                nxt = max(nxt, i + 1) if not steps[i][0] else nxt
                if steps[i][0]:
                    sl, Bsl = wsl[steps[i][2] % NSLOT]
                    steps[i][1](sl, Bsl)
                else:
                    steps[i][1](None, None)

        def group(gi, NQ, x_src, row0, halo_kind, mkT, B_mk, mv, B_mv, kt_scr, B_kt, v_scr, B_v, blocks, conv_out):
            ntile = (NQ + 127) // 128
            tl = [(t, min(128, NQ - t * 128)) for t in range(ntile)]
            st = {}

            def s_load(_, __):
                for t, nt in tl:
                    kb.dma(sp, xg[:nt, t, :], x_src[t * 128:t * 128 + nt, :], [], [B_xg], B_xg)
                for t, nt in tl:
                    rmsnorm_T(xg[:nt, t, :], B_xg, nt, G_MIX, hT, B_hT, t * 128, xn, B_xn, sq, B_sq, 16)
            steps.append(([], s_load))

            for which, c0, outd in ((0, C_K, k_o), (1, C_V, v_o)):
                for cb in range(4):
                    def s_kvout(sl, Bsl, cb=cb, outd=outd):
                        w = sl[:, 0:KC * 512].rearrange("p (k c) -> p k c", c=512)
                        for t, nt in tl:
                            b = next_ps()
                            mm_acc(b, ps[b][:nt, :], [(hT[:, k, t * 128:t * 128 + nt], w[:, k, :]) for k in range(KC)],
                                   [Bsl, B_hT])
                            evac(t, ostage[:nt, t % 2, 0:512], ps[b][:nt, :], [psb[b]], [B_ost])
                            kb.dma(pool, outd[row0 + t * 128:row0 + t * 128 + nt, cb * 512:(cb + 1) * 512],
                                   ostage[:nt, t % 2, 0:512], [B_ost], [], B_ost)
                    steps.append(([(0, s_win[:, c0 + cb * 512:c0 + (cb + 1) * 512], KC, 512, B_win["kv"])], s_kvout))

            if STAGE < 2:
                return
            is_s = (halo_kind == "cache")
            sc = {}

            def open_scope():
                kb.barrier()
                sc["es"] = ExitStack()
                sc["old"] = kb.es
                kb.es = sc["es"]

            def close_scope(_=None, __=None):
                kb.barrier()
                kb.es = sc["old"]
                sc["es"].close()

            def wstep(src, nk, Bsrc, fn, c=512):
                steps.append(([(0, src, nk, c, Bsrc)], fn))

            def branch_out(bidx, gname, wsrc_fn, wk_n, Bw, rhs_key):
                for cb in range(4):
                    def s_gate(sl, Bsl, cb=cb):
                        w = sl[:, 0:KC * 512].rearrange("p (k c) -> p k c", c=512)
                        for j in range(4):
                            b = next_ps()
                            mm_acc(b, ps[b][:, :NQ], [(w[:, k, j * 128:(j + 1) * 128], hT[:, k, :NQ]) for k in range(KC)],
                                   [Bsl, B_hT])
                            kb.op(act, lambda e, j=j, b=b: e.activation(out=sig[:, j, :NQ], in_=ps[b][:, :NQ],
                                                                        func=ACT.Sigmoid), [psb[b]], [B_sig])
                    wstep(s_win[:, C_G + bidx * D + cb * 512:C_G + bidx * D + (cb + 1) * 512], KC, B_win[gname], s_gate)

                    def s_y(sl, Bsl, cb=cb):
                        rhsT, BrhsT = sc[rhs_key]
                        w = sl[:, 0:wk_n * 512].rearrange("p (k c) -> p k c", c=512)
                        for j in range(4):
                            c = cb * 4 + j
                            b = next_ps()
                            mm_acc(b, ps[b][:, :NQ], [(w[:, k, j * 128:(j + 1) * 128], rhsT[:, k, :NQ]) for k in range(wk_n)],
                                   [Bsl, BrhsT])
                            if bidx == 0:
                                kb.op(dve, lambda e, j=j, b=b, c=c: e.tensor_tensor(out=merged[:, c, :NQ], in0=ps[b][:, :NQ],
                                                                                  in1=sig[:, j, :NQ], op=ALU.mult),
                                      [psb[b], B_sig], [B_mg])
                            else:
                                kb.op(dve, lambda e, j=j, b=b: e.tensor_tensor(out=tmpf[:, j % 2, :NQ], in0=ps[b][:, :NQ],
                                                                               in1=sig[:, j, :NQ], op=ALU.mult),
                                      [psb[b], B_sig], [B_tmpf])
                                kb.op(dve, lambda e, j=j, c=c: e.tensor_tensor(out=merged[:, c, :NQ], in0=merged[:, c, :NQ],
                                                                               in1=tmpf[:, j % 2, :NQ], op=ALU.add),
                                      [B_tmpf, B_mg], [B_mg])
                    wstep(wsrc_fn(cb), wk_n, Bw, s_y)

            def s_conv_open(_, __):
                open_scope()
                sc["uext"] = kb.sb("uext", [128, 8, 32 + GT], F32)
                sc["cacc"] = kb.sb("cacc", [128, 8, GT], F32)
                sc["ub"] = kb.sb("ub", [128, 8, 32 + GT], BF16)
                sc["zT"] = kb.sb("zT", [128, 8, GT], BF16)
                sc["hh"] = kb.sb("hh", [128, KC, 32], BF16)
                sc["st"] = kb.sb("cst", [128, 3, GT], F32)
                sc["identf"] = kb.sb("identf", [128, 128], F32)
                uext, B_u = sc["uext"]
                hh, B_hh = sc["hh"]
                cacc_, B_xh = sc["cacc"]
                xh = cacc_[:, 0:4, :].rearrange("p a c -> p (a c)")
                identf, B_if = sc["identf"]
                kb.op(dve, lambda e: e.tensor_copy(out=identf[:, :], in_=ident[:, :]), [B_c], [B_if])
                if not is_s:
                    kb.dma(sp, xh[:32, :], xhalo[gi], [], [B_xh], B_xh)
                    rmsnorm_T(xh[:32, :], B_xh, 32, G_MIX, hh, B_hh, 0, xn, B_xn, sq, B_sq, 16)
                else:
                    kb.op(dve, lambda e: e.memset(xh[:32, 0:DCONV], 0.0), [], [B_xh])
                    kb.dma(sp, xh[2:32, 0:DCONV], cconv[:, :], [], [B_xh], B_xh)
                    kb.op(act, lambda e: e.activation(out=xn[:32, 0:DCONV], in_=xh[:32, 0:DCONV], func=ACT.Copy),
                          [B_xh], [B_xn])
                    b = next_ps()
                    pv = ps[b][:].bitcast(BF16).rearrange("p (j c) -> p j c", c=128)
                    for cc in range(8):
                        kb.op(pe, lambda e, cc=cc, pv=pv: e.transpose(pv[:, cc, :32], xn[:32, cc * 128:(cc + 1) * 128],
                                                                      ident[:32, :32]), [B_xn, B_c], [psb[b]])
                    kb.op(dve, lambda e, pv=pv: e.tensor_copy(out=uext[:, :, 0:32], in_=pv[:, :, :32]), [psb[b]], [B_u])
            steps.append(([], s_conv_open))

            for half_i, cbase in ((0, C_AGT), (1, C_AGT + DCONV)):
                for cb in range(2):
                    def s_agt(sl, Bsl, half_i=half_i, cb=cb):
                        uext, B_u = sc["uext"]
                        hh, B_hh = sc["hh"]
                        w = sl[:, 0:KC * 512].rearrange("p (k c) -> p k c", c=512)
                        for j in range(4):
                            cc = cb * 4 + j
                            parts = [(32, NQ, hT, B_hT)]
                            if not is_s:
                                parts.append((0, 32, hh, B_hh))
                            for (o0, n, src, Bs) in parts:
                                b = next_ps()
                                mm_acc(b, ps[b][:, :n], [(w[:, k, j * 128:(j + 1) * 128], src[:, k, :n]) for k in range(KC)],
                                       [Bsl, Bs])
                                if half_i == 0:
                                    evac(j, uext[:, cc, o0:o0 + n], ps[b][:, :n], [psb[b]], [B_u])
                                else:
                                    kb.op(act, lambda e, b=b, n=n: e.activation(out=tmpf[:, 0, :n], in_=ps[b][:, :n],
                                                                                func=ACT.Sigmoid), [psb[b]], [B_tmpf])
                                    kb.op(dve, lambda e, cc=cc, o0=o0, n=n: e.tensor_tensor(
                                        out=uext[:, cc, o0:o0 + n], in0=uext[:, cc, o0:o0 + n], in1=tmpf[:, 0, :n],
                                        op=ALU.mult), [B_tmpf, B_u], [B_u])
                    wstep(s_win[:, cbase + cb * 512:cbase + (cb + 1) * 512], KC, B_win["agt"], s_agt)

            def s_conv_pre(_, __):
                uext, B_u = sc["uext"]
                identf, B_if = sc["identf"]
                ub, B_ub = sc["ub"]
                xh, B_xh = ostage[:, :, :].rearrange("p a c -> p (a c)"), B_ost
                kb.op(act, lambda e: e.activation(out=ub[:, :, 0:32 + NQ], in_=uext[:, :, 0:32 + NQ], func=ACT.Copy),
                      [B_u], [B_ub])
                if conv_out is not None:
                    for half in range(2):
                        b = next_ps()
                        for j in range(4):
                            cc = half * 4 + j
                            kb.op(pe, lambda e, cc=cc, b=b, j=j: e.transpose(ps[b][:32, j * 128:(j + 1) * 128],
                                                                            uext[:, cc, NQ:NQ + 32], identf[:, :]),
                                  [B_u, B_if], [psb[b]])
                        evac(half, xh[:32, half * 512:(half + 1) * 512], ps[b][:32, :], [psb[b]], [B_xh])
                    kb.dma(pool, conv_out[:, :], xh[2:32, 0:DCONV], [B_xh], [], B_xh)
            steps.append(([], s_conv_pre))
            for cc in range(8):
                def s_dw(sl, Bsl, cc=cc):
                    ub, B_ub = sc["ub"]
                    cacc, B_ca = sc["cacc"]
                    b = next_ps()
                    mm_acc(b, ps[b][:, :NQ], [(sl[:, k * 128:(k + 1) * 128], ub[:, cc, 2 + k:2 + k + NQ]) for k in range(31)],
                           [Bsl, B_ub])
                    kb.op(act, lambda e, b=b: e.activation(out=cacc[:, cc, :NQ], in_=ps[b][:, :NQ], func=ACT.Identity,
                                                           bias=pcv[:, 31, cc:cc + 1]), [psb[b], B_pcv], [B_ca])
                steps.append(([(0, s_dg[cc], 1, 31 * 128, dgB)], s_dw))

            def s_conv(_, __):
                uext, B_u = sc["uext"]
                cacc, B_ca = sc["cacc"]
                zT, B_z = sc["zT"]
                cst, B_st = sc["st"]
                ysq, B_ysq = sc["ub"]
                kb.op(act, lambda e: e.activation(out=zT[:, :, :NQ], in_=cacc[:, :, :NQ], func=ACT.Copy), [B_ca], [B_z])
                kb.op(act, lambda e: e.activation(out=ysq[:, :, :NQ], in_=cacc[:, :, :NQ], func=ACT.Square), [B_ca], [B_ysq])
                b1 = next_ps()
                mm_acc(b1, ps[b1][:, :NQ], [(ones[:, :], zT[:, k, :NQ]) for k in range(8)], [B_z, B_c])
                b2 = next_ps()
                mm_acc(b2, ps[b2][:, :NQ], [(ones[:, :], ysq[:, k, :NQ]) for k in range(8)], [B_ysq, B_c])
                mean, msq, var = cst[:, 0, :NQ], cst[:, 1, :NQ], cst[:, 2, :NQ]
                kb.op(dve, lambda e: e.tensor_scalar(out=mean, in0=ps[b1][:, :NQ], scalar1=1.0 / DCONV, scalar2=None,
                                                     op0=ALU.mult), [psb[b1]], [B_st])
                kb.op(dve, lambda e: e.tensor_tensor(out=msq, in0=mean, in1=mean, op=ALU.mult), [B_st], [B_st])
                kb.op(dve, lambda e: e.scalar_tensor_tensor(out=var, in0=ps[b2][:, :NQ], scalar=1.0 / DCONV, in1=msq,
                                                            op0=ALU.mult, op1=ALU.subtract), [psb[b2], B_st], [B_st])
                kb.op(act, lambda e: e.activation(out=var, in_=var, func=ACT.Sqrt, bias=EPS), [B_st], [B_st])
                kb.op(dve, lambda e: e.reciprocal(out=var, in_=var), [B_st], [B_st])
                for cc in range(8):
                    kb.op(dve, lambda e, cc=cc: e.tensor_tensor(out=cacc[:, cc, :NQ], in0=cacc[:, cc, :NQ], in1=mean,
                                                                op=ALU.subtract), [B_st, B_ca], [B_ca])
                    kb.op(dve, lambda e, cc=cc: e.tensor_tensor(out=cacc[:, cc, :NQ], in0=cacc[:, cc, :NQ], in1=var,
                                                                op=ALU.mult), [B_st, B_ca], [B_ca])
                    kb.op(act, lambda e, cc=cc: e.activation(out=zT[:, cc, :NQ], in_=cacc[:, cc, :NQ], func=ACT.Silu,
                                                             scale=pcv[:, 32, cc:cc + 1], bias=pcv[:, 33, cc:cc + 1]),
                          [B_ca, B_pcv, B_z], [B_z])
            steps.append(([], s_conv))
            branch_out(0, "g0", lambda cb: s_wco[:, cb * 512:(cb + 1) * 512], 8, B_wco, "zT")
            steps.append(([], close_scope))

            def s_mem_open(_, __):
                open_scope()
                sc["mqT"] = kb.sb("mqT", [128, 8, GT], BF16)
                sc["moT"] = kb.sb("moT", [128, 8, GT], BF16)
                sc["mP"] = kb.sb("mP", [128, 2, GT], BF16)
                sc["mr"] = kb.sb("mr", [128, GT], F32)
            steps.append(([], s_mem_open))
            for cb in range(2):
                def s_mq(sl, Bsl, cb=cb):
                    mqT, B_mq = sc["mqT"]
                    w = sl[:, 0:KC * 512].rearrange("p (k c) -> p k c", c=512)
                    for j in range(4):
                        b = next_ps()
                        mm_acc(b, ps[b][:, :NQ], [(w[:, k, j * 128:(j + 1) * 128], hT[:, k, :NQ]) for k in range(KC)],
                               [Bsl, B_hT])
                        evac(j, mqT[:, cb * 4 + j, :NQ], ps[b][:, :NQ], [psb[b]], [B_mq], scale=1.0 / 16.0)
                wstep(s_win[:, C_MQ + cb * 512:C_MQ + (cb + 1) * 512], KC, B_win["mq"], s_mq)

            def s_mem(_, __):
                mqT, B_mq = sc["mqT"]
                moT, B_mo = sc["moT"]
                mP, B_mP = sc["mP"]
                mr, B_mr = sc["mr"]
                for hd in range(4):
                    for mt in range(2):
                        b = next_ps()
                        mm_acc(b, ps[b][:, :NQ], [(mkT[:, hd * 2 + hf, mt * 128:(mt + 1) * 128], mqT[:, hd * 2 + hf, :NQ])
                                                  for hf in range(2)], [B_mk, B_mq])
                        kb.op(act, lambda e, b=b, mt=mt: e.activation(out=mP[:, mt, :NQ], in_=ps[b][:, :NQ], func=ACT.Exp),
                              [psb[b]], [B_mP])
                    bl = next_ps()
                    mm_acc(bl, ps[bl][:, :NQ], [(ones[:, :], mP[:, mt, :NQ]) for mt in range(2)], [B_mP, B_c])
                    kb.op(dve, lambda e, bl=bl: e.reciprocal(out=mr[:, :NQ], in_=ps[bl][:, :NQ]), [psb[bl]], [B_mr])
                    for dh in range(2):
                        b = next_ps()
                        mm_acc(b, ps[b][:, :NQ], [(mv[:, mt, hd * 256 + dh * 128:hd * 256 + (dh + 1) * 128], mP[:, mt, :NQ])
                                                  for mt in range(2)], [B_mv, B_mP])
                        kb.op(dve, lambda e, b=b, hd=hd, dh=dh: e.tensor_tensor(out=moT[:, hd * 2 + dh, :NQ], in0=ps[b][:, :NQ],
                                                                               in1=mr[:, :NQ], op=ALU.mult),
                              [psb[b], B_mr], [B_mo])
            steps.append(([], s_mem))
            branch_out(2, "g2", lambda cb: s_wmo[:, cb * 512:(cb + 1) * 512], 8, B_wmo, "moT")
            steps.append(([], close_scope))

            def s_att_open(_, __):
                open_scope()
                sc["qT"] = kb.sb("qT", [128, 16, GT], BF16)
                sc["onT"] = kb.sb("onT", [128, 16, GT], BF16)
                sc["kt"] = [kb.sb("akt%d" % i, [128, 2048], BF16) for i in range(2)]
                sc["vv"] = [kb.sb("avv%d" % i, [128, 16, 128], BF16) for i in range(2)]
                sc["P"] = [kb.sb("aP%d" % i, [128, GT], BF16) for i in range(4)]
                sc["fin"] = kb.sb("afin", [128, 4, GT], F32)
                sc["osq"] = kb.sb("aosq", [128, GT], BF16)
            steps.append(([], s_att_open))
            for cb in range(4):
                def s_q(sl, Bsl, cb=cb):
                    qT, B_q = sc["qT"]
                    w = sl[:, 0:KC * 512].rearrange("p (k c) -> p k c", c=512)
                    for j in range(4):
                        b = next_ps()
                        mm_acc(b, ps[b][:, :NQ], [(w[:, k, j * 128:(j + 1) * 128], hT[:, k, :NQ]) for k in range(KC)],
                               [Bsl, B_hT])
                        evac(j, qT[:, cb * 4 + j, :NQ], ps[b][:, :NQ], [psb[b]], [B_q], scale=0.125)
                wstep(s_win[:, C_Q + cb * 512:C_Q + (cb + 1) * 512], KC, B_win["q"], s_q)

            segs = []
            if not is_s:
                for s_ in range(gi):
                    segs.append((s_ * 2048, [(128, "full", None)] * 16))
                dblk = [(128, "diag", kb_) for kb_ in range(4)] + [(128, "bias", 1 + (bk - 4) // 4) for bk in range(4, 16)]
                segs.append((gi * 2048, dblk))
            else:
                nb_tot = PAST // 128
                k0 = 0
                while nb_tot > 0:
                    n = min(16, nb_tot)
                    segs.append((k0, [(128, "full", None)] * n))
                    k0 += n * 128
                    nb_tot -= n
                if len(segs[-1][1]) < 16:
                    segs[-1][1].append((NSAMP, "full", None))
                else:
                    segs.append((k0, [(NSAMP, "full", None)]))

            def s_att(_, __):
                qT, B_q = sc["qT"]
                onT, B_on = sc["onT"]
                fin, B_fin = sc["fin"]
                osq, B_osq = sc["osq"]
                ktv = kt_scr
                vvv = v_scr
                li = [0]
                pi = [0]
                nblk_all = sum(len(s_[1]) for s_ in segs)
                bO = [4, 5, 6, 7]
                r1, r2, t1, o = fin[:, 0, :NQ], fin[:, 1, :NQ], fin[:, 2, :NQ], fin[:, 3, :NQ]

                items = []
                for h in range(16):
                    bi = 0
                    for (k0, blks) in segs:
                        for bl, (kn, kind, arg) in enumerate(blks):
                            items.append(dict(h=h, k0=k0, blks=blks, bl=bl, kn=kn, kind=kind, arg=arg,
                                              first=(bi == 0), last=(bi == nblk_all - 1), bi=bi))
                            bi += 1

                def emit_S(it):
                    h = it["h"]
                    if it["bl"] == 0:
                        ktt, B_ktt = sc["kt"][li[0] % 2]
                        vvt, B_vvt = sc["vv"][li[0] % 2]
                        li[0] += 1
                        blks, k0 = it["blks"], it["k0"]
                        nk = sum(b_[0] for b_ in blks)
                        nfull = sum(1 for b_ in blks if b_[0] == 128)
                        kb.dma(sp, ktt[:, :nk], ktv[h, :, k0:k0 + nk], [B_kt], [B_ktt], B_ktt)
                        if nfull:
                            kb.dma(sp, vvt[:, :nfull, :], vvv[h, :, k0 // 128:k0 // 128 + nfull, :], [B_v], [B_vvt], B_vvt)
                        if nfull < len(blks):
                            kn_ = blks[-1][0]
                            kb.dma(sp, vvt[:kn_, nfull, :], vvv[h, :kn_, k0 // 128 + nfull, :], [B_v], [B_vvt], B_vvt)
                        cur["kt"] = (ktt, B_ktt)
                        cur["vv"] = (vvt, B_vvt)
                    ktt, B_ktt = cur["kt"]
                    it["vv"] = cur["vv"]
                    kn, kind, arg, bl = it["kn"], it["kind"], it["arg"], it["bl"]
                    c0 = 128 * arg if kind == "diag" else 0
                    it["c0"] = c0
                    it["P"] = []
                    for mp in range(2):
                        bS = next_ps("s")
                        p0 = mp * 64
                        kb.op(pe, lambda e, bS=bS, p0=p0: e.matmul(
                            ps[bS][:kn, c0:NQ], ktt[p0:p0 + 64, bl * 128:bl * 128 + kn], qT[p0:p0 + 64, h, c0:NQ],
                            start=True, stop=True), [B_ktt, B_q], [psb[bS]])
                        Pt, B_P = sc["P"][pi[0] % 4]
                        pi[0] += 1
                        it["P"].append((Pt, B_P))
                        if kind == "bias":
                            kb.op(act, lambda e, bS=bS, Pt=Pt: e.activation(
                                out=Pt[:kn, c0:NQ], in_=ps[bS][:kn, c0:NQ], func=ACT.Exp,
                                bias=cols[:kn, 8 + arg:9 + arg]), [psb[bS], B_cols], [B_P])
                        else:
                            kb.op(act, lambda e, bS=bS, Pt=Pt: e.activation(
                                out=Pt[:kn, c0:NQ], in_=ps[bS][:kn, c0:NQ], func=ACT.Exp), [psb[bS]], [B_P])
                        if kind == "diag":
                            kb.op(pool, lambda e, Pt=Pt: e.memset(Pt[64:128, c0:c0 + 64], 0.0), [], [B_P])

                def emit_PV(it):
                    kn, bl, c0, first, last = it["kn"], it["bl"], it["c0"], it["first"], it["last"]
                    vvt, B_vvt = it["vv"]
                    for mp in range(2):
                        Pt, B_P = it["P"][mp]
                        kb.op(pe, lambda e, mp=mp, Pt=Pt: e.matmul(
                            ps[bO[2 * mp]][:, c0:NQ], vvt[:kn, bl, :], Pt[:kn, c0:NQ], start=first, stop=last),
                            [B_vvt, B_P], [psb[bO[2 * mp]]])
                        kb.op(pe, lambda e, mp=mp, Pt=Pt: e.matmul(
                            ps[bO[2 * mp + 1]][:, c0:NQ], ones[:kn, :], Pt[:kn, c0:NQ], start=first, stop=last),
                            [B_c, B_P], [psb[bO[2 * mp + 1]]])

                def fin1():
                    kb.op(dve, lambda e: e.reciprocal(out=r1, in_=ps[5][:, :NQ]), [psb[5]], [B_fin])
                    kb.op(dve, lambda e: e.reciprocal(out=r2, in_=ps[7][:, :NQ]), [psb[7]], [B_fin])
                    kb.op(dve, lambda e: e.tensor_tensor(out=t1, in0=ps[4][:, :NQ], in1=r1, op=ALU.mult), [psb[4], B_fin], [B_fin])
                    kb.op(dve, lambda e: e.tensor_tensor(out=r2, in0=ps[6][:, :NQ], in1=r2, op=ALU.mult), [psb[6], B_fin], [B_fin])
                    kb.op(dve, lambda e: e.scalar_tensor_tensor(out=o, in0=r2, scalar=cols[:, 1:2], in1=t1, op0=ALU.mult,
                                                                op1=ALU.add), [B_fin, B_cols], [B_fin])
                    kb.op(act, lambda e: e.activation(out=osq[:, :NQ], in_=o, func=ACT.Square), [B_fin], [B_osq])

                def fin2(h):
                    bs_ = next_ps("s")
                    kb.op(pe, lambda e, bs_=bs_: e.matmul(ps[bs_][:, :NQ], ones[:, :], osq[:, :NQ], start=True, stop=True),
                          [B_osq, B_c], [psb[bs_]])
                    kb.op(act, lambda e, bs_=bs_: e.activation(out=r1, in_=ps[bs_][:, :NQ], func=ACT.Sqrt, scale=1.0 / 128,
                                                               bias=1e-5), [psb[bs_]], [B_fin])
                    kb.op(dve, lambda e: e.reciprocal(out=r1, in_=r1), [B_fin], [B_fin])
                    kb.op(dve, lambda e, h=h: e.scalar_tensor_tensor(out=onT[:, h, :NQ], in0=o, scalar=cols[:, 2:3], in1=r1,
                                                                     op0=ALU.mult, op1=ALU.mult), [B_fin, B_cols], [B_on])

                cur = {}
                n_it = len(items)
                pend_fin2 = None
                emit_S(items[0])
                for i in range(n_it):
                    it = items[i]
                    if i + 1 < n_it:
                        emit_S(items[i + 1])
                    emit_PV(it)
                    if pend_fin2 is not None and (it["bi"] >= min(1, nblk_all - 1)):
                        fin2(pend_fin2)
                        pend_fin2 = None
                    if it["last"]:
                        fin1()
                        pend_fin2 = it["h"]
                if pend_fin2 is not None:
                    fin2(pend_fin2)
            steps.append(([], s_att))
            branch_out(1, "g1", lambda cb: s_wao[:, cb * 512:(cb + 1) * 512], KC, B_wao, "onT")
            steps.append(([], close_scope))

            sc["merged"] = (merged, B_mg)
            for cb in range(4):
                def s_wout(sl, Bsl, cb=cb):
                    w = sl[:, 0:KC * 512].rearrange("p (k c) -> p k c", c=512)
                    for t, nt in tl:
                        b = next_ps()
                        mm_acc(b, ps[b][:nt, :], [(merged[:, k, t * 128:t * 128 + nt], w[:, k, :]) for k in range(KC)],
                               [Bsl, B_mg])
                        kb.op(dve, lambda e, b=b, t=t, nt=nt: e.tensor_tensor(
                            out=xg[:nt, t, cb * 512:(cb + 1) * 512], in0=xg[:nt, t, cb * 512:(cb + 1) * 512],
                            in1=ps[b][:nt, :], op=ALU.add), [psb[b], B_xg], [B_xg])
                wstep(s_wo[:, cb * 512:(cb + 1) * 512], KC, B_wo, s_wout)

            def s_moe_open(_, __):
                open_scope()
                sc["acc"] = kb.sb("macc", [128, 4, D], F32)
                sc["aT"] = [kb.sb("maT%d" % i, [128, 2, GT], BF16) for i in range(2)]
                sc["sg"] = [kb.sb("msg%d" % i, [128, GT], F32) for i in range(2)]
                sc["comb"] = kb.sb("mcomb", [128, 4, 32], F32)
                sc["mtmp"] = [kb.sb("mtmp%d" % i, [128, 512], F32) for i in range(3)]
                sc["rt"] = kb.sb("mrt", [128, 96], F32)
                sc["gfin"] = kb.sb("gfin", [128, D], F32)
                acc, B_acc = sc["acc"]
                sc["accp"] = Buf("accp")
                comb, B_comb = sc["comb"]
                rt, B_rt = sc["rt"]
                gfin, B_gfin = sc["gfin"]
                kb.dma(sp, gfin[:, :], norm_final.partition_broadcast(128), [], [B_gfin], B_gfin)
                kb.op(pool, lambda e: e.memset(acc[:, :, :], 0.0), [], [B_acc, sc["accp"]])
                for t, nt in tl:
                    rmsnorm_T(xg[:nt, t, :], B_xg, nt, G_FFN, hT, B_hT, t * 128, xn, B_xn, sq, B_sq, 16)
                for t, nt in tl:
                    b = next_ps()
                    mm_acc(b, ps[b][:nt, :36], [(hT[:, k, t * 128:t * 128 + nt], wr_sb[:, k, :]) for k in range(KC)],
                           [B_hT, B_wrs])
                    lg = rt[:nt, 0:36]
                    gmax, ngmax, gsum, emax, nemax, m2, den = (rt[:nt, 36 + i:37 + i] for i in range(7))
                    gmask = rt[:nt, 44:48]
                    pen = rt[:nt, 48:52]
                    ge = rt[:nt, 52:56]
                    ee = rt[:nt, 56:88]
                    kb.op(dve, lambda e, b=b, nt=nt, lg=lg: e.tensor_tensor(out=lg, in0=ps[b][:nt, :36], in1=rbias[:nt, :],
                                                                         op=ALU.add), [psb[b], B_rb], [B_rt])
                    kb.op(dve, lambda e, lg=lg, gmax=gmax: e.reduce_max(out=gmax, in_=lg[:, 0:4], axis=AX.X), [B_rt], [B_rt])
                    kb.op(dve, lambda e, gmax=gmax, ngmax=ngmax: e.tensor_scalar(out=ngmax, in0=gmax, scalar1=-1.0, scalar2=None,
                                                                                 op0=ALU.mult), [B_rt], [B_rt])
                    kb.op(dve, lambda e, lg=lg, gmax=gmax, gmask=gmask: e.tensor_scalar(out=gmask, in0=lg[:, 0:4], scalar1=gmax,
                                                                                      scalar2=None, op0=ALU.is_ge),
                          [B_rt], [B_rt])
                    kb.op(dve, lambda e, gsum=gsum: e.memset(gsum, 0.0), [], [B_rt])
                    kb.op(act, lambda e, lg=lg, ge=ge, ngmax=ngmax, gsum=gsum: e.activation(
                        out=ge, in_=lg[:, 0:4], func=ACT.Exp, bias=ngmax, accum_out=gsum), [B_rt], [B_rt])
                    kb.op(dve, lambda e, gmask=gmask, pen=pen: e.tensor_scalar(out=pen, in0=gmask, scalar1=-1.0, scalar2=1e30,
                                                                              op0=ALU.add, op1=ALU.mult), [B_rt], [B_rt])
                    for g in range(4):
                        kb.op(dve, lambda e, g=g, lg=lg, pen=pen, ee=ee: e.tensor_scalar(
                            out=ee[:, g * 8:(g + 1) * 8], in0=lg[:, 4 + g * 8:12 + g * 8], scalar1=pen[:, g:g + 1],
                            scalar2=None, op0=ALU.add), [B_rt], [B_rt])
                    kb.op(dve, lambda e, ee=ee, emax=emax: e.reduce_max(out=emax, in_=ee, axis=AX.X), [B_rt], [B_rt])
                    kb.op(dve, lambda e, emax=emax, nemax=nemax: e.tensor_scalar(out=nemax, in0=emax, scalar1=-1.0, scalar2=None,
                                                                                 op0=ALU.mult), [B_rt], [B_rt])
                    kb.op(act, lambda e, ee=ee, nemax=nemax: e.activation(out=ee, in_=ee, func=ACT.Exp, bias=nemax),
                          [B_rt], [B_rt])
                    e2 = rt[:nt, 0:32]
                    kb.op(dve, lambda e, ee=ee, e2=e2: e.scalar_tensor_tensor(out=e2, in0=ee, scalar=1.0, in1=ee, op0=ALU.is_lt,
                                                                             op1=ALU.mult), [B_rt], [B_rt])
                    kb.op(dve, lambda e, e2=e2, m2=m2: e.reduce_max(out=m2, in_=e2, axis=AX.X), [B_rt], [B_rt])
                    kb.op(dve, lambda e, m2=m2, den=den, gsum=gsum: e.scalar_tensor_tensor(
                        out=den, in0=m2, scalar=1.0, in1=gsum, op0=ALU.add, op1=ALU.mult), [B_rt], [B_rt])
                    kb.op(dve, lambda e, den=den: e.reciprocal(out=den, in_=den), [B_rt], [B_rt])
                    kb.op(dve, lambda e, ee=ee, e2=e2, m2=m2: e.scalar_tensor_tensor(out=e2, in0=ee, scalar=m2, in1=ee,
                                                                                   op0=ALU.is_ge, op1=ALU.mult), [B_rt], [B_rt])
                    kb.op(dve, lambda e, e2=e2, den=den, t=t, nt=nt: e.tensor_scalar(out=comb[:nt, t, :], in0=e2, scalar1=den,
                                                                                    scalar2=None, op0=ALU.mult),
                          [B_rt], [B_comb])
            steps.append(([], s_moe_open))

            mtc = [0]
            for ex in range(NEXP):
                def s_gu(sl, Bsl, ex=ex):
                    aT, B_aT = sc["aT"][ex % 2]
                    wg = sl[:, 0:KC * 256].rearrange("p (k c) -> p k c", c=256)
                    wu = sl[:, KC * 256:2 * KC * 256].rearrange("p (k c) -> p k c", c=256)
                    for hc in range(2):
                        sg, B_sg = sc["sg"][hc]
                        bg = next_ps()
                        mm_acc(bg, ps[bg][:, :NQ], [(wg[:, k, hc * 128:(hc + 1) * 128], hT[:, k, :NQ]) for k in range(KC)],
                               [Bsl, B_hT])
                        bu = next_ps()
                        mm_acc(bu, ps[bu][:, :NQ], [(wu[:, k, hc * 128:(hc + 1) * 128], hT[:, k, :NQ]) for k in range(KC)],
                               [Bsl, B_hT])
                        kb.op(act, lambda e, bg=bg, sg=sg: e.activation(out=sg[:, :NQ], in_=ps[bg][:, :NQ], func=ACT.Silu),
                              [psb[bg]], [B_sg])
                        kb.op(dve, lambda e, bu=bu, sg=sg, hc=hc, aT=aT: e.tensor_tensor(out=aT[:, hc, :NQ], in0=ps[bu][:, :NQ],
                                                                                        in1=sg[:, :NQ], op=ALU.mult),
                              [psb[bu], B_sg], [B_aT])
                steps.append(([(0, s_eg[ex], KC, 256, B_exp[ex // 4]), (KC * 256, s_eu[ex], KC, 256, B_exp[ex // 4])], s_gu))

                def s_dn(sl, Bsl, ex=ex):
                    aT, B_aT = sc["aT"][ex % 2]
                    acc, B_acc = sc["acc"]
                    B_accp = sc["accp"]
                    comb, B_comb = sc["comb"]
                    wd = sl[:, 0:2 * D].rearrange("p (k c) -> p k c", c=D)
                    for t, nt in tl:
                        for cb in range(4):
                            b = next_ps()
                            mm_acc(b, ps[b][:nt, :], [(aT[:, hc, t * 128:t * 128 + nt], wd[:, hc, cb * 512:(cb + 1) * 512])
                                                      for hc in range(2)], [Bsl, B_aT])
                            if MOE_SPLIT and (t * 4 + cb) % 3 == 2:
                                mt_, B_mt = sc["mtmp"][mtc[0] % 3]
                                mtc[0] += 1
                                kb.op(act, lambda e, b=b, t=t, nt=nt, mt_=mt_: e.activation(
                                    out=mt_[:nt, :], in_=ps[b][:nt, :], func=ACT.Copy, scale=comb[:nt, t, ex:ex + 1]),
                                    [psb[b], B_comb], [B_mt])
                                kb.op(pool, lambda e, t=t, nt=nt, cb=cb, mt_=mt_: e.tensor_tensor(
                                    out=acc[:nt, t, cb * 512:(cb + 1) * 512], in0=acc[:nt, t, cb * 512:(cb + 1) * 512],
                                    in1=mt_[:nt, :], op=ALU.add), [B_mt, B_accp], [B_accp])
                            else:
                                kb.op(dve, lambda e, b=b, t=t, nt=nt, cb=cb: e.scalar_tensor_tensor(
                                    out=acc[:nt, t, cb * 512:(cb + 1) * 512], in0=ps[b][:nt, :], scalar=comb[:nt, t, ex:ex + 1],
                                    in1=acc[:nt, t, cb * 512:(cb + 1) * 512], op0=ALU.mult, op1=ALU.add),
                                    [psb[b], B_comb, B_acc], [B_acc])
                steps.append(([(0, s_ed[ex], 2, D, B_exp[ex // 4])], s_dn))

            def s_final(_, __):
                acc, B_acc = sc["acc"]
                B_accp = sc["accp"]
                gfin, B_gfin = sc["gfin"]
                for t, nt in tl:
                    c_ss = cols[:nt, 20:21]
                    c_r = cols[:nt, 21:22]
                    kb.op(dve, lambda e, t=t, nt=nt: e.tensor_tensor(out=acc[:nt, t, :], in0=acc[:nt, t, :], in1=xg[:nt, t, :],
                                                                    op=ALU.add), [B_xg, B_acc, B_accp], [B_acc, B_accp])
                    kb.op(dve, lambda e, c_ss=c_ss: e.memset(c_ss, 0.0), [], [B_cols])
                    kb.op(act, lambda e, t=t, nt=nt, c_ss=c_ss: e.activation(out=sq[:nt, :], in_=acc[:nt, t, :], func=ACT.Square,
                                                                            accum_out=c_ss), [B_acc, B_cols], [B_sq, B_cols])
                    kb.op(act, lambda e, c_ss=c_ss, c_r=c_r: e.activation(out=c_r, in_=c_ss, func=ACT.Sqrt, scale=1.0 / D,
                                                                          bias=EPS), [B_cols], [B_cols])
                    kb.op(dve, lambda e, c_r=c_r: e.reciprocal(out=c_r, in_=c_r), [B_cols], [B_cols])
                    kb.op(dve, lambda e, t=t, nt=nt, c_r=c_r: e.scalar_tensor_tensor(
                        out=acc[:nt, t, :], in0=acc[:nt, t, :], scalar=c_r, in1=gfin[:nt, :], op0=ALU.mult, op1=ALU.mult),
                        [B_acc, B_cols, B_gfin], [B_acc])
                    kb.dma(pool, y_o[row0 + t * 128:row0 + t * 128 + nt, :], acc[:nt, t, :], [B_acc], [], B_acc)
            steps.append(([], s_final))
            steps.append(([], close_scope))

        for gi in range(NGRP_):
            blocks = None
            group(gi, GT, xfull[gi * 4 * GT:(gi * 4 + 1) * GT, :], gi * GT, "x", mkT_p, B_mkp, mv_p, B_mvp,
                  s_ktp, B_ktp, s_vp, B_vp, blocks, convp_o if gi == NGRP_ - 1 else None)
        def s_mem_sample(_, __):
            cmb = xn[:, :].rearrange("p (t c) -> p t c", c=1024)
            for t in range(2):
                kb.dma(pool, cmb[:, t, :], cmk[t * 128:(t + 1) * 128, :], [], [B_xn], B_xn)
                kb.dma(pool, mv_p[:, t, :], cmv[t * 128:(t + 1) * 128, :], [], [B_mvp], B_mvp)
            for t in range(2):
                b = next_ps()
                pv = ps[b][:].bitcast(BF16).rearrange("p (j c) -> p j c", c=128)
                for cc in range(8):
                    kb.op(pe, lambda e, cc=cc, pv=pv, t=t: e.transpose(pv[:, cc, :], cmb[:, t, cc * 128:(cc + 1) * 128],
                                                                      ident[:, :]), [B_xn, B_c], [psb[b]])
                evac(t, mkT_p[:, :, t * 128:(t + 1) * 128], pv[:, :, :], [psb[b]], [B_mkp])
        steps.append(([], s_mem_sample))
        group(NGRP_, NSAMP, xsamp, NGRP_ * GT, "cache", mkT_p, B_mkp, mv_p, B_mvp, s_kts, B_kts, s_vs, B_vs, None, convs_o)
        run_steps()

        kb.barrier()
    return nc


_NC_CACHE = {}


def kernel(**inp):
    inp = {k: np.asarray(v) for k, v in inp.items()}
    if "nc" not in _NC_CACHE:
        _NC_CACHE["nc"] = build_program()
    nc = _NC_CACHE["nc"]
    xp = inp["x_prompt"]
    in_maps = []
    wnames = ["norm_mix", "w_in", "w_dw", "b_dw", "conv_ln_g", "conv_ln_b", "w_conv_out", "lambda_q1", "lambda_k1",
              "lambda_q2", "lambda_k2", "subln_g", "w_attn_out", "norm_mem", "w_mem_k", "w_mem_v", "w_mem_out",
              "w_out", "norm_ffn", "w_router_grp", "b_router_grp", "w_router_exp", "b_router_exp", "w_exp_gate",
              "w_exp_up", "w_exp_down"]
    wts = {n: np.ascontiguousarray(inp[n][0]) for n in wnames}
    wts["norm_final"] = np.ascontiguousarray(inp["norm_final"])
    for c in range(8):
        b, j = c // 4, c % 4
        xb = xp[b].reshape(16, GT, D)
        order = []
        for i in range(4):
            order.append(4 * i + j)
            order += [4 * i + m for m in range(4) if m != j]
        xfull = np.ascontiguousarray(xb[order].reshape(SEQ, D))
        xhalo = np.zeros((NGRP, 32, D), np.float32)
        for i in range(4):
            g = 4 * i + j
            if g > 0:
                xhalo[i] = xp[b, g * GT - 32:g * GT]
        gp = np.array([j] + [m for m in range(4) if m != j], np.float32)
        m = dict(wts)
        m.update({
            "xfull": xfull, "xhalo": xhalo, "gpos": np.ascontiguousarray(np.broadcast_to(gp, (128, 4))),
            "xsamp": np.ascontiguousarray(inp["x_sample"][c]),
            "cconv": np.ascontiguousarray(inp["cache_conv"][0, c]),
            "ck": np.ascontiguousarray(inp["cache_diff_k"][0, c].reshape(PAST, D)),
            "cv": np.ascontiguousarray(inp["cache_diff_v"][0, c].reshape(PAST, D)),
            "cmk": np.ascontiguousarray(inp["cache_mem_k"][0, c].reshape(256, 1024)),
            "cmv": np.ascontiguousarray(inp["cache_mem_v"][0, c].reshape(256, 1024)),
            "memx": np.ascontiguousarray(inp["mem_prompt"][b]),
        })
        in_maps.append(m)
    res = run_bass_kernel_spmd(nc, in_maps, core_ids=list(range(8)))
    R = res.results
    y_p = np.zeros((2, SEQ, D), np.float32)
    k_p = np.zeros((1, 2, SEQ, 16, 128), np.float32)
    v_p = np.zeros((1, 2, SEQ, 16, 128), np.float32)
    y_s = np.zeros((8, NSAMP, D), np.float32)
    k_s = np.zeros((1, 8, NSAMP, 16, 128), np.float32)
    v_s = np.zeros((1, 8, NSAMP, 16, 128), np.float32)
    conv_p = np.zeros((1, 2, 30, DCONV), np.float32)
    conv_s = np.zeros((1, 8, 30, DCONV), np.float32)
    mk_p = np.zeros((1, 2, 256, 4, 256), np.float32)
    mv_p = np.zeros((1, 2, 256, 4, 256), np.float32)
    for c in range(8):
        b, j = c // 4, c % 4
        r = R[c]
        for i in range(4):
            g = 4 * i + j
            y_p[b, g * GT:(g + 1) * GT] = r["y"][i * GT:(i + 1) * GT]
            k_p[0, b, g * GT:(g + 1) * GT] = r["kout"][i * GT:(i + 1) * GT].reshape(GT, 16, 128)
            v_p[0, b, g * GT:(g + 1) * GT] = r["vout"][i * GT:(i + 1) * GT].reshape(GT, 16, 128)
        y_s[c] = r["y"][NGRP * GT:]
        k_s[0, c] = r["kout"][NGRP * GT:].reshape(NSAMP, 16, 128)
        v_s[0, c] = r["vout"][NGRP * GT:].reshape(NSAMP, 16, 128)
        conv_s[0, c] = r["convs"]
        if j == 3:
            conv_p[0, b] = r["convp"]
        if j == 0:
            mk_p[0, b] = r["memk"].reshape(256, 4, 256)
            mv_p[0, b] = r["memv"].reshape(256, 4, 256)
    return (y_p, y_s, conv_p, k_p, v_p, mk_p, mv_p, conv_s, k_s, v_s)
```

```python
import math
from contextlib import ExitStack

import numpy as np
import concourse.bass as bass
import concourse.mybir as mybir
from concourse.bass_utils import run_bass_kernel_spmd

F32 = mybir.dt.float32
BF16 = mybir.dt.bfloat16
ACT = mybir.ActivationFunctionType
ALU = mybir.AluOpType
AX = mybir.AxisListType

D = 2048
KC = 16
SEQ = 8192
NGRP = 4
DBG_STOP = None
GT = 512
DCONV = 1024
NSAMP = 64
PAST = 4096
NIN = 15360
NEXP = 32
DEXP = 256
EPS = 1e-6
NEG = -30000.0
C_AGT, C_Q, C_K, C_V, C_MQ, C_G = 0, 2048, 4096, 6144, 8192, 9216
STAGE = 2
MOE_SPLIT = True


class Buf:
    def __init__(self, name):
        self.name = name
        self.w = {}
        self.r = {}
        self.dsem = None
        self.dcount = 0


class Eng:
    def __init__(self, kb, e, name, is_pe=False):
        self.e = e
        self.name = name
        self.sem = kb.newsem("e_" + name)
        self.n = 0
        self.seen = {}
        self.is_pe = is_pe

    def wait(self, sem, val):
        if val <= 0:
            return
        if self.is_pe and sem is self.sem:
            return
        if self.seen.get(sem, 0) >= val:
            return
        self.e.wait_ge(sem, val)
        self.seen[sem] = val


class KB:
    def __init__(self, nc, es):
        self.nc = nc
        self.es = es
        self.sem_es = es
        self.nsem = 0
        self.pe = Eng(self, nc.tensor, "pe", True)
        self.act = Eng(self, nc.scalar, "act")
        self.dve = Eng(self, nc.vector, "dve")
        self.pool = Eng(self, nc.gpsimd, "pool")
        self.sp = Eng(self, nc.sync, "sp")
        self.engs = [self.pe, self.act, self.dve, self.pool, self.sp]
        self.dma_bufs = []

    def newsem(self, name):
        self.nsem += 1
        return self.sem_es.enter_context(self.nc.semaphore(name + "_%d" % self.nsem))

    def sb(self, name, shape, dt):
        self.nsb = getattr(self, "nsb", 0) + 1
        name = "%s_%d" % (name, self.nsb)
        t = self.es.enter_context(self.nc.sbuf_tensor(name, shape, dt))
        return t, Buf(name)

    def _deps(self, eng, reads, writes):
        for b in reads:
            for s, v in b.w.items():
                eng.wait(s, v)
        for b in writes:
            for s, v in b.w.items():
                eng.wait(s, v)
            for s, v in b.r.items():
                eng.wait(s, v)

    def _mark(self, sem, val, reads, writes):
        for b in reads:
            if b.r.get(sem, 0) < val:
                b.r[sem] = val
        for b in writes:
            if b.w.get(sem, 0) < val:
                b.w[sem] = val

    def op(self, eng, fn, reads=(), writes=()):
        self._deps(eng, reads, writes)
        ins = fn(eng.e)
        eng.n += 1
        ins.then_inc(eng.sem, 1)
        self._mark(eng.sem, eng.n, reads, writes)

    def dma(self, q, out, in_, reads, writes, owner, throttle=None):
        self._deps(q, reads, writes)
        if throttle is not None:
            hist, depth = throttle
            if len(hist) >= depth:
                ps_, pv_ = hist[len(hist) - depth]
                q.wait(ps_, pv_)
        if owner.dsem is None:
            owner.dsem = self.newsem("d_" + owner.name)
            self.dma_bufs.append(owner)
        ins = q.e.dma_start(out=out, in_=in_)
        owner.dcount += 16
        ins.then_inc(owner.dsem, 16)
        if throttle is not None:
            throttle[0].append((owner.dsem, owner.dcount))
        self._mark(owner.dsem, owner.dcount, reads, writes)

    def sync_bufs(self, bufs):
        for e in self.engs:
            for b in bufs:
                for sm, v in list(b.w.items()) + list(b.r.items()):
                    e.wait(sm, v)

    def barrier(self):
        for e in self.engs:
            for f in self.engs:
                if f is not e:
                    e.wait(f.sem, f.n)
            for b in self.dma_bufs:
                e.wait(b.dsem, b.dcount)


def build_program():
    nc = bass.Bass("TRN2", target_bir_lowering=False)

    def din(name, shape):
        return nc.dram_tensor(name, list(shape), F32, kind="ExternalInput").ap()

    def dout(name, shape):
        return nc.dram_tensor(name, list(shape), F32, kind="ExternalOutput").ap()

    def dscr(name, shape, dt=BF16):
        return nc.dram_tensor(name, list(shape), dt, kind="Internal").ap()

    xfull = din("xfull", [SEQ, D])
    xhalo = din("xhalo", [NGRP, 32, D])
    gpos = din("gpos", [128, 4])
    xsamp = din("xsamp", [NSAMP, D])
    cconv = din("cconv", [30, DCONV])
    ck = din("ck", [PAST, D])
    cv = din("cv", [PAST, D])
    cmk = din("cmk", [256, 1024])
    cmv = din("cmv", [256, 1024])
    memx = din("memx", [256, D])
    norm_mix = din("norm_mix", [D])
    w_in = din("w_in", [D, NIN])
    w_dw = din("w_dw", [31, DCONV])
    b_dw = din("b_dw", [DCONV])
    ln_g = din("conv_ln_g", [DCONV])
    ln_b = din("conv_ln_b", [DCONV])
    w_co = din("w_conv_out", [DCONV, D])
    lq1 = din("lambda_q1", [64])
    lk1 = din("lambda_k1", [64])
    lq2 = din("lambda_q2", [64])
    lk2 = din("lambda_k2", [64])
    subg = din("subln_g", [128])
    w_ao = din("w_attn_out", [D, D])
    norm_mem = din("norm_mem", [D])
    w_mk = din("w_mem_k", [D, 1024])
    w_mv = din("w_mem_v", [D, 1024])
    w_mo = din("w_mem_out", [1024, D])
    w_o = din("w_out", [D, D])
    norm_ffn = din("norm_ffn", [D])
    w_rg = din("w_router_grp", [D, 4])
    b_rg = din("b_router_grp", [4])
    w_re = din("w_router_exp", [D, 32])
    b_re = din("b_router_exp", [32])
    w_eg = din("w_exp_gate", [NEXP, D, DEXP])
    w_eu = din("w_exp_up", [NEXP, D, DEXP])
    w_ed = din("w_exp_down", [NEXP, DEXP, D])
    norm_final = din("norm_final", [D])

    NTOK = (SEQ // 2048) * GT + NSAMP
    y_o = dout("y", [NTOK, D])
    k_o = dout("kout", [NTOK, D])
    v_o = dout("vout", [NTOK, D])
    convp_o = dout("convp", [30, DCONV])
    convs_o = dout("convs", [30, DCONV])
    memk_o = dout("memk", [256, 1024])
    memv_o = dout("memv", [256, 1024])

    s_win = dscr("s_win", [D, NIN])
    s_wco = dscr("s_wco", [DCONV, D])
    s_wao = dscr("s_wao", [D, D])
    s_wmk = dscr("s_wmk", [D, 1024])
    s_wmv = dscr("s_wmv", [D, 1024])
    s_wmo = dscr("s_wmo", [1024, D])
    s_wo = dscr("s_wo", [D, D])
    s_wr = dscr("s_wr", [D, 36])
    s_eg = dscr("s_eg", [NEXP, D, DEXP])
    s_eu = dscr("s_eu", [NEXP, D, DEXP])
    s_ed = dscr("s_ed", [NEXP, DEXP, D])
    NBP = SEQ // 128
    NBS = PAST // 128 + 1
    NGRP_ = SEQ // 2048
    s_ktp = dscr("s_ktp", [16, 128, SEQ])
    s_vp = dscr("s_vp", [16, 128, NBP, 128])
    s_dg = dscr("s_dg", [8, 128, 31 * 128])
    s_kts = dscr("s_kts", [16, 128, NBS * 128])
    s_vs = dscr("s_vs", [16, 128, NBS, 128])

    with ExitStack() as es:
        es.enter_context(nc.allow_non_contiguous_dma(reason="small parameter loads"))
        kb = KB(nc, es)
        pe, act, dve, pool, sp = kb.pe, kb.act, kb.dve, kb.pool, kb.sp

        ps = []
        psb = []
        for i in range(8):
            t = es.enter_context(nc.psum_tensor("ps%d" % i, [128, 512], F32))
            ps.append(t)
            psb.append(Buf("ps%d" % i))
        rr = {"a": 0, "s": 0}

        def next_ps(pool_name="a"):
            if pool_name == "a":
                i = rr["a"] % 8
                rr["a"] += 1
            else:
                i = rr["s"] % 4
                rr["s"] += 1
            return i

        if DBG_STOP == "pm1":
            kb.dma(sp, y_o[0:128, :], xfull[0:128, :], [], [], Buf("t"))
            kb.barrier()
            return nc
        if DBG_STOP == "p0a":
            kb.barrier()
            return nc
        ident, B_c = kb.sb("ident", [128, 128], BF16)
        ones, _ = kb.sb("ones", [128, 128], BF16)
        kb.op(pool, lambda e: e.memset(ident[:], 0.0), [], [B_c])
        kb.op(pool, lambda e: e.affine_select(out=ident[:], in_=ident[:], pattern=[[-1, 128]],
                                              compare_op=ALU.not_equal, fill=1.0, base=0,
                                              channel_multiplier=1), [B_c], [B_c])
        kb.op(pool, lambda e: e.memset(ones[:], 1.0), [], [B_c])

        cols, B_cols = kb.sb("cols", [128, 64], F32)
        kb.op(dve, lambda e: e.memset(cols[:], 0.0), [], [B_cols])
        pfm, B_pfm = kb.sb("pfm", [128, 3, 16], F32)
        for i, src in enumerate((norm_mix, norm_mem, norm_ffn)):
            kb.dma(sp, pfm[:, i, :], src.rearrange("(k p) -> p k", p=128), [], [B_pfm], B_pfm)
        pcv, B_pcv = kb.sb("pcv", [128, 34 + 3, 8], F32)
        kb.dma(sp, pcv[:, 0:31, :], w_dw.rearrange("t (k p) -> p t k", p=128), [], [B_pcv], B_pcv)
        kb.dma(sp, pcv[:, 31, :], b_dw.rearrange("(k p) -> p k", p=128), [], [B_pcv], B_pcv)
        kb.dma(sp, pcv[:, 32, :], ln_g.rearrange("(k p) -> p k", p=128), [], [B_pcv], B_pcv)
        kb.dma(sp, pcv[:, 33, :], ln_b.rearrange("(k p) -> p k", p=128), [], [B_pcv], B_pcv)
        lamt, B_lam = kb.sb("lamt", [128, 4, 64], F32)
        for i, src in enumerate((lq1, lk1, lq2, lk2)):
            kb.dma(sp, lamt[:, i, :], src.partition_broadcast(128), [], [B_lam], B_lam)
        subgc, B_subg = kb.sb("subgc", [128, 1], F32)
        kb.dma(sp, subgc[:, :], subg.rearrange("(p o) -> p o", o=1), [], [B_subg], B_subg)
        gpt, B_gp = kb.sb("gpt", [128, 4], F32)
        kb.dma(sp, gpt[:, :], gpos[:, :], [], [B_gp], B_gp)
        rbias, B_rb = kb.sb("rbias", [128, 36], F32)
        kb.dma(sp, rbias[:, 0:4], b_rg.partition_broadcast(128), [], [B_rb], B_rb)
        kb.dma(sp, rbias[:, 4:36], b_re.partition_broadcast(128), [], [B_rb], B_rb)
        wr_f, B_wrf = kb.sb("wr_f", [128, KC, 36], F32)
        wr_sb, B_wrs = kb.sb("wr_sb", [128, KC, 36], BF16)
        kb.dma(sp, wr_f[:, :, 0:4], w_rg.rearrange("(k p) c -> p k c", p=128), [], [B_wrf], B_wrf)
        kb.dma(sp, wr_f[:, :, 4:36], w_re.rearrange("(k p) c -> p k c", p=128), [], [B_wrf], B_wrf)
        kb.op(dve, lambda e: e.tensor_copy(out=wr_sb[:, :, :], in_=wr_f[:, :, :]), [B_wrf], [B_wrs])

        gbc, B_gbc = kb.sb("gbc", [128, 2, KC, 128], BF16)

        def fill_gbc(dst, src_i):
            for k in range(KC):
                kb.op(dve, lambda e, k=k: e.tensor_copy(out=dst[:, k, :], in_=pfm[:, src_i, k:k + 1].to_broadcast([128, 128])),
                      [B_pfm], [B_gbc])
        fill_gbc(gbc[:, 0, :, :], 0)
        fill_gbc(gbc[:, 1, :, :], 2)
        G_MIX, G_FFN = gbc[:, 0, :, :], gbc[:, 1, :, :]
        lam_init = 0.8 - 0.6 * math.exp(-0.3 * 0)
        ltmp, B_lt = kb.sb("ltmp", [128, 2, 64], F32)
        kb.op(dve, lambda e: e.tensor_tensor(out=ltmp[:, 0, :], in0=lamt[:, 0, :], in1=lamt[:, 1, :], op=ALU.mult),
              [B_lam], [B_lt])
        kb.op(dve, lambda e: e.tensor_tensor(out=ltmp[:, 1, :], in0=lamt[:, 2, :], in1=lamt[:, 3, :], op=ALU.mult),
              [B_lam], [B_lt])
        kb.op(dve, lambda e: e.reduce_sum(out=cols[:, 4:6], in_=ltmp[:, :, :], axis=AX.X), [B_lt], [B_cols])
        kb.op(act, lambda e: e.activation(out=cols[:, 6:8], in_=cols[:, 4:6], func=ACT.Exp), [B_cols], [B_cols])
        kb.op(dve, lambda e: e.tensor_tensor(out=cols[:, 0:1], in0=cols[:, 6:7], in1=cols[:, 7:8], op=ALU.subtract),
              [B_cols], [B_cols])
        kb.op(dve, lambda e: e.tensor_scalar(out=cols[:, 0:1], in0=cols[:, 0:1], scalar1=lam_init, scalar2=None,
                                             op0=ALU.add), [B_cols], [B_cols])
        kb.op(dve, lambda e: e.tensor_scalar(out=cols[:, 1:2], in0=cols[:, 0:1], scalar1=-1.0, scalar2=None,
                                             op0=ALU.mult), [B_cols], [B_cols])
        kb.op(dve, lambda e: e.tensor_scalar(out=cols[:, 2:3], in0=subgc[:, 0:1], scalar1=(1.0 - lam_init),
                                             scalar2=None, op0=ALU.mult), [B_subg, B_cols], [B_cols])
        for m in range(1, 4):
            kb.op(dve, lambda e, m=m: e.tensor_scalar(out=cols[:, 8 + m:9 + m], in0=gpt[:, m:m + 1],
                                                      scalar1=gpt[:, 0:1], scalar2=NEG, op0=ALU.is_gt,
                                                      op1=ALU.mult), [B_gp, B_cols], [B_cols])

        B_win = {}
        cth = ([], 2)
        for nm, c0, c1 in (("kv", C_K, C_MQ), ("agt", C_AGT, C_Q), ("g0", C_G, C_G + D), ("mq", C_MQ, C_G),
                           ("g2", C_G + 2 * D, NIN), ("q", C_Q, C_K), ("g1", C_G + D, C_G + 2 * D)):
            b = Buf("cw_" + nm)
            B_win[nm] = b
            for r in range(0, D, 512):
                kb.dma(pool, s_win[r:r + 512, c0:c1], w_in[r:r + 512, c0:c1], [], [b], b, cth)

        def cast_simple(name, dst, src, rows):
            b = Buf("cw_" + name)
            step = 512
            for r in range(0, rows, step):
                kb.dma(pool, dst[r:r + step, :], src[r:r + step, :], [], [b], b, cth)
            return b

        B_wmk = cast_simple("wmk", s_wmk, w_mk, D)
        B_wmv = cast_simple("wmv", s_wmv, w_mv, D)
        B_wco = cast_simple("wco", s_wco, w_co, DCONV)
        B_wao = cast_simple("wao", s_wao, w_ao, D)
        B_wmo = cast_simple("wmo", s_wmo, w_mo, 1024)
        B_wo = cast_simple("wo", s_wo, w_o, D)
        B_exp = []
        for g8 in range(NEXP // 4):
            b = Buf("cw_e%d" % g8)
            B_exp.append(b)
            for e in range(g8 * 4, g8 * 4 + 4):
                kb.dma(pool, s_eg[e], w_eg[e], [], [b], b, cth)
                kb.dma(pool, s_eu[e], w_eu[e], [], [b], b, cth)
                kb.dma(pool, s_ed[e], w_ed[e], [], [b], b, cth)

        if DBG_STOP == "p0":
            kb.barrier()
            return nc
        nsts = [kb.sb("nst%d" % i, [128, 2], F32) for i in range(2)]
        nsti = [0]

        def norm_prep(xt, Bx, nt, xn, Bxn):
            nst, B_nst = nsts[nsti[0] % 2]
            nsti[0] += 1
            c_ss = nst[:nt, 0:1]
            c_r = nst[:nt, 1:2]
            kb.op(dve, lambda e: e.memset(c_ss, 0.0), [], [B_nst])
            kb.op(act, lambda e: e.activation(out=xn[:nt, :], in_=xt, func=ACT.Square, accum_out=c_ss),
                  [Bx, B_nst], [Bxn, B_nst])
            kb.op(act, lambda e: e.activation(out=c_r, in_=c_ss, func=ACT.Sqrt, scale=1.0 / D, bias=EPS),
                  [B_nst], [B_nst])
            kb.op(dve, lambda e: e.reciprocal(out=c_r, in_=c_r), [B_nst], [B_nst])
            kb.op(act, lambda e: e.activation(out=xn[:nt, :], in_=xt, func=ACT.Copy, scale=c_r),
                  [Bx, B_nst], [Bxn])

        def norm_tr(nt, gi, hT, BhT, t0, xn, Bxn):
            for half in range(2):
                b = next_ps()
                pv = ps[b][:].bitcast(BF16).rearrange("p (j c) -> p j c", c=128)
                for j in range(8):
                    k = half * 8 + j
                    kb.op(pe, lambda e, j=j, k=k: e.transpose(pv[:, j, :nt], xn[:nt, k * 128:(k + 1) * 128],
                                                              ident[:nt, :nt]), [Bxn, B_c], [psb[b]])
                kb.op(dve, lambda e, half=half, pv=pv: e.tensor_tensor(
                    out=hT[:, half * 8:half * 8 + 8, t0:t0 + nt], in0=pv[:, :, :nt],
                    in1=gi[:, half * 8:half * 8 + 8, :nt], op=ALU.mult), [psb[b], B_gbc], [BhT])

        def rmsnorm_T(xt, Bx, nt, gi, hT, BhT, t0, xn, Bxn, sq, Bsq, col0):
            norm_prep(xt, Bx, nt, xn, Bxn)
            norm_tr(nt, gi, hT, BhT, t0, xn, Bxn)

        def evac(i, out_ap, in_ap, reads, writes, scale=None):
            if i % 2 == 0:
                if scale is None:
                    kb.op(act, lambda e: e.activation(out=out_ap, in_=in_ap, func=ACT.Copy), reads, writes)
                else:
                    kb.op(act, lambda e: e.activation(out=out_ap, in_=in_ap, func=ACT.Copy, scale=scale), reads, writes)
            else:
                if scale is None:
                    kb.op(dve, lambda e: e.tensor_copy(out=out_ap, in_=in_ap), reads, writes)
                else:
                    kb.op(dve, lambda e: e.tensor_scalar(out=out_ap, in0=in_ap, scalar1=scale, scalar2=None,
                                                         op0=ALU.mult), reads, writes)

        def mm_acc(b, out_ap, pairs, reads):
            n = len(pairs)
            for i, (l, r) in enumerate(pairs):
                kb.op(pe, lambda e, l=l, r=r, i=i: e.matmul(out_ap, l, r, start=(i == 0), stop=(i == n - 1)),
                      reads, [psb[b]])

        B_ktp, B_vp, B_kts, B_vs = Buf("ktp"), Buf("vp"), Buf("kts"), Buf("vs")
        dgB = Buf("s_dg")
        with ExitStack() as es0:
            kb_es = kb.es
            kb.es = es0
            B_dg = dgB
            dgs = [kb.sb("dgs%d" % i, [128, 31, 128], BF16) for i in range(2)]
            for cc in range(8):
                dg_, Bdg_ = dgs[cc % 2]
                for k in range(31):
                    kb.op(dve, lambda e, cc=cc, k=k, dg_=dg_: e.tensor_scalar(out=dg_[:, k, :], in0=ident[:, :],
                                                                            scalar1=pcv[:, k, cc:cc + 1], scalar2=None,
                                                                            op0=ALU.mult), [B_c, B_pcv], [Bdg_])
                kb.dma(sp, s_dg[cc], dg_[:, :, :].rearrange("p k c -> p (k c)"), [Bdg_], [B_dg], Bdg_)
            kb.sync_bufs([d_[1] for d_ in dgs])
            kb.es = kb_es
        with ExitStack() as es1:
            kb_es = kb.es
            kb.es = es1
            p1w = [kb.sb("p1w%d" % i, [128, KC, 512], BF16) for i in range(3)]
            xs = [kb.sb("p1x%d" % i, [128, D], F32) for i in range(2)]
            xn1s = [kb.sb("p1xn%d" % i, [128, D], BF16) for i in range(2)]
            xn1, B_xn1 = xn1s[0]
            sq1, B_sq1 = xn1, B_xn1
            CH = 1024
            hTs = [kb.sb("p1hT%d" % i, [128, KC, CH], BF16) for i in range(2)]
            kts = [kb.sb("p1kt%d" % i, [128, 4, GT], BF16) for i in range(2)]
            vss = [kb.sb("p1v%d" % i, [128, 4, 512], BF16) for i in range(2)]
            ckb = [kb.sb("p1ck%d" % i, [128, D], BF16) for i in range(2)]
            ckf = [kb.sb("p1ckf%d" % i, [128, D], F32) for i in range(2)]
            ktc = [kb.sb("p1ktc%d" % i, [128, 16, 256], BF16) for i in range(2)]
            xi = [0]
            ki = [0]
            wq = {"n": 0, "loaded": 0}
            NCHK = SEQ // CH
            NCH = NCHK + 1
            vp_v = s_vp.rearrange("h p nb d -> p h nb d")
            vs_v = s_vs.rearrange("h p nb d -> p h nb d")
            ktp_v = s_ktp.rearrange("h p n -> p h n")
            kts_v = s_kts.rearrange("h p n -> p h n")

            def p1_wload(upto):
                while wq["loaded"] < min(upto, NCH * 8):
                    i = wq["loaded"]
                    blk = i % 8
                    w, Bw = p1w[i % 3]
                    kb.dma(sp, w[:, :, :], s_win[:, C_K + blk * 512:C_K + (blk + 1) * 512].rearrange(
                        "(k p) c -> p k c", p=128), [B_win["kv"]], [Bw], Bw)
                    wq["loaded"] += 1

            def kv_norm(ci, src_rows, ntok):
                hT, BhT = hTs[ci % 2]
                ntile = (ntok + 127) // 128
                for t in range(ntile):
                    nt = min(128, ntok - t * 128)
                    xt, Bx = xs[xi[0] % 2]
                    xi[0] += 1
                    kb.dma(sp, xt[:nt, :], src_rows[t * 128:t * 128 + nt, :], [], [Bx], Bx)
                    rmsnorm_T(xt[:nt, :], Bx, nt, G_MIX, hT, BhT, t * 128, xn1, B_xn1, sq1, B_sq1, 16)

            cache_nb = [0]
            cache_ld = [0]

            def cache_load():
                nb = cache_ld[0]
                if nb >= PAST // 128:
                    return
                cache_ld[0] += 1
                ct, Bct = ckb[nb % 2]
                cf, Bcf = ckf[nb % 2]
                kb.dma(sp, cf[:, :], ck[nb * 128:(nb + 1) * 128, :], [], [Bcf], Bcf)
                kb.op(act, lambda e, ct=ct, cf=cf: e.activation(out=ct[:, :], in_=cf[:, :], func=ACT.Copy), [Bcf], [Bct])

            def cache_blocks(n):
                for _ in range(n):
                    nb = cache_nb[0]
                    if nb >= PAST // 128:
                        return
                    cache_nb[0] += 1
                    while cache_ld[0] <= nb:
                        cache_load()
                    ct, Bct = ckb[nb % 2]
                    kt, Bk = ktc[(nb // 2) % 2]
                    for half in range(2):
                        b = next_ps()
                        pv = ps[b][:].bitcast(BF16).rearrange("p (j c) -> p j c", c=128)
                        for j in range(8):
                            h = half * 8 + j
                            kb.op(pe, lambda e, j=j, h=h, pv=pv, ct=ct: e.transpose(pv[:, j, :], ct[:, h * 128:(h + 1) * 128],
                                                                                  ident[:, :]), [Bct, B_c], [psb[b]])
                        evac(half, kt[:, half * 8:half * 8 + 8, (nb % 2) * 128:(nb % 2 + 1) * 128], pv[:, :, :],
                             [psb[b]], [Bk])
                    if nb % 2 == 1:
                        kb.dma(sp, kts_v[:, :, (nb - 1) * 128:(nb + 1) * 128], kt[:, :, :], [Bk], [B_kts], Bk)
                    cache_load()

            pend = []

            def norm_sched(ci_next, src_rows, ntok):
                hTn, BhTn = hTs[ci_next % 2]
                ntile = (ntok + 127) // 128
                jobs = []
                for t in range(ntile):
                    nt = min(128, ntok - t * 128)
                    jobs.append((t, nt))

                def prep(t, nt):
                    xt, Bx = xs[xi[0] % 2]
                    xnb, Bxnb = xn1s[xi[0] % 2]
                    xi[0] += 1
                    kb.dma(sp, xt[:nt, :], src_rows[t * 128:t * 128 + nt, :], [], [Bx], Bx)
                    norm_prep(xt[:nt, :], Bx, nt, xnb, Bxnb)
                    return (nt, hTn, BhTn, t * 128, xnb, Bxnb)
                return jobs, prep

            def kv_chunk(ci, ntok, kt_dst_fn, v_dst_fn, Bkt, Bv, nxt=None, ncache=0):
                hT, BhT = hTs[ci % 2]
                jobs, prep = nxt if nxt is not None else ([], None)
                per_blk = (len(jobs) + 7) // 8 if jobs else 0
                ji = 0
                for blk in range(8):
                    while pend:
                        a_ = pend.pop(0)
                        norm_tr(a_[0], G_MIX, a_[1], a_[2], a_[3], a_[4], a_[5])
                    for _ in range(per_blk):
                        if ji < len(jobs):
                            pend.append(prep(*jobs[ji]))
                            ji += 1
                    if ncache and blk % 2 == 1:
                        cache_blocks(ncache)
                    i = wq["n"]
                    wq["n"] += 1
                    p1_wload(i + 2)
                    w, Bw = p1w[i % 3]
                    for c0 in range(0, ntok, GT):
                        nq = min(GT, ntok - c0)
                        ntile = (nq + 127) // 128
                        if blk < 4:
                            kt, Bk = kts[ki[0] % 2]
                            ki[0] += 1
                            for hh in range(4):
                                b = next_ps()
                                mm_acc(b, ps[b][:, :nq], [(w[:, k, hh * 128:(hh + 1) * 128], hT[:, k, c0:c0 + nq])
                                                          for k in range(KC)], [Bw, BhT])
                                evac(hh, kt[:, hh, :nq], ps[b][:, :nq], [psb[b]], [Bk])
                            kb.dma(sp, kt_dst_fn(blk, c0, nq), kt[:, :, :nq], [Bk], [Bkt], Bk)
                        else:
                            cb = blk - 4
                            vs_, Bvs = vss[ki[0] % 2]
                            ki[0] += 1
                            for t in range(ntile):
                                nt = min(128, nq - t * 128)
                                b = next_ps()
                                mm_acc(b, ps[b][:nt, :], [(hT[:, k, c0 + t * 128:c0 + t * 128 + nt], w[:, k, :])
                                                          for k in range(KC)], [Bw, BhT])
                                evac(t, vs_[:nt, t, :], ps[b][:nt, :], [psb[b]], [Bvs])
                                kb.dma(sp, v_dst_fn(c0 // 128 + t, nt, cb), vs_[:nt, t, :].rearrange("p (h d) -> p h d", d=128),
                                       [Bvs], [Bv], Bvs)
                while pend:
                    a_ = pend.pop(0)
                    norm_tr(a_[0], G_MIX, a_[1], a_[2], a_[3], a_[4], a_[5])

            kv_norm(0, xfull[0:CH, :], CH)
            cache_load()
            for ci in range(NCHK):
                if ci + 1 < NCHK:
                    mid = norm_sched(ci + 1, xfull[(ci + 1) * CH:(ci + 2) * CH, :], CH)
                else:
                    mid = norm_sched(NCHK, xsamp, NSAMP)
                kv_chunk(ci, CH,
                         lambda blk, c0, nq, ci=ci: ktp_v[:, blk * 4:blk * 4 + 4, ci * CH + c0:ci * CH + c0 + nq],
                         lambda tt, nt, cb, ci=ci: vp_v[:nt, cb * 4:cb * 4 + 4, ci * (CH // 128) + tt, :], B_ktp, B_vp, mid,
                         ncache=1)
            kv_chunk(NCHK, NSAMP, lambda blk, c0, nq: kts_v[:, blk * 4:blk * 4 + 4, PAST:PAST + NSAMP],
                     lambda tt, nt, cb: vs_v[:nt, cb * 4:cb * 4 + 4, NBS - 1, :], B_kts, B_vs)
            for nb0 in range(PAST // 128):
                kb.dma(pool, vs_v[:, :, nb0, :],
                       cv[nb0 * 128:(nb0 + 1) * 128, :].rearrange("p (h d) -> p h d", d=128),
                       [], [B_vs], B_vs)
            cache_blocks(PAST // 128)
            kb.sync_bufs([x_[1] for x_ in (p1w + xs + xn1s + hTs + kts + vss + ckb + ckf + ktc)])
            kb.es = kb_es

        if DBG_STOP == "p1":
            return nc
        mkT_p, B_mkp = kb.sb("mkT_p", [128, 8, 256], BF16)
        mv_p, B_mvp = kb.sb("mv_p", [128, 2, 1024], BF16)

        xn, B_xn = kb.sb("xn", [128, D], BF16)
        sq, B_sq = xn, B_xn
        ostage, B_ost = kb.sb("ostage", [128, 2, 512], F32)
        with ExitStack() as es2:
            kb_es = kb.es
            kb.es = es2
            mx, B_mx = kb.sb("mx", [128, 2, D], F32)
            gmem, _ = kb.sb("gmem", [128, KC, 128], BF16)
            fill_gbc(gmem, 1)
            hmT, B_hmT = kb.sb("hmT", [128, KC, 256], BF16)
            wm, B_wm = kb.sb("wm", [128, KC, 1024], BF16)
            for t in range(2):
                kb.dma(sp, mx[:, t, :], memx[t * 128:(t + 1) * 128, :], [], [B_mx], B_mx)
            for t in range(2):
                rmsnorm_T(mx[:, t, :], B_mx, 128, gmem[:, :, :], hmT, B_hmT, t * 128, xn, B_xn, sq, B_sq, 16)
            if DBG_STOP == "p2a":
                kb.barrier()
                return nc
            for which, (sw, Bsw, outd) in enumerate(((s_wmk, B_wmk, memk_o), (s_wmv, B_wmv, memv_o))):
                if DBG_STOP == "p2b" and which == 1:
                    kb.barrier()
                    return nc
                kb.dma(sp, wm[:, :, :], sw.rearrange("(k p) c -> p k c", p=128), [Bsw], [B_wm], B_wm)
                if which == 0:
                    for cc in range(8):
                        b = next_ps()
                        mm_acc(b, ps[b][:, :256], [(wm[:, k, cc * 128:(cc + 1) * 128], hmT[:, k, :]) for k in range(KC)],
                               [B_wm, B_hmT])
                        evac(cc, mkT_p[:, cc, :], ps[b][:, :256], [psb[b]], [B_mkp])
                for t in range(2):
                    for cb in range(2):
                        b = next_ps()
                        mm_acc(b, ps[b][:, :], [(hmT[:, k, t * 128:(t + 1) * 128], wm[:, k, cb * 512:(cb + 1) * 512])
                                                for k in range(KC)], [B_wm, B_hmT])
                        evac(0, ostage[:, cb, :], ps[b][:, :], [psb[b]], [B_ost])
                        if which == 1:
                            kb.op(dve, lambda e, t=t, cb=cb: e.tensor_copy(out=mv_p[:, t, cb * 512:(cb + 1) * 512],
                                                                           in_=ostage[:, cb, :]), [B_ost], [B_mvp])
                        kb.dma(pool, outd[t * 128:(t + 1) * 128, cb * 512:(cb + 1) * 512], ostage[:, cb, :], [B_ost], [], B_ost)
            if DBG_STOP == "p2c":
                kb.barrier()
                return nc
            kb.barrier()
            kb.es = kb_es

        xg, B_xg = kb.sb("xg", [128, 4, D], F32)
        hT, B_hT = kb.sb("hT", [128, KC, GT], BF16)
        NSLOT = 2
        wsl = [kb.sb("ws%d" % i, [128, 8192], BF16) for i in range(NSLOT)]
        merged, B_mg = kb.sb("merged", [128, KC, GT], BF16)
        sig, B_sig = kb.sb("sig", [128, 4, GT], F32)
        tmpf, B_tmpf = kb.sb("tmpf", [128, 2, GT], F32)


        if DBG_STOP == "p2":
            return nc
        steps = []

        def run_steps():
            emitted = [0]

            def emit_load(i):
                loads = steps[i][0]
                if not loads:
                    return
                sl, Bsl = wsl[steps[i][2] % NSLOT]
                for off, src, nk, c, Bsrc in loads:
                    dst = sl[:, off:off + nk * c].rearrange("p (k c) -> p k c", c=c)
                    kb.dma(sp, dst, src.rearrange("(k p) c -> p k c", p=128), [Bsrc], [Bsl], Bsl)

            widx = 0
            for i, st in enumerate(steps):
                if st[0]:
                    steps[i] = (st[0], st[1], widx)
                    widx += 1
                else:
                    steps[i] = (st[0], st[1], -1)
            n = len(steps)
            nxt = 0
            for i in range(n):
                ahead = 0
                j = i
                while j < n and ahead < NSLOT - 1:
                    if steps[j][0]:
                        ahead += 1
                        if j >= nxt:
                            emit_load(j)
                            nxt = j + 1
                    j += 1
                nxt = max(nxt, i + 1) if not steps[i][0] else nxt
                if steps[i][0]:
                    sl, Bsl = wsl[steps[i][2] % NSLOT]
                    steps[i][1](sl, Bsl)
                else:
                    steps[i][1](None, None)

        def group(gi, NQ, x_src, row0, halo_kind, mkT, B_mk, mv, B_mv, kt_scr, B_kt, v_scr, B_v, blocks, conv_out):
            ntile = (NQ + 127) // 128
            tl = [(t, min(128, NQ - t * 128)) for t in range(ntile)]
            st = {}

            def s_load(_, __):
                for t, nt in tl:
                    kb.dma(sp, xg[:nt, t, :], x_src[t * 128:t * 128 + nt, :], [], [B_xg], B_xg)
                for t, nt in tl:
                    rmsnorm_T(xg[:nt, t, :], B_xg, nt, G_MIX, hT, B_hT, t * 128, xn, B_xn, sq, B_sq, 16)
            steps.append(([], s_load))

            for which, c0, outd in ((0, C_K, k_o), (1, C_V, v_o)):
                for cb in range(4):
                    def s_kvout(sl, Bsl, cb=cb, outd=outd):
                        w = sl[:, 0:KC * 512].rearrange("p (k c) -> p k c", c=512)
                        for t, nt in tl:
                            b = next_ps()
                            mm_acc(b, ps[b][:nt, :], [(hT[:, k, t * 128:t * 128 + nt], w[:, k, :]) for k in range(KC)],
                                   [Bsl, B_hT])
                            evac(t, ostage[:nt, t % 2, 0:512], ps[b][:nt, :], [psb[b]], [B_ost])
                            kb.dma(pool, outd[row0 + t * 128:row0 + t * 128 + nt, cb * 512:(cb + 1) * 512],
                                   ostage[:nt, t % 2, 0:512], [B_ost], [], B_ost)
                    steps.append(([(0, s_win[:, c0 + cb * 512:c0 + (cb + 1) * 512], KC, 512, B_win["kv"])], s_kvout))

            if STAGE < 2:
                return
            is_s = (halo_kind == "cache")
            sc = {}

            def open_scope():
                kb.barrier()
                sc["es"] = ExitStack()
                sc["old"] = kb.es
                kb.es = sc["es"]

            def close_scope(_=None, __=None):
                kb.barrier()
                kb.es = sc["old"]
                sc["es"].close()

            def wstep(src, nk, Bsrc, fn, c=512):
                steps.append(([(0, src, nk, c, Bsrc)], fn))

            def branch_out(bidx, gname, wsrc_fn, wk_n, Bw, rhs_key):
                for cb in range(4):
                    def s_gate(sl, Bsl, cb=cb):
                        w = sl[:, 0:KC * 512].rearrange("p (k c) -> p k c", c=512)
                        for j in range(4):
                            b = next_ps()
                            mm_acc(b, ps[b][:, :NQ], [(w[:, k, j * 128:(j + 1) * 128], hT[:, k, :NQ]) for k in range(KC)],
                                   [Bsl, B_hT])
                            kb.op(act, lambda e, j=j, b=b: e.activation(out=sig[:, j, :NQ], in_=ps[b][:, :NQ],
                                                                        func=ACT.Sigmoid), [psb[b]], [B_sig])
                    wstep(s_win[:, C_G + bidx * D + cb * 512:C_G + bidx * D + (cb + 1) * 512], KC, B_win[gname], s_gate)

                    def s_y(sl, Bsl, cb=cb):
                        rhsT, BrhsT = sc[rhs_key]
                        w = sl[:, 0:wk_n * 512].rearrange("p (k c) -> p k c", c=512)
                        for j in range(4):
                            c = cb * 4 + j
                            b = next_ps()
                            mm_acc(b, ps[b][:, :NQ], [(w[:, k, j * 128:(j + 1) * 128], rhsT[:, k, :NQ]) for k in range(wk_n)],
                                   [Bsl, BrhsT])
                            if bidx == 0:
                                kb.op(dve, lambda e, j=j, b=b, c=c: e.tensor_tensor(out=merged[:, c, :NQ], in0=ps[b][:, :NQ],
                                                                                  in1=sig[:, j, :NQ], op=ALU.mult),
                                      [psb[b], B_sig], [B_mg])
                            else:
                                kb.op(dve, lambda e, j=j, b=b: e.tensor_tensor(out=tmpf[:, j % 2, :NQ], in0=ps[b][:, :NQ],
                                                                               in1=sig[:, j, :NQ], op=ALU.mult),
                                      [psb[b], B_sig], [B_tmpf])
                                kb.op(dve, lambda e, j=j, c=c: e.tensor_tensor(out=merged[:, c, :NQ], in0=merged[:, c, :NQ],
                                                                               in1=tmpf[:, j % 2, :NQ], op=ALU.add),
                                      [B_tmpf, B_mg], [B_mg])
                    wstep(wsrc_fn(cb), wk_n, Bw, s_y)

            def s_conv_open(_, __):
                open_scope()
                sc["uext"] = kb.sb("uext", [128, 8, 32 + GT], F32)
                sc["cacc"] = kb.sb("cacc", [128, 8, GT], F32)
                sc["ub"] = kb.sb("ub", [128, 8, 32 + GT], BF16)
                sc["zT"] = kb.sb("zT", [128, 8, GT], BF16)
                sc["hh"] = kb.sb("hh", [128, KC, 32], BF16)
                sc["st"] = kb.sb("cst", [128, 3, GT], F32)
                sc["identf"] = kb.sb("identf", [128, 128], F32)
                uext, B_u = sc["uext"]
                hh, B_hh = sc["hh"]
                cacc_, B_xh = sc["cacc"]
                xh = cacc_[:, 0:4, :].rearrange("p a c -> p (a c)")
                identf, B_if = sc["identf"]
                kb.op(dve, lambda e: e.tensor_copy(out=identf[:, :], in_=ident[:, :]), [B_c], [B_if])
                if not is_s:
                    kb.dma(sp, xh[:32, :], xhalo[gi], [], [B_xh], B_xh)
                    rmsnorm_T(xh[:32, :], B_xh, 32, G_MIX, hh, B_hh, 0, xn, B_xn, sq, B_sq, 16)
                else:
                    kb.op(dve, lambda e: e.memset(xh[:32, 0:DCONV], 0.0), [], [B_xh])
                    kb.dma(sp, xh[2:32, 0:DCONV], cconv[:, :], [], [B_xh], B_xh)
                    kb.op(act, lambda e: e.activation(out=xn[:32, 0:DCONV], in_=xh[:32, 0:DCONV], func=ACT.Copy),
                          [B_xh], [B_xn])
                    b = next_ps()
                    pv = ps[b][:].bitcast(BF16).rearrange("p (j c) -> p j c", c=128)
                    for cc in range(8):
                        kb.op(pe, lambda e, cc=cc, pv=pv: e.transpose(pv[:, cc, :32], xn[:32, cc * 128:(cc + 1) * 128],
                                                                      ident[:32, :32]), [B_xn, B_c], [psb[b]])
                    kb.op(dve, lambda e, pv=pv: e.tensor_copy(out=uext[:, :, 0:32], in_=pv[:, :, :32]), [psb[b]], [B_u])
            steps.append(([], s_conv_open))

            for half_i, cbase in ((0, C_AGT), (1, C_AGT + DCONV)):
                for cb in range(2):
                    def s_agt(sl, Bsl, half_i=half_i, cb=cb):
                        uext, B_u = sc["uext"]
                        hh, B_hh = sc["hh"]
                        w = sl[:, 0:KC * 512].rearrange("p (k c) -> p k c", c=512)
                        for j in range(4):
                            cc = cb * 4 + j
                            parts = [(32, NQ, hT, B_hT)]
                            if not is_s:
                                parts.append((0, 32, hh, B_hh))
                            for (o0, n, src, Bs) in parts:
                                b = next_ps()
                                mm_acc(b, ps[b][:, :n], [(w[:, k, j * 128:(j + 1) * 128], src[:, k, :n]) for k in range(KC)],
                                       [Bsl, Bs])
                                if half_i == 0:
                                    evac(j, uext[:, cc, o0:o0 + n], ps[b][:, :n], [psb[b]], [B_u])
                                else:
                                    kb.op(act, lambda e, b=b, n=n: e.activation(out=tmpf[:, 0, :n], in_=ps[b][:, :n],
                                                                                func=ACT.Sigmoid), [psb[b]], [B_tmpf])
                                    kb.op(dve, lambda e, cc=cc, o0=o0, n=n: e.tensor_tensor(
                                        out=uext[:, cc, o0:o0 + n], in0=uext[:, cc, o0:o0 + n], in1=tmpf[:, 0, :n],
                                        op=ALU.mult), [B_tmpf, B_u], [B_u])
                    wstep(s_win[:, cbase + cb * 512:cbase + (cb + 1) * 512], KC, B_win["agt"], s_agt)

            def s_conv_pre(_, __):
                uext, B_u = sc["uext"]
                identf, B_if = sc["identf"]
                ub, B_ub = sc["ub"]
                xh, B_xh = ostage[:, :, :].rearrange("p a c -> p (a c)"), B_ost
                kb.op(act, lambda e: e.activation(out=ub[:, :, 0:32 + NQ], in_=uext[:, :, 0:32 + NQ], func=ACT.Copy),
                      [B_u], [B_ub])
                if conv_out is not None:
                    for half in range(2):
                        b = next_ps()
                        for j in range(4):
                            cc = half * 4 + j
                            kb.op(pe, lambda e, cc=cc, b=b, j=j: e.transpose(ps[b][:32, j * 128:(j + 1) * 128],
                                                                            uext[:, cc, NQ:NQ + 32], identf[:, :]),
                                  [B_u, B_if], [psb[b]])
                        evac(half, xh[:32, half * 512:(half + 1) * 512], ps[b][:32, :], [psb[b]], [B_xh])
                    kb.dma(pool, conv_out[:, :], xh[2:32, 0:DCONV], [B_xh], [], B_xh)
            steps.append(([], s_conv_pre))
            for cc in range(8):
                def s_dw(sl, Bsl, cc=cc):
                    ub, B_ub = sc["ub"]
                    cacc, B_ca = sc["cacc"]
                    b = next_ps()
                    mm_acc(b, ps[b][:, :NQ], [(sl[:, k * 128:(k + 1) * 128], ub[:, cc, 2 + k:2 + k + NQ]) for k in range(31)],
                           [Bsl, B_ub])
                    kb.op(act, lambda e, b=b: e.activation(out=cacc[:, cc, :NQ], in_=ps[b][:, :NQ], func=ACT.Identity,
                                                           bias=pcv[:, 31, cc:cc + 1]), [psb[b], B_pcv], [B_ca])
                steps.append(([(0, s_dg[cc], 1, 31 * 128, dgB)], s_dw))

            def s_conv(_, __):
                uext, B_u = sc["uext"]
                cacc, B_ca = sc["cacc"]
                zT, B_z = sc["zT"]
                cst, B_st = sc["st"]
                ysq, B_ysq = sc["ub"]
                kb.op(act, lambda e: e.activation(out=zT[:, :, :NQ], in_=cacc[:, :, :NQ], func=ACT.Copy), [B_ca], [B_z])
                kb.op(act, lambda e: e.activation(out=ysq[:, :, :NQ], in_=cacc[:, :, :NQ], func=ACT.Square), [B_ca], [B_ysq])
                b1 = next_ps()
                mm_acc(b1, ps[b1][:, :NQ], [(ones[:, :], zT[:, k, :NQ]) for k in range(8)], [B_z, B_c])
                b2 = next_ps()
                mm_acc(b2, ps[b2][:, :NQ], [(ones[:, :], ysq[:, k, :NQ]) for k in range(8)], [B_ysq, B_c])
                mean, msq, var = cst[:, 0, :NQ], cst[:, 1, :NQ], cst[:, 2, :NQ]
                kb.op(dve, lambda e: e.tensor_scalar(out=mean, in0=ps[b1][:, :NQ], scalar1=1.0 / DCONV, scalar2=None,
                                                     op0=ALU.mult), [psb[b1]], [B_st])
                kb.op(dve, lambda e: e.tensor_tensor(out=msq, in0=mean, in1=mean, op=ALU.mult), [B_st], [B_st])
                kb.op(dve, lambda e: e.scalar_tensor_tensor(out=var, in0=ps[b2][:, :NQ], scalar=1.0 / DCONV, in1=msq,
                                                            op0=ALU.mult, op1=ALU.subtract), [psb[b2], B_st], [B_st])
                kb.op(act, lambda e: e.activation(out=var, in_=var, func=ACT.Sqrt, bias=EPS), [B_st], [B_st])
                kb.op(dve, lambda e: e.reciprocal(out=var, in_=var), [B_st], [B_st])
                for cc in range(8):
                    kb.op(dve, lambda e, cc=cc: e.tensor_tensor(out=cacc[:, cc, :NQ], in0=cacc[:, cc, :NQ], in1=mean,
                                                                op=ALU.subtract), [B_st, B_ca], [B_ca])
                    kb.op(dve, lambda e, cc=cc: e.tensor_tensor(out=cacc[:, cc, :NQ], in0=cacc[:, cc, :NQ], in1=var,
                                                                op=ALU.mult), [B_st, B_ca], [B_ca])
                    kb.op(act, lambda e, cc=cc: e.activation(out=zT[:, cc, :NQ], in_=cacc[:, cc, :NQ], func=ACT.Silu,
                                                             scale=pcv[:, 32, cc:cc + 1], bias=pcv[:, 33, cc:cc + 1]),
                          [B_ca, B_pcv, B_z], [B_z])
            steps.append(([], s_conv))
            branch_out(0, "g0", lambda cb: s_wco[:, cb * 512:(cb + 1) * 512], 8, B_wco, "zT")
            steps.append(([], close_scope))

            def s_mem_open(_, __):
                open_scope()
                sc["mqT"] = kb.sb("mqT", [128, 8, GT], BF16)
                sc["moT"] = kb.sb("moT", [128, 8, GT], BF16)
                sc["mP"] = kb.sb("mP", [128, 2, GT], BF16)
                sc["mr"] = kb.sb("mr", [128, GT], F32)
            steps.append(([], s_mem_open))
            for cb in range(2):
                def s_mq(sl, Bsl, cb=cb):
                    mqT, B_mq = sc["mqT"]
                    w = sl[:, 0:KC * 512].rearrange("p (k c) -> p k c", c=512)
                    for j in range(4):
                        b = next_ps()
                        mm_acc(b, ps[b][:, :NQ], [(w[:, k, j * 128:(j + 1) * 128], hT[:, k, :NQ]) for k in range(KC)],
                               [Bsl, B_hT])
                        evac(j, mqT[:, cb * 4 + j, :NQ], ps[b][:, :NQ], [psb[b]], [B_mq], scale=1.0 / 16.0)
                wstep(s_win[:, C_MQ + cb * 512:C_MQ + (cb + 1) * 512], KC, B_win["mq"], s_mq)

            def s_mem(_, __):
                mqT, B_mq = sc["mqT"]
                moT, B_mo = sc["moT"]
                mP, B_mP = sc["mP"]
                mr, B_mr = sc["mr"]
                for hd in range(4):
                    for mt in range(2):
                        b = next_ps()
                        mm_acc(b, ps[b][:, :NQ], [(mkT[:, hd * 2 + hf, mt * 128:(mt + 1) * 128], mqT[:, hd * 2 + hf, :NQ])
                                                  for hf in range(2)], [B_mk, B_mq])
                        kb.op(act, lambda e, b=b, mt=mt: e.activation(out=mP[:, mt, :NQ], in_=ps[b][:, :NQ], func=ACT.Exp),
                              [psb[b]], [B_mP])
                    bl = next_ps()
                    mm_acc(bl, ps[bl][:, :NQ], [(ones[:, :], mP[:, mt, :NQ]) for mt in range(2)], [B_mP, B_c])
                    kb.op(dve, lambda e, bl=bl: e.reciprocal(out=mr[:, :NQ], in_=ps[bl][:, :NQ]), [psb[bl]], [B_mr])
                    for dh in range(2):
                        b = next_ps()
                        mm_acc(b, ps[b][:, :NQ], [(mv[:, mt, hd * 256 + dh * 128:hd * 256 + (dh + 1) * 128], mP[:, mt, :NQ])
                                                  for mt in range(2)], [B_mv, B_mP])
                        kb.op(dve, lambda e, b=b, hd=hd, dh=dh: e.tensor_tensor(out=moT[:, hd * 2 + dh, :NQ], in0=ps[b][:, :NQ],
                                                                               in1=mr[:, :NQ], op=ALU.mult),
                              [psb[b], B_mr], [B_mo])
            steps.append(([], s_mem))
            branch_out(2, "g2", lambda cb: s_wmo[:, cb * 512:(cb + 1) * 512], 8, B_wmo, "moT")
            steps.append(([], close_scope))

            def s_att_open(_, __):
                open_scope()
                sc["qT"] = kb.sb("qT", [128, 16, GT], BF16)
                sc["onT"] = kb.sb("onT", [128, 16, GT], BF16)
                sc["kt"] = [kb.sb("akt%d" % i, [128, 2048], BF16) for i in range(2)]
                sc["vv"] = [kb.sb("avv%d" % i, [128, 16, 128], BF16) for i in range(2)]
                sc["P"] = [kb.sb("aP%d" % i, [128, GT], BF16) for i in range(4)]
                sc["fin"] = kb.sb("afin", [128, 4, GT], F32)
                sc["osq"] = kb.sb("aosq", [128, GT], BF16)
            steps.append(([], s_att_open))
            for cb in range(4):
                def s_q(sl, Bsl, cb=cb):
                    qT, B_q = sc["qT"]
                    w = sl[:, 0:KC * 512].rearrange("p (k c) -> p k c", c=512)
                    for j in range(4):
                        b = next_ps()
                        mm_acc(b, ps[b][:, :NQ], [(w[:, k, j * 128:(j + 1) * 128], hT[:, k, :NQ]) for k in range(KC)],
                               [Bsl, B_hT])
                        evac(j, qT[:, cb * 4 + j, :NQ], ps[b][:, :NQ], [psb[b]], [B_q], scale=0.125)
                wstep(s_win[:, C_Q + cb * 512:C_Q + (cb + 1) * 512], KC, B_win["q"], s_q)

            segs = []
            if not is_s:
                for s_ in range(gi):
                    segs.append((s_ * 2048, [(128, "full", None)] * 16))
                dblk = [(128, "diag", kb_) for kb_ in range(4)] + [(128, "bias", 1 + (bk - 4) // 4) for bk in range(4, 16)]
                segs.append((gi * 2048, dblk))
            else:
                nb_tot = PAST // 128
                k0 = 0
                while nb_tot > 0:
                    n = min(16, nb_tot)
                    segs.append((k0, [(128, "full", None)] * n))
                    k0 += n * 128
                    nb_tot -= n
                if len(segs[-1][1]) < 16:
                    segs[-1][1].append((NSAMP, "full", None))
                else:
                    segs.append((k0, [(NSAMP, "full", None)]))

            def s_att(_, __):
                qT, B_q = sc["qT"]
                onT, B_on = sc["onT"]
                fin, B_fin = sc["fin"]
                osq, B_osq = sc["osq"]
                ktv = kt_scr
                vvv = v_scr
                li = [0]
                pi = [0]
                nblk_all = sum(len(s_[1]) for s_ in segs)
                bO = [4, 5, 6, 7]
                r1, r2, t1, o = fin[:, 0, :NQ], fin[:, 1, :NQ], fin[:, 2, :NQ], fin[:, 3, :NQ]

                items = []
                for h in range(16):
                    bi = 0
                    for (k0, blks) in segs:
                        for bl, (kn, kind, arg) in enumerate(blks):
                            items.append(dict(h=h, k0=k0, blks=blks, bl=bl, kn=kn, kind=kind, arg=arg,
                                              first=(bi == 0), last=(bi == nblk_all - 1), bi=bi))
                            bi += 1

                def emit_S(it):
                    h = it["h"]
                    if it["bl"] == 0:
                        ktt, B_ktt = sc["kt"][li[0] % 2]
                        vvt, B_vvt = sc["vv"][li[0] % 2]
                        li[0] += 1
                        blks, k0 = it["blks"], it["k0"]
                        nk = sum(b_[0] for b_ in blks)
                        nfull = sum(1 for b_ in blks if b_[0] == 128)
                        kb.dma(sp, ktt[:, :nk], ktv[h, :, k0:k0 + nk], [B_kt], [B_ktt], B_ktt)
                        if nfull:
                            kb.dma(sp, vvt[:, :nfull, :], vvv[h, :, k0 // 128:k0 // 128 + nfull, :], [B_v], [B_vvt], B_vvt)
                        if nfull < len(blks):
                            kn_ = blks[-1][0]
                            kb.dma(sp, vvt[:kn_, nfull, :], vvv[h, :kn_, k0 // 128 + nfull, :], [B_v], [B_vvt], B_vvt)
                        cur["kt"] = (ktt, B_ktt)
                        cur["vv"] = (vvt, B_vvt)
                    ktt, B_ktt = cur["kt"]
                    it["vv"] = cur["vv"]
                    kn, kind, arg, bl = it["kn"], it["kind"], it["arg"], it["bl"]
                    c0 = 128 * arg if kind == "diag" else 0
                    it["c0"] = c0
                    it["P"] = []
                    for mp in range(2):
                        bS = next_ps("s")
                        p0 = mp * 64
                        kb.op(pe, lambda e, bS=bS, p0=p0: e.matmul(
                            ps[bS][:kn, c0:NQ], ktt[p0:p0 + 64, bl * 128:bl * 128 + kn], qT[p0:p0 + 64, h, c0:NQ],
                            start=True, stop=True), [B_ktt, B_q], [psb[bS]])
                        Pt, B_P = sc["P"][pi[0] % 4]
                        pi[0] += 1
                        it["P"].append((Pt, B_P))
                        if kind == "bias":
                            kb.op(act, lambda e, bS=bS, Pt=Pt: e.activation(
                                out=Pt[:kn, c0:NQ], in_=ps[bS][:kn, c0:NQ], func=ACT.Exp,
                                bias=cols[:kn, 8 + arg:9 + arg]), [psb[bS], B_cols], [B_P])
                        else:
                            kb.op(act, lambda e, bS=bS, Pt=Pt: e.activation(
                                out=Pt[:kn, c0:NQ], in_=ps[bS][:kn, c0:NQ], func=ACT.Exp), [psb[bS]], [B_P])
                        if kind == "diag":
                            kb.op(pool, lambda e, Pt=Pt: e.memset(Pt[64:128, c0:c0 + 64], 0.0), [], [B_P])

                def emit_PV(it):
                    kn, bl, c0, first, last = it["kn"], it["bl"], it["c0"], it["first"], it["last"]
                    vvt, B_vvt = it["vv"]
                    for mp in range(2):
                        Pt, B_P = it["P"][mp]
                        kb.op(pe, lambda e, mp=mp, Pt=Pt: e.matmul(
                            ps[bO[2 * mp]][:, c0:NQ], vvt[:kn, bl, :], Pt[:kn, c0:NQ], start=first, stop=last),
                            [B_vvt, B_P], [psb[bO[2 * mp]]])
                        kb.op(pe, lambda e, mp=mp, Pt=Pt: e.matmul(
                            ps[bO[2 * mp + 1]][:, c0:NQ], ones[:kn, :], Pt[:kn, c0:NQ], start=first, stop=last),
                            [B_c, B_P], [psb[bO[2 * mp + 1]]])

                def fin1():
                    kb.op(dve, lambda e: e.reciprocal(out=r1, in_=ps[5][:, :NQ]), [psb[5]], [B_fin])
                    kb.op(dve, lambda e: e.reciprocal(out=r2, in_=ps[7][:, :NQ]), [psb[7]], [B_fin])
                    kb.op(dve, lambda e: e.tensor_tensor(out=t1, in0=ps[4][:, :NQ], in1=r1, op=ALU.mult), [psb[4], B_fin], [B_fin])
                    kb.op(dve, lambda e: e.tensor_tensor(out=r2, in0=ps[6][:, :NQ], in1=r2, op=ALU.mult), [psb[6], B_fin], [B_fin])
                    kb.op(dve, lambda e: e.scalar_tensor_tensor(out=o, in0=r2, scalar=cols[:, 1:2], in1=t1, op0=ALU.mult,
                                                                op1=ALU.add), [B_fin, B_cols], [B_fin])
                    kb.op(act, lambda e: e.activation(out=osq[:, :NQ], in_=o, func=ACT.Square), [B_fin], [B_osq])

                def fin2(h):
                    bs_ = next_ps("s")
                    kb.op(pe, lambda e, bs_=bs_: e.matmul(ps[bs_][:, :NQ], ones[:, :], osq[:, :NQ], start=True, stop=True),
                          [B_osq, B_c], [psb[bs_]])
                    kb.op(act, lambda e, bs_=bs_: e.activation(out=r1, in_=ps[bs_][:, :NQ], func=ACT.Sqrt, scale=1.0 / 128,
                                                               bias=1e-5), [psb[bs_]], [B_fin])
                    kb.op(dve, lambda e: e.reciprocal(out=r1, in_=r1), [B_fin], [B_fin])
                    kb.op(dve, lambda e, h=h: e.scalar_tensor_tensor(out=onT[:, h, :NQ], in0=o, scalar=cols[:, 2:3], in1=r1,
                                                                     op0=ALU.mult, op1=ALU.mult), [B_fin, B_cols], [B_on])

                cur = {}
                n_it = len(items)
                pend_fin2 = None
                emit_S(items[0])
                for i in range(n_it):
                    it = items[i]
                    if i + 1 < n_it:
                        emit_S(items[i + 1])
                    emit_PV(it)
                    if pend_fin2 is not None and (it["bi"] >= min(1, nblk_all - 1)):
                        fin2(pend_fin2)
                        pend_fin2 = None
                    if it["last"]:
                        fin1()
                        pend_fin2 = it["h"]
                if pend_fin2 is not None:
                    fin2(pend_fin2)
            steps.append(([], s_att))
            branch_out(1, "g1", lambda cb: s_wao[:, cb * 512:(cb + 1) * 512], KC, B_wao, "onT")
            steps.append(([], close_scope))

            sc["merged"] = (merged, B_mg)
            for cb in range(4):
                def s_wout(sl, Bsl, cb=cb):
                    w = sl[:, 0:KC * 512].rearrange("p (k c) -> p k c", c=512)
                    for t, nt in tl:
                        b = next_ps()
                        mm_acc(b, ps[b][:nt, :], [(merged[:, k, t * 128:t * 128 + nt], w[:, k, :]) for k in range(KC)],
                               [Bsl, B_mg])
                        kb.op(dve, lambda e, b=b, t=t, nt=nt: e.tensor_tensor(
                            out=xg[:nt, t, cb * 512:(cb + 1) * 512], in0=xg[:nt, t, cb * 512:(cb + 1) * 512],
                            in1=ps[b][:nt, :], op=ALU.add), [psb[b], B_xg], [B_xg])
                wstep(s_wo[:, cb * 512:(cb + 1) * 512], KC, B_wo, s_wout)

            def s_moe_open(_, __):
                open_scope()
                sc["acc"] = kb.sb("macc", [128, 4, D], F32)
                sc["aT"] = [kb.sb("maT%d" % i, [128, 2, GT], BF16) for i in range(2)]
                sc["sg"] = [kb.sb("msg%d" % i, [128, GT], F32) for i in range(2)]
                sc["comb"] = kb.sb("mcomb", [128, 4, 32], F32)
                sc["mtmp"] = [kb.sb("mtmp%d" % i, [128, 512], F32) for i in range(3)]
                sc["rt"] = kb.sb("mrt", [128, 96], F32)
                sc["gfin"] = kb.sb("gfin", [128, D], F32)
                acc, B_acc = sc["acc"]
                sc["accp"] = Buf("accp")
                comb, B_comb = sc["comb"]
                rt, B_rt = sc["rt"]
                gfin, B_gfin = sc["gfin"]
                kb.dma(sp, gfin[:, :], norm_final.partition_broadcast(128), [], [B_gfin], B_gfin)
                kb.op(pool, lambda e: e.memset(acc[:, :, :], 0.0), [], [B_acc, sc["accp"]])
                for t, nt in tl:
                    rmsnorm_T(xg[:nt, t, :], B_xg, nt, G_FFN, hT, B_hT, t * 128, xn, B_xn, sq, B_sq, 16)
                for t, nt in tl:
                    b = next_ps()
                    mm_acc(b, ps[b][:nt, :36], [(hT[:, k, t * 128:t * 128 + nt], wr_sb[:, k, :]) for k in range(KC)],
                           [B_hT, B_wrs])
                    lg = rt[:nt, 0:36]
                    gmax, ngmax, gsum, emax, nemax, m2, den = (rt[:nt, 36 + i:37 + i] for i in range(7))
                    gmask = rt[:nt, 44:48]
                    pen = rt[:nt, 48:52]
                    ge = rt[:nt, 52:56]
                    ee = rt[:nt, 56:88]
                    kb.op(dve, lambda e, b=b, nt=nt, lg=lg: e.tensor_tensor(out=lg, in0=ps[b][:nt, :36], in1=rbias[:nt, :],
                                                                         op=ALU.add), [psb[b], B_rb], [B_rt])
                    kb.op(dve, lambda e, lg=lg, gmax=gmax: e.reduce_max(out=gmax, in_=lg[:, 0:4], axis=AX.X), [B_rt], [B_rt])
                    kb.op(dve, lambda e, gmax=gmax, ngmax=ngmax: e.tensor_scalar(out=ngmax, in0=gmax, scalar1=-1.0, scalar2=None,
                                                                                 op0=ALU.mult), [B_rt], [B_rt])
                    kb.op(dve, lambda e, lg=lg, gmax=gmax, gmask=gmask: e.tensor_scalar(out=gmask, in0=lg[:, 0:4], scalar1=gmax,
                                                                                      scalar2=None, op0=ALU.is_ge),
                          [B_rt], [B_rt])
                    kb.op(dve, lambda e, gsum=gsum: e.memset(gsum, 0.0), [], [B_rt])
                    kb.op(act, lambda e, lg=lg, ge=ge, ngmax=ngmax, gsum=gsum: e.activation(
                        out=ge, in_=lg[:, 0:4], func=ACT.Exp, bias=ngmax, accum_out=gsum), [B_rt], [B_rt])
                    kb.op(dve, lambda e, gmask=gmask, pen=pen: e.tensor_scalar(out=pen, in0=gmask, scalar1=-1.0, scalar2=1e30,
                                                                              op0=ALU.add, op1=ALU.mult), [B_rt], [B_rt])
                    for g in range(4):
                        kb.op(dve, lambda e, g=g, lg=lg, pen=pen, ee=ee: e.tensor_scalar(
                            out=ee[:, g * 8:(g + 1) * 8], in0=lg[:, 4 + g * 8:12 + g * 8], scalar1=pen[:, g:g + 1],
                            scalar2=None, op0=ALU.add), [B_rt], [B_rt])
                    kb.op(dve, lambda e, ee=ee, emax=emax: e.reduce_max(out=emax, in_=ee, axis=AX.X), [B_rt], [B_rt])
                    kb.op(dve, lambda e, emax=emax, nemax=nemax: e.tensor_scalar(out=nemax, in0=emax, scalar1=-1.0, scalar2=None,
                                                                                 op0=ALU.mult), [B_rt], [B_rt])
                    kb.op(act, lambda e, ee=ee, nemax=nemax: e.activation(out=ee, in_=ee, func=ACT.Exp, bias=nemax),
                          [B_rt], [B_rt])
                    e2 = rt[:nt, 0:32]
                    kb.op(dve, lambda e, ee=ee, e2=e2: e.scalar_tensor_tensor(out=e2, in0=ee, scalar=1.0, in1=ee, op0=ALU.is_lt,
                                                                             op1=ALU.mult), [B_rt], [B_rt])
                    kb.op(dve, lambda e, e2=e2, m2=m2: e.reduce_max(out=m2, in_=e2, axis=AX.X), [B_rt], [B_rt])
                    kb.op(dve, lambda e, m2=m2, den=den, gsum=gsum: e.scalar_tensor_tensor(
                        out=den, in0=m2, scalar=1.0, in1=gsum, op0=ALU.add, op1=ALU.mult), [B_rt], [B_rt])
                    kb.op(dve, lambda e, den=den: e.reciprocal(out=den, in_=den), [B_rt], [B_rt])
                    kb.op(dve, lambda e, ee=ee, e2=e2, m2=m2: e.scalar_tensor_tensor(out=e2, in0=ee, scalar=m2, in1=ee,
                                                                                   op0=ALU.is_ge, op1=ALU.mult), [B_rt], [B_rt])
                    kb.op(dve, lambda e, e2=e2, den=den, t=t, nt=nt: e.tensor_scalar(out=comb[:nt, t, :], in0=e2, scalar1=den,
                                                                                    scalar2=None, op0=ALU.mult),
                          [B_rt], [B_comb])
            steps.append(([], s_moe_open))

            mtc = [0]
            for ex in range(NEXP):
                def s_gu(sl, Bsl, ex=ex):
                    aT, B_aT = sc["aT"][ex % 2]
                    wg = sl[:, 0:KC * 256].rearrange("p (k c) -> p k c", c=256)
                    wu = sl[:, KC * 256:2 * KC * 256].rearrange("p (k c) -> p k c", c=256)
                    for hc in range(2):
                        sg, B_sg = sc["sg"][hc]
                        bg = next_ps()
                        mm_acc(bg, ps[bg][:, :NQ], [(wg[:, k, hc * 128:(hc + 1) * 128], hT[:, k, :NQ]) for k in range(KC)],
                               [Bsl, B_hT])
                        bu = next_ps()
                        mm_acc(bu, ps[bu][:, :NQ], [(wu[:, k, hc * 128:(hc + 1) * 128], hT[:, k, :NQ]) for k in range(KC)],
                               [Bsl, B_hT])
                        kb.op(act, lambda e, bg=bg, sg=sg: e.activation(out=sg[:, :NQ], in_=ps[bg][:, :NQ], func=ACT.Silu),
                              [psb[bg]], [B_sg])
                        kb.op(dve, lambda e, bu=bu, sg=sg, hc=hc, aT=aT: e.tensor_tensor(out=aT[:, hc, :NQ], in0=ps[bu][:, :NQ],
                                                                                        in1=sg[:, :NQ], op=ALU.mult),
                              [psb[bu], B_sg], [B_aT])
                steps.append(([(0, s_eg[ex], KC, 256, B_exp[ex // 4]), (KC * 256, s_eu[ex], KC, 256, B_exp[ex // 4])], s_gu))

                def s_dn(sl, Bsl, ex=ex):
                    aT, B_aT = sc["aT"][ex % 2]
                    acc, B_acc = sc["acc"]
                    B_accp = sc["accp"]
                    comb, B_comb = sc["comb"]
                    wd = sl[:, 0:2 * D].rearrange("p (k c) -> p k c", c=D)
                    for t, nt in tl:
                        for cb in range(4):
                            b = next_ps()
                            mm_acc(b, ps[b][:nt, :], [(aT[:, hc, t * 128:t * 128 + nt], wd[:, hc, cb * 512:(cb + 1) * 512])
                                                      for hc in range(2)], [Bsl, B_aT])
                            if MOE_SPLIT and (t * 4 + cb) % 3 == 2:
                                mt_, B_mt = sc["mtmp"][mtc[0] % 3]
                                mtc[0] += 1
                                kb.op(act, lambda e, b=b, t=t, nt=nt, mt_=mt_: e.activation(
                                    out=mt_[:nt, :], in_=ps[b][:nt, :], func=ACT.Copy, scale=comb[:nt, t, ex:ex + 1]),
                                    [psb[b], B_comb], [B_mt])
                                kb.op(pool, lambda e, t=t, nt=nt, cb=cb, mt_=mt_: e.tensor_tensor(
                                    out=acc[:nt, t, cb * 512:(cb + 1) * 512], in0=acc[:nt, t, cb * 512:(cb + 1) * 512],
                                    in1=mt_[:nt, :], op=ALU.add), [B_mt, B_accp], [B_accp])
                            else:
                                kb.op(dve, lambda e, b=b, t=t, nt=nt, cb=cb: e.scalar_tensor_tensor(
                                    out=acc[:nt, t, cb * 512:(cb + 1) * 512], in0=ps[b][:nt, :], scalar=comb[:nt, t, ex:ex + 1],
                                    in1=acc[:nt, t, cb * 512:(cb + 1) * 512], op0=ALU.mult, op1=ALU.add),
                                    [psb[b], B_comb, B_acc], [B_acc])
                steps.append(([(0, s_ed[ex], 2, D, B_exp[ex // 4])], s_dn))

            def s_final(_, __):
                acc, B_acc = sc["acc"]
                B_accp = sc["accp"]
                gfin, B_gfin = sc["gfin"]
                for t, nt in tl:
                    c_ss = cols[:nt, 20:21]
                    c_r = cols[:nt, 21:22]
                    kb.op(dve, lambda e, t=t, nt=nt: e.tensor_tensor(out=acc[:nt, t, :], in0=acc[:nt, t, :], in1=xg[:nt, t, :],
                                                                    op=ALU.add), [B_xg, B_acc, B_accp], [B_acc, B_accp])
                    kb.op(dve, lambda e, c_ss=c_ss: e.memset(c_ss, 0.0), [], [B_cols])
                    kb.op(act, lambda e, t=t, nt=nt, c_ss=c_ss: e.activation(out=sq[:nt, :], in_=acc[:nt, t, :], func=ACT.Square,
                                                                            accum_out=c_ss), [B_acc, B_cols], [B_sq, B_cols])
                    kb.op(act, lambda e, c_ss=c_ss, c_r=c_r: e.activation(out=c_r, in_=c_ss, func=ACT.Sqrt, scale=1.0 / D,
                                                                          bias=EPS), [B_cols], [B_cols])
                    kb.op(dve, lambda e, c_r=c_r: e.reciprocal(out=c_r, in_=c_r), [B_cols], [B_cols])
                    kb.op(dve, lambda e, t=t, nt=nt, c_r=c_r: e.scalar_tensor_tensor(
                        out=acc[:nt, t, :], in0=acc[:nt, t, :], scalar=c_r, in1=gfin[:nt, :], op0=ALU.mult, op1=ALU.mult),
                        [B_acc, B_cols, B_gfin], [B_acc])
                    kb.dma(pool, y_o[row0 + t * 128:row0 + t * 128 + nt, :], acc[:nt, t, :], [B_acc], [], B_acc)
            steps.append(([], s_final))
            steps.append(([], close_scope))

        for gi in range(NGRP_):
            blocks = None
            group(gi, GT, xfull[gi * 4 * GT:(gi * 4 + 1) * GT, :], gi * GT, "x", mkT_p, B_mkp, mv_p, B_mvp,
                  s_ktp, B_ktp, s_vp, B_vp, blocks, convp_o if gi == NGRP_ - 1 else None)
        def s_mem_sample(_, __):
            cmb = xn[:, :].rearrange("p (t c) -> p t c", c=1024)
            for t in range(2):
                kb.dma(pool, cmb[:, t, :], cmk[t * 128:(t + 1) * 128, :], [], [B_xn], B_xn)
                kb.dma(pool, mv_p[:, t, :], cmv[t * 128:(t + 1) * 128, :], [], [B_mvp], B_mvp)
            for t in range(2):
                b = next_ps()
                pv = ps[b][:].bitcast(BF16).rearrange("p (j c) -> p j c", c=128)
                for cc in range(8):
                    kb.op(pe, lambda e, cc=cc, pv=pv, t=t: e.transpose(pv[:, cc, :], cmb[:, t, cc * 128:(cc + 1) * 128],
                                                                      ident[:, :]), [B_xn, B_c], [psb[b]])
                evac(t, mkT_p[:, :, t * 128:(t + 1) * 128], pv[:, :, :], [psb[b]], [B_mkp])
        steps.append(([], s_mem_sample))
        group(NGRP_, NSAMP, xsamp, NGRP_ * GT, "cache", mkT_p, B_mkp, mv_p, B_mvp, s_kts, B_kts, s_vs, B_vs, None, convs_o)
        run_steps()

        kb.barrier()
    return nc


_NC_CACHE = {}


def kernel(**inp):
    inp = {k: np.asarray(v) for k, v in inp.items()}
    if "nc" not in _NC_CACHE:
        _NC_CACHE["nc"] = build_program()
    nc = _NC_CACHE["nc"]
    xp = inp["x_prompt"]
    in_maps = []
    wnames = ["norm_mix", "w_in", "w_dw", "b_dw", "conv_ln_g", "conv_ln_b", "w_conv_out", "lambda_q1", "lambda_k1",
              "lambda_q2", "lambda_k2", "subln_g", "w_attn_out", "norm_mem", "w_mem_k", "w_mem_v", "w_mem_out",
              "w_out", "norm_ffn", "w_router_grp", "b_router_grp", "w_router_exp", "b_router_exp", "w_exp_gate",
              "w_exp_up", "w_exp_down"]
    wts = {n: np.ascontiguousarray(inp[n][0]) for n in wnames}
    wts["norm_final"] = np.ascontiguousarray(inp["norm_final"])
    for c in range(8):
        b, j = c // 4, c % 4
        xb = xp[b].reshape(16, GT, D)
        order = []
        for i in range(4):
            order.append(4 * i + j)
            order += [4 * i + m for m in range(4) if m != j]
        xfull = np.ascontiguousarray(xb[order].reshape(SEQ, D))
        xhalo = np.zeros((NGRP, 32, D), np.float32)
        for i in range(4):
            g = 4 * i + j
            if g > 0:
                xhalo[i] = xp[b, g * GT - 32:g * GT]
        gp = np.array([j] + [m for m in range(4) if m != j], np.float32)
        m = dict(wts)
        m.update({
            "xfull": xfull, "xhalo": xhalo, "gpos": np.ascontiguousarray(np.broadcast_to(gp, (128, 4))),
            "xsamp": np.ascontiguousarray(inp["x_sample"][c]),
            "cconv": np.ascontiguousarray(inp["cache_conv"][0, c]),
            "ck": np.ascontiguousarray(inp["cache_diff_k"][0, c].reshape(PAST, D)),
            "cv": np.ascontiguousarray(inp["cache_diff_v"][0, c].reshape(PAST, D)),
            "cmk": np.ascontiguousarray(inp["cache_mem_k"][0, c].reshape(256, 1024)),
            "cmv": np.ascontiguousarray(inp["cache_mem_v"][0, c].reshape(256, 1024)),
            "memx": np.ascontiguousarray(inp["mem_prompt"][b]),
        })
        in_maps.append(m)
    res = run_bass_kernel_spmd(nc, in_maps, core_ids=list(range(8)))
    R = res.results
    y_p = np.zeros((2, SEQ, D), np.float32)
    k_p = np.zeros((1, 2, SEQ, 16, 128), np.float32)
    v_p = np.zeros((1, 2, SEQ, 16, 128), np.float32)
    y_s = np.zeros((8, NSAMP, D), np.float32)
    k_s = np.zeros((1, 8, NSAMP, 16, 128), np.float32)
    v_s = np.zeros((1, 8, NSAMP, 16, 128), np.float32)
    conv_p = np.zeros((1, 2, 30, DCONV), np.float32)
    conv_s = np.zeros((1, 8, 30, DCONV), np.float32)
    mk_p = np.zeros((1, 2, 256, 4, 256), np.float32)
    mv_p = np.zeros((1, 2, 256, 4, 256), np.float32)
    for c in range(8):
        b, j = c // 4, c % 4
        r = R[c]
        for i in range(4):
            g = 4 * i + j
            y_p[b, g * GT:(g + 1) * GT] = r["y"][i * GT:(i + 1) * GT]
            k_p[0, b, g * GT:(g + 1) * GT] = r["kout"][i * GT:(i + 1) * GT].reshape(GT, 16, 128)
            v_p[0, b, g * GT:(g + 1) * GT] = r["vout"][i * GT:(i + 1) * GT].reshape(GT, 16, 128)
        y_s[c] = r["y"][NGRP * GT:]
        k_s[0, c] = r["kout"][NGRP * GT:].reshape(NSAMP, 16, 128)
        v_s[0, c] = r["vout"][NGRP * GT:].reshape(NSAMP, 16, 128)
        conv_s[0, c] = r["convs"]
        if j == 3:
            conv_p[0, b] = r["convp"]
        if j == 0:
            mk_p[0, b] = r["memk"].reshape(256, 4, 256)
            mv_p[0, b] = r["memv"].reshape(256, 4, 256)
    return (y_p, y_s, conv_p, k_p, v_p, mk_p, mv_p, conv_s, k_s, v_s)
```

```python
import math
from contextlib import ExitStack

import numpy as np
import concourse.bass as bass
import concourse.mybir as mybir
from concourse.bass_utils import run_bass_kernel_spmd

F32 = mybir.dt.float32
BF16 = mybir.dt.bfloat16
ACT = mybir.ActivationFunctionType
ALU = mybir.AluOpType
AX = mybir.AxisListType

D = 2048
KC = 16
SEQ = 8192
NGRP = 4
DBG_STOP = None
GT = 512
DCONV = 1024
NSAMP = 64
PAST = 4096
NIN = 15360
NEXP = 32
DEXP = 256
EPS = 1e-6
NEG = -30000.0
C_AGT, C_Q, C_K, C_V, C_MQ, C_G = 0, 2048, 4096, 6144, 8192, 9216
STAGE = 2
MOE_SPLIT = True


class Buf:
    def __init__(self, name):
        self.name = name
        self.w = {}
        self.r = {}
        self.dsem = None
        self.dcount = 0


class Eng:
    def __init__(self, kb, e, name, is_pe=False):
        self.e = e
        self.name = name
        self.sem = kb.newsem("e_" + name)
        self.n = 0
        self.seen = {}
        self.is_pe = is_pe

    def wait(self, sem, val):
        if val <= 0:
            return
        if self.is_pe and sem is self.sem:
            return
        if self.seen.get(sem, 0) >= val:
            return
        self.e.wait_ge(sem, val)
        self.seen[sem] = val


class KB:
    def __init__(self, nc, es):
        self.nc = nc
        self.es = es
        self.sem_es = es
        self.nsem = 0
        self.pe = Eng(self, nc.tensor, "pe", True)
        self.act = Eng(self, nc.scalar, "act")
        self.dve = Eng(self, nc.vector, "dve")
        self.pool = Eng(self, nc.gpsimd, "pool")
        self.sp = Eng(self, nc.sync, "sp")
        self.engs = [self.pe, self.act, self.dve, self.pool, self.sp]
        self.dma_bufs = []

    def newsem(self, name):
        self.nsem += 1
        return self.sem_es.enter_context(self.nc.semaphore(name + "_%d" % self.nsem))

    def sb(self, name, shape, dt):
        self.nsb = getattr(self, "nsb", 0) + 1
        name = "%s_%d" % (name, self.nsb)
        t = self.es.enter_context(self.nc.sbuf_tensor(name, shape, dt))
        return t, Buf(name)

    def _deps(self, eng, reads, writes):
        for b in reads:
            for s, v in b.w.items():
                eng.wait(s, v)
        for b in writes:
            for s, v in b.w.items():
                eng.wait(s, v)
            for s, v in b.r.items():
                eng.wait(s, v)

    def _mark(self, sem, val, reads, writes):
        for b in reads:
            if b.r.get(sem, 0) < val:
                b.r[sem] = val
        for b in writes:
            if b.w.get(sem, 0) < val:
                b.w[sem] = val

    def op(self, eng, fn, reads=(), writes=()):
        self._deps(eng, reads, writes)
        ins = fn(eng.e)
        eng.n += 1
        ins.then_inc(eng.sem, 1)
        self._mark(eng.sem, eng.n, reads, writes)

    def dma(self, q, out, in_, reads, writes, owner, throttle=None):
        self._deps(q, reads, writes)
        if throttle is not None:
            hist, depth = throttle
            if len(hist) >= depth:
                ps_, pv_ = hist[len(hist) - depth]
                q.wait(ps_, pv_)
        if owner.dsem is None:
            owner.dsem = self.newsem("d_" + owner.name)
            self.dma_bufs.append(owner)
        ins = q.e.dma_start(out=out, in_=in_)
        owner.dcount += 16
        ins.then_inc(owner.dsem, 16)
        if throttle is not None:
            throttle[0].append((owner.dsem, owner.dcount))
        self._mark(owner.dsem, owner.dcount, reads, writes)

    def sync_bufs(self, bufs):
        for e in self.engs:
            for b in bufs:
                for sm, v in list(b.w.items()) + list(b.r.items()):
                    e.wait(sm, v)

    def barrier(self):
        for e in self.engs:
            for f in self.engs:
                if f is not e:
                    e.wait(f.sem, f.n)
            for b in self.dma_bufs:
                e.wait(b.dsem, b.dcount)


def build_program():
    nc = bass.Bass("TRN2", target_bir_lowering=False)

    def din(name, shape):
        return nc.dram_tensor(name, list(shape), F32, kind="ExternalInput").ap()

    def dout(name, shape):
        return nc.dram_tensor(name, list(shape), F32, kind="ExternalOutput").ap()

    def dscr(name, shape, dt=BF16):
        return nc.dram_tensor(name, list(shape), dt, kind="Internal").ap()

    xfull = din("xfull", [SEQ, D])
    xhalo = din("xhalo", [NGRP, 32, D])
    gpos = din("gpos", [128, 4])
    xsamp = din("xsamp", [NSAMP, D])
    cconv = din("cconv", [30, DCONV])
    ck = din("ck", [PAST, D])
    cv = din("cv", [PAST, D])
    cmk = din("cmk", [256, 1024])
    cmv = din("cmv", [256, 1024])
    memx = din("memx", [256, D])
    norm_mix = din("norm_mix", [D])
    w_in = din("w_in", [D, NIN])
    w_dw = din("w_dw", [31, DCONV])
    b_dw = din("b_dw", [DCONV])
    ln_g = din("conv_ln_g", [DCONV])
    ln_b = din("conv_ln_b", [DCONV])
    w_co = din("w_conv_out", [DCONV, D])
    lq1 = din("lambda_q1", [64])
    lk1 = din("lambda_k1", [64])
    lq2 = din("lambda_q2", [64])
    lk2 = din("lambda_k2", [64])
    subg = din("subln_g", [128])
    w_ao = din("w_attn_out", [D, D])
    norm_mem = din("norm_mem", [D])
    w_mk = din("w_mem_k", [D, 1024])
    w_mv = din("w_mem_v", [D, 1024])
    w_mo = din("w_mem_out", [1024, D])
    w_o = din("w_out", [D, D])
    norm_ffn = din("norm_ffn", [D])
    w_rg = din("w_router_grp", [D, 4])
    b_rg = din("b_router_grp", [4])
    w_re = din("w_router_exp", [D, 32])
    b_re = din("b_router_exp", [32])
    w_eg = din("w_exp_gate", [NEXP, D, DEXP])
    w_eu = din("w_exp_up", [NEXP, D, DEXP])
    w_ed = din("w_exp_down", [NEXP, DEXP, D])
    norm_final = din("norm_final", [D])

    NTOK = (SEQ // 2048) * GT + NSAMP
    y_o = dout("y", [NTOK, D])
    k_o = dout("kout", [NTOK, D])
    v_o = dout("vout", [NTOK, D])
    convp_o = dout("convp", [30, DCONV])
    convs_o = dout("convs", [30, DCONV])
    memk_o = dout("memk", [256, 1024])
    memv_o = dout("memv", [256, 1024])

    s_win = dscr("s_win", [NIN // 512, 128, KC * 512])
    s_wco = dscr("s_wco", [4, 128, 8 * 512])
    s_wao = dscr("s_wao", [4, 128, KC * 512])
    s_wmk = dscr("s_wmk", [D, 1024])
    s_wmv = dscr("s_wmv", [D, 1024])
    s_wmo = dscr("s_wmo", [4, 128, 8 * 512])
    s_wo = dscr("s_wo", [4, 128, KC * 512])
    s_wr = dscr("s_wr", [D, 36])
    s_egu = dscr("s_egu", [NEXP, 128, 2 * KC * DEXP])
    s_ed = dscr("s_ed", [NEXP, 128, 2 * D])
    NBP = SEQ // 128
    NBS = PAST // 128 + 1
    NGRP_ = SEQ // 2048
    s_ktp = dscr("s_ktp", [16, 128, SEQ])
    s_vp = dscr("s_vp", [16, 128, NBP, 128])
    s_dg = dscr("s_dg", [8, 128, 31 * 128])
    s_kts = dscr("s_kts", [16, 128, NBS * 128])
    s_vs = dscr("s_vs", [16, 128, NBS, 128])

    with ExitStack() as es:
        es.enter_context(nc.allow_non_contiguous_dma(reason="small parameter loads"))
        kb = KB(nc, es)
        pe, act, dve, pool, sp = kb.pe, kb.act, kb.dve, kb.pool, kb.sp

        ps = []
        psb = []
        for i in range(8):
            t = es.enter_context(nc.psum_tensor("ps%d" % i, [128, 512], F32))
            ps.append(t)
            psb.append(Buf("ps%d" % i))
        rr = {"a": 0, "s": 0}

        def next_ps(pool_name="a"):
            if pool_name == "a":
                i = rr["a"] % 8
                rr["a"] += 1
            else:
                i = rr["s"] % 4
                rr["s"] += 1
            return i

        if DBG_STOP == "pm1":
            kb.dma(sp, y_o[0:128, :], xfull[0:128, :], [], [], Buf("t"))
            kb.barrier()
            return nc
        if DBG_STOP == "p0a":
            kb.barrier()
            return nc
        ident, B_c = kb.sb("ident", [128, 128], BF16)
        ones, _ = kb.sb("ones", [128, 128], BF16)
        kb.op(pool, lambda e: e.memset(ident[:], 0.0), [], [B_c])
        kb.op(pool, lambda e: e.affine_select(out=ident[:], in_=ident[:], pattern=[[-1, 128]],
                                              compare_op=ALU.not_equal, fill=1.0, base=0,
                                              channel_multiplier=1), [B_c], [B_c])
        kb.op(pool, lambda e: e.memset(ones[:], 1.0), [], [B_c])

        cols, B_cols = kb.sb("cols", [128, 64], F32)
        kb.op(dve, lambda e: e.memset(cols[:], 0.0), [], [B_cols])
        pfm, B_pfm = kb.sb("pfm", [128, 3, 16], F32)
        for i, src in enumerate((norm_mix, norm_mem, norm_ffn)):
            kb.dma(sp, pfm[:, i, :], src.rearrange("(k p) -> p k", p=128), [], [B_pfm], B_pfm)
        pcv, B_pcv = kb.sb("pcv", [128, 34 + 3, 8], F32)
        kb.dma(sp, pcv[:, 0:31, :], w_dw.rearrange("t (k p) -> p t k", p=128), [], [B_pcv], B_pcv)
        kb.dma(sp, pcv[:, 31, :], b_dw.rearrange("(k p) -> p k", p=128), [], [B_pcv], B_pcv)
        kb.dma(sp, pcv[:, 32, :], ln_g.rearrange("(k p) -> p k", p=128), [], [B_pcv], B_pcv)
        kb.dma(sp, pcv[:, 33, :], ln_b.rearrange("(k p) -> p k", p=128), [], [B_pcv], B_pcv)
        lamt, B_lam = kb.sb("lamt", [128, 4, 64], F32)
        for i, src in enumerate((lq1, lk1, lq2, lk2)):
            kb.dma(sp, lamt[:, i, :], src.partition_broadcast(128), [], [B_lam], B_lam)
        subgc, B_subg = kb.sb("subgc", [128, 1], F32)
        kb.dma(sp, subgc[:, :], subg.rearrange("(p o) -> p o", o=1), [], [B_subg], B_subg)
        gpt, B_gp = kb.sb("gpt", [128, 4], F32)
        kb.dma(sp, gpt[:, :], gpos[:, :], [], [B_gp], B_gp)
        rbias, B_rb = kb.sb("rbias", [128, 36], F32)
        kb.dma(sp, rbias[:, 0:4], b_rg.partition_broadcast(128), [], [B_rb], B_rb)
        kb.dma(sp, rbias[:, 4:36], b_re.partition_broadcast(128), [], [B_rb], B_rb)
        wr_f, B_wrf = kb.sb("wr_f", [128, KC, 36], F32)
        wr_sb, B_wrs = kb.sb("wr_sb", [128, KC, 36], BF16)
        kb.dma(sp, wr_f[:, :, 0:4], w_rg.rearrange("(k p) c -> p k c", p=128), [], [B_wrf], B_wrf)
        kb.dma(sp, wr_f[:, :, 4:36], w_re.rearrange("(k p) c -> p k c", p=128), [], [B_wrf], B_wrf)
        kb.op(dve, lambda e: e.tensor_copy(out=wr_sb[:, :, :], in_=wr_f[:, :, :]), [B_wrf], [B_wrs])

        gbc, B_gbc = kb.sb("gbc", [128, 2, KC, 128], BF16)

        def fill_gbc(dst, src_i):
            for k in range(KC):
                kb.op(dve, lambda e, k=k: e.tensor_copy(out=dst[:, k, :], in_=pfm[:, src_i, k:k + 1].to_broadcast([128, 128])),
                      [B_pfm], [B_gbc])
        fill_gbc(gbc[:, 0, :, :], 0)
        fill_gbc(gbc[:, 1, :, :], 2)
        G_MIX, G_FFN = gbc[:, 0, :, :], gbc[:, 1, :, :]
        lam_init = 0.8 - 0.6 * math.exp(-0.3 * 0)
        ltmp, B_lt = kb.sb("ltmp", [128, 2, 64], F32)
        kb.op(dve, lambda e: e.tensor_tensor(out=ltmp[:, 0, :], in0=lamt[:, 0, :], in1=lamt[:, 1, :], op=ALU.mult),
              [B_lam], [B_lt])
        kb.op(dve, lambda e: e.tensor_tensor(out=ltmp[:, 1, :], in0=lamt[:, 2, :], in1=lamt[:, 3, :], op=ALU.mult),
              [B_lam], [B_lt])
        kb.op(dve, lambda e: e.reduce_sum(out=cols[:, 4:6], in_=ltmp[:, :, :], axis=AX.X), [B_lt], [B_cols])
        kb.op(act, lambda e: e.activation(out=cols[:, 6:8], in_=cols[:, 4:6], func=ACT.Exp), [B_cols], [B_cols])
        kb.op(dve, lambda e: e.tensor_tensor(out=cols[:, 0:1], in0=cols[:, 6:7], in1=cols[:, 7:8], op=ALU.subtract),
              [B_cols], [B_cols])
        kb.op(dve, lambda e: e.tensor_scalar(out=cols[:, 0:1], in0=cols[:, 0:1], scalar1=lam_init, scalar2=None,
                                             op0=ALU.add), [B_cols], [B_cols])
        kb.op(dve, lambda e: e.tensor_scalar(out=cols[:, 1:2], in0=cols[:, 0:1], scalar1=-1.0, scalar2=None,
                                             op0=ALU.mult), [B_cols], [B_cols])
        kb.op(dve, lambda e: e.tensor_scalar(out=cols[:, 2:3], in0=subgc[:, 0:1], scalar1=(1.0 - lam_init),
                                             scalar2=None, op0=ALU.mult), [B_subg, B_cols], [B_cols])
        for m in range(1, 4):
            kb.op(dve, lambda e, m=m: e.tensor_scalar(out=cols[:, 8 + m:9 + m], in0=gpt[:, m:m + 1],
                                                      scalar1=gpt[:, 0:1], scalar2=NEG, op0=ALU.is_gt,
                                                      op1=ALU.mult), [B_gp, B_cols], [B_cols])

        B_win = {}
        cth = ([], 2)
        def blk_cast(dst2d, src_cols, nk, c, b):
            kb.dma(pool, dst2d.rearrange("p (k c) -> p k c", c=c), src_cols.rearrange("(k p) c -> p k c", p=128),
                   [], [b], b, cth)

        for nm, c0, c1 in (("kv", C_K, C_MQ), ("agt", C_AGT, C_Q), ("g0", C_G, C_G + D), ("mq", C_MQ, C_G),
                           ("g2", C_G + 2 * D, NIN), ("q", C_Q, C_K), ("g1", C_G + D, C_G + 2 * D)):
            b = Buf("cw_" + nm)
            B_win[nm] = b
            for cc0 in range(c0, c1, 512):
                blk_cast(s_win[cc0 // 512], w_in[:, cc0:cc0 + 512], KC, 512, b)

        def cast_simple(name, dst, src, rows):
            b = Buf("cw_" + name)
            step = 512
            for r in range(0, rows, step):
                kb.dma(pool, dst[r:r + step, :], src[r:r + step, :], [], [b], b, cth)
            return b

        def cast_blocks(name, dst, src, rows):
            b = Buf("cw_" + name)
            for cb in range(4):
                blk_cast(dst[cb], src[:, cb * 512:(cb + 1) * 512], rows // 128, 512, b)
            return b

        B_wmk = cast_simple("wmk", s_wmk, w_mk, D)
        B_wmv = cast_simple("wmv", s_wmv, w_mv, D)
        B_wco = cast_blocks("wco", s_wco, w_co, DCONV)
        B_wao = cast_blocks("wao", s_wao, w_ao, D)
        B_wmo = cast_blocks("wmo", s_wmo, w_mo, 1024)
        B_wo = cast_blocks("wo", s_wo, w_o, D)
        B_exp = []
        for g8 in range(NEXP // 4):
            b = Buf("cw_e%d" % g8)
            B_exp.append(b)
            for e in range(g8 * 4, g8 * 4 + 4):
                blk_cast(s_egu[e][:, 0:KC * DEXP], w_eg[e], KC, DEXP, b)
                blk_cast(s_egu[e][:, KC * DEXP:2 * KC * DEXP], w_eu[e], KC, DEXP, b)
                blk_cast(s_ed[e], w_ed[e], 2, D, b)

        if DBG_STOP == "p0":
            kb.barrier()
            return nc
        nsts = [kb.sb("nst%d" % i, [128, 2], F32) for i in range(2)]
        nsti = [0]

        def norm_prep(xt, Bx, nt, xn, Bxn):
            nst, B_nst = nsts[nsti[0] % 2]
            nsti[0] += 1
            c_ss = nst[:nt, 0:1]
            c_r = nst[:nt, 1:2]
            kb.op(dve, lambda e: e.memset(c_ss, 0.0), [], [B_nst])
            kb.op(act, lambda e: e.activation(out=xn[:nt, :], in_=xt, func=ACT.Square, accum_out=c_ss),
                  [Bx, B_nst], [Bxn, B_nst])
            kb.op(act, lambda e: e.activation(out=c_r, in_=c_ss, func=ACT.Sqrt, scale=1.0 / D, bias=EPS),
                  [B_nst], [B_nst])
            kb.op(dve, lambda e: e.reciprocal(out=c_r, in_=c_r), [B_nst], [B_nst])
            kb.op(act, lambda e: e.activation(out=xn[:nt, :], in_=xt, func=ACT.Copy, scale=c_r),
                  [Bx, B_nst], [Bxn])

        def norm_tr(nt, gi, hT, BhT, t0, xn, Bxn):
            for half in range(2):
                b = next_ps()
                pv = ps[b][:].bitcast(BF16).rearrange("p (j c) -> p j c", c=128)
                for j in range(8):
                    k = half * 8 + j
                    kb.op(pe, lambda e, j=j, k=k: e.transpose(pv[:, j, :nt], xn[:nt, k * 128:(k + 1) * 128],
                                                              ident[:nt, :nt]), [Bxn, B_c], [psb[b]])
                kb.op(dve, lambda e, half=half, pv=pv: e.tensor_tensor(
                    out=hT[:, half * 8:half * 8 + 8, t0:t0 + nt], in0=pv[:, :, :nt],
                    in1=gi[:, half * 8:half * 8 + 8, :nt], op=ALU.mult), [psb[b], B_gbc], [BhT])

        def rmsnorm_T(xt, Bx, nt, gi, hT, BhT, t0, xn, Bxn, sq, Bsq, col0):
            norm_prep(xt, Bx, nt, xn, Bxn)
            norm_tr(nt, gi, hT, BhT, t0, xn, Bxn)

        def evac(i, out_ap, in_ap, reads, writes, scale=None):
            if i % 2 == 0:
                if scale is None:
                    kb.op(act, lambda e: e.activation(out=out_ap, in_=in_ap, func=ACT.Copy), reads, writes)
                else:
                    kb.op(act, lambda e: e.activation(out=out_ap, in_=in_ap, func=ACT.Copy, scale=scale), reads, writes)
            else:
                if scale is None:
                    kb.op(dve, lambda e: e.tensor_copy(out=out_ap, in_=in_ap), reads, writes)
                else:
                    kb.op(dve, lambda e: e.tensor_scalar(out=out_ap, in0=in_ap, scalar1=scale, scalar2=None,
                                                         op0=ALU.mult), reads, writes)

        def mm_acc(b, out_ap, pairs, reads):
            n = len(pairs)
            for i, (l, r) in enumerate(pairs):
                kb.op(pe, lambda e, l=l, r=r, i=i: e.matmul(out_ap, l, r, start=(i == 0), stop=(i == n - 1)),
                      reads, [psb[b]])

        B_ktp, B_vp, B_kts, B_vs = Buf("ktp"), Buf("vp"), Buf("kts"), Buf("vs")
        dgB = Buf("s_dg")
        with ExitStack() as es0:
            kb_es = kb.es
            kb.es = es0
            B_dg = dgB
            dgs = [kb.sb("dgs%d" % i, [128, 31, 128], BF16) for i in range(2)]
            for cc in range(8):
                dg_, Bdg_ = dgs[cc % 2]
                for k in range(31):
                    kb.op(dve, lambda e, cc=cc, k=k, dg_=dg_: e.tensor_scalar(out=dg_[:, k, :], in0=ident[:, :],
                                                                            scalar1=pcv[:, k, cc:cc + 1], scalar2=None,
                                                                            op0=ALU.mult), [B_c, B_pcv], [Bdg_])
                kb.dma(sp, s_dg[cc], dg_[:, :, :].rearrange("p k c -> p (k c)"), [Bdg_], [B_dg], Bdg_)
            kb.sync_bufs([d_[1] for d_ in dgs])
            kb.es = kb_es
        with ExitStack() as es1:
            kb_es = kb.es
            kb.es = es1
            p1w = [kb.sb("p1w%d" % i, [128, KC, 512], BF16) for i in range(3)]
            xs = [kb.sb("p1x%d" % i, [128, D], F32) for i in range(2)]
            xn1s = [kb.sb("p1xn%d" % i, [128, D], BF16) for i in range(2)]
            xn1, B_xn1 = xn1s[0]
            sq1, B_sq1 = xn1, B_xn1
            CH = 1024
            hTs = [kb.sb("p1hT%d" % i, [128, KC, CH], BF16) for i in range(2)]
            kts = [kb.sb("p1kt%d" % i, [128, 4, GT], BF16) for i in range(2)]
            vss = [kb.sb("p1v%d" % i, [128, 4, 512], BF16) for i in range(2)]
            ckb = [kb.sb("p1ck%d" % i, [128, D], BF16) for i in range(2)]
            ckf = [kb.sb("p1ckf%d" % i, [128, D], F32) for i in range(2)]
            ktc = [kb.sb("p1ktc%d" % i, [128, 16, 256], BF16) for i in range(2)]
            xi = [0]
            ki = [0]
            wq = {"n": 0, "loaded": 0}
            NCHK = SEQ // CH
            NCH = NCHK + 1
            vp_v = s_vp.rearrange("h p nb d -> p h nb d")
            vs_v = s_vs.rearrange("h p nb d -> p h nb d")
            ktp_v = s_ktp.rearrange("h p n -> p h n")
            kts_v = s_kts.rearrange("h p n -> p h n")

            def p1_wload(upto):
                while wq["loaded"] < min(upto, NCH * 8):
                    i = wq["loaded"]
                    blk = i % 8
                    w, Bw = p1w[i % 3]
                    kb.dma(sp, w[:, :, :].rearrange("p k c -> p (k c)"), s_win[C_K // 512 + blk], [B_win["kv"]], [Bw], Bw)
                    wq["loaded"] += 1

            def kv_norm(ci, src_rows, ntok):
                hT, BhT = hTs[ci % 2]
                ntile = (ntok + 127) // 128
                for t in range(ntile):
                    nt = min(128, ntok - t * 128)
                    xt, Bx = xs[xi[0] % 2]
                    xi[0] += 1
                    kb.dma(sp, xt[:nt, :], src_rows[t * 128:t * 128 + nt, :], [], [Bx], Bx)
                    rmsnorm_T(xt[:nt, :], Bx, nt, G_MIX, hT, BhT, t * 128, xn1, B_xn1, sq1, B_sq1, 16)

            cache_nb = [0]
            cache_ld = [0]

            def cache_load():
                nb = cache_ld[0]
                if nb >= PAST // 128:
                    return
                cache_ld[0] += 1
                ct, Bct = ckb[nb % 2]
                cf, Bcf = ckf[nb % 2]
                kb.dma(sp, cf[:, :], ck[nb * 128:(nb + 1) * 128, :], [], [Bcf], Bcf)
                kb.op(act, lambda e, ct=ct, cf=cf: e.activation(out=ct[:, :], in_=cf[:, :], func=ACT.Copy), [Bcf], [Bct])

            def cache_blocks(n):
                for _ in range(n):
                    nb = cache_nb[0]
                    if nb >= PAST // 128:
                        return
                    cache_nb[0] += 1
                    while cache_ld[0] <= nb:
                        cache_load()
                    ct, Bct = ckb[nb % 2]
                    kt, Bk = ktc[(nb // 2) % 2]
                    for half in range(2):
                        b = next_ps()
                        pv = ps[b][:].bitcast(BF16).rearrange("p (j c) -> p j c", c=128)
                        for j in range(8):
                            h = half * 8 + j
                            kb.op(pe, lambda e, j=j, h=h, pv=pv, ct=ct: e.transpose(pv[:, j, :], ct[:, h * 128:(h + 1) * 128],
                                                                                  ident[:, :]), [Bct, B_c], [psb[b]])
                        evac(half, kt[:, half * 8:half * 8 + 8, (nb % 2) * 128:(nb % 2 + 1) * 128], pv[:, :, :],
                             [psb[b]], [Bk])
                    if nb % 2 == 1:
                        kb.dma(sp, kts_v[:, :, (nb - 1) * 128:(nb + 1) * 128], kt[:, :, :], [Bk], [B_kts], Bk)
                    cache_load()

            pend = []

            def norm_sched(ci_next, src_rows, ntok):
                hTn, BhTn = hTs[ci_next % 2]
                ntile = (ntok + 127) // 128
                jobs = []
                for t in range(ntile):
                    nt = min(128, ntok - t * 128)
                    jobs.append((t, nt))

                def prep(t, nt):
                    xt, Bx = xs[xi[0] % 2]
                    xnb, Bxnb = xn1s[xi[0] % 2]
                    xi[0] += 1
                    kb.dma(sp, xt[:nt, :], src_rows[t * 128:t * 128 + nt, :], [], [Bx], Bx)
                    norm_prep(xt[:nt, :], Bx, nt, xnb, Bxnb)
                    return (nt, hTn, BhTn, t * 128, xnb, Bxnb)
                return jobs, prep

            def kv_chunk(ci, ntok, kt_dst_fn, v_dst_fn, Bkt, Bv, nxt=None, ncache=0):
                hT, BhT = hTs[ci % 2]
                jobs, prep = nxt if nxt is not None else ([], None)
                per_blk = (len(jobs) + 7) // 8 if jobs else 0
                ji = 0
                for blk in range(8):
                    while pend:
                        a_ = pend.pop(0)
                        norm_tr(a_[0], G_MIX, a_[1], a_[2], a_[3], a_[4], a_[5])
                    for _ in range(per_blk):
                        if ji < len(jobs):
                            pend.append(prep(*jobs[ji]))
                            ji += 1
                    if ncache and blk % 2 == 1:
                        cache_blocks(ncache)
                    i = wq["n"]
                    wq["n"] += 1
                    p1_wload(i + 2)
                    w, Bw = p1w[i % 3]
                    for c0 in range(0, ntok, GT):
                        nq = min(GT, ntok - c0)
                        ntile = (nq + 127) // 128
                        if blk < 4:
                            kt, Bk = kts[ki[0] % 2]
                            ki[0] += 1
                            for hh in range(4):
                                b = next_ps()
                                mm_acc(b, ps[b][:, :nq], [(w[:, k, hh * 128:(hh + 1) * 128], hT[:, k, c0:c0 + nq])
                                                          for k in range(KC)], [Bw, BhT])
                                evac(hh, kt[:, hh, :nq], ps[b][:, :nq], [psb[b]], [Bk])
                            kb.dma(sp, kt_dst_fn(blk, c0, nq), kt[:, :, :nq], [Bk], [Bkt], Bk)
                        else:
                            cb = blk - 4
                            vs_, Bvs = vss[ki[0] % 2]
                            ki[0] += 1
                            for t in range(ntile):
                                nt = min(128, nq - t * 128)
                                b = next_ps()
                                mm_acc(b, ps[b][:nt, :], [(hT[:, k, c0 + t * 128:c0 + t * 128 + nt], w[:, k, :])
                                                          for k in range(KC)], [Bw, BhT])
                                evac(t, vs_[:nt, t, :], ps[b][:nt, :], [psb[b]], [Bvs])
                                kb.dma(sp, v_dst_fn(c0 // 128 + t, nt, cb), vs_[:nt, t, :].rearrange("p (h d) -> p h d", d=128),
                                       [Bvs], [Bv], Bvs)
                while pend:
                    a_ = pend.pop(0)
                    norm_tr(a_[0], G_MIX, a_[1], a_[2], a_[3], a_[4], a_[5])

            kv_norm(0, xfull[0:CH, :], CH)
            cache_load()
            for ci in range(NCHK):
                if ci + 1 < NCHK:
                    mid = norm_sched(ci + 1, xfull[(ci + 1) * CH:(ci + 2) * CH, :], CH)
                else:
                    mid = norm_sched(NCHK, xsamp, NSAMP)
                kv_chunk(ci, CH,
                         lambda blk, c0, nq, ci=ci: ktp_v[:, blk * 4:blk * 4 + 4, ci * CH + c0:ci * CH + c0 + nq],
                         lambda tt, nt, cb, ci=ci: vp_v[:nt, cb * 4:cb * 4 + 4, ci * (CH // 128) + tt, :], B_ktp, B_vp, mid,
                         ncache=1)
            kv_chunk(NCHK, NSAMP, lambda blk, c0, nq: kts_v[:, blk * 4:blk * 4 + 4, PAST:PAST + NSAMP],
                     lambda tt, nt, cb: vs_v[:nt, cb * 4:cb * 4 + 4, NBS - 1, :], B_kts, B_vs)
            for nb0 in range(PAST // 128):
                kb.dma(pool, vs_v[:, :, nb0, :],
                       cv[nb0 * 128:(nb0 + 1) * 128, :].rearrange("p (h d) -> p h d", d=128),
                       [], [B_vs], B_vs)
            cache_blocks(PAST // 128)
            kb.sync_bufs([x_[1] for x_ in (p1w + xs + xn1s + hTs + kts + vss + ckb + ckf + ktc)])
            kb.es = kb_es

        if DBG_STOP == "p1":
            return nc
        mkT_p, B_mkp = kb.sb("mkT_p", [128, 8, 256], BF16)
        mv_p, B_mvp = kb.sb("mv_p", [128, 2, 1024], BF16)

        xn, B_xn = kb.sb("xn", [128, D], BF16)
        sq, B_sq = xn, B_xn
        ostage, B_ost = kb.sb("ostage", [128, 2, 512], F32)
        with ExitStack() as es2:
            kb_es = kb.es
            kb.es = es2
            mx, B_mx = kb.sb("mx", [128, 2, D], F32)
            gmem, _ = kb.sb("gmem", [128, KC, 128], BF16)
            fill_gbc(gmem, 1)
            hmT, B_hmT = kb.sb("hmT", [128, KC, 256], BF16)
            wm, B_wm = kb.sb("wm", [128, KC, 1024], BF16)
            for t in range(2):
                kb.dma(sp, mx[:, t, :], memx[t * 128:(t + 1) * 128, :], [], [B_mx], B_mx)
            for t in range(2):
                rmsnorm_T(mx[:, t, :], B_mx, 128, gmem[:, :, :], hmT, B_hmT, t * 128, xn, B_xn, sq, B_sq, 16)
            if DBG_STOP == "p2a":
                kb.barrier()
                return nc
            for which, (sw, Bsw, outd) in enumerate(((s_wmk, B_wmk, memk_o), (s_wmv, B_wmv, memv_o))):
                if DBG_STOP == "p2b" and which == 1:
                    kb.barrier()
                    return nc
                kb.dma(sp, wm[:, :, :], sw.rearrange("(k p) c -> p k c", p=128), [Bsw], [B_wm], B_wm)
                if which == 0:
                    for cc in range(8):
                        b = next_ps()
                        mm_acc(b, ps[b][:, :256], [(wm[:, k, cc * 128:(cc + 1) * 128], hmT[:, k, :]) for k in range(KC)],
                               [B_wm, B_hmT])
                        evac(cc, mkT_p[:, cc, :], ps[b][:, :256], [psb[b]], [B_mkp])
                for t in range(2):
                    for cb in range(2):
                        b = next_ps()
                        mm_acc(b, ps[b][:, :], [(hmT[:, k, t * 128:(t + 1) * 128], wm[:, k, cb * 512:(cb + 1) * 512])
                                                for k in range(KC)], [B_wm, B_hmT])
                        evac(0, ostage[:, cb, :], ps[b][:, :], [psb[b]], [B_ost])
                        if which == 1:
                            kb.op(dve, lambda e, t=t, cb=cb: e.tensor_copy(out=mv_p[:, t, cb * 512:(cb + 1) * 512],
                                                                           in_=ostage[:, cb, :]), [B_ost], [B_mvp])
                        kb.dma(pool, outd[t * 128:(t + 1) * 128, cb * 512:(cb + 1) * 512], ostage[:, cb, :], [B_ost], [], B_ost)
            if DBG_STOP == "p2c":
                kb.barrier()
                return nc
            kb.sync_bufs([B_mx, B_hmT, B_wm, B_gbc])
            kb.es = kb_es

        xg, B_xg = kb.sb("xg", [128, 4, D], F32)
        hT, B_hT = kb.sb("hT", [128, KC, GT], BF16)
        NSLOT = 2
        wsl = [kb.sb("ws%d" % i, [128, 8192], BF16) for i in range(NSLOT)]
        merged, B_mg = kb.sb("merged", [128, KC, GT], BF16)
        sig, B_sig = kb.sb("sig", [128, 4, GT], F32)
        tmpf, B_tmpf = kb.sb("tmpf", [128, 2, GT], F32)


        if DBG_STOP == "p2":
            return nc
        steps = []

        def run_steps():
            emitted = [0]

            def emit_load(i):
                loads = steps[i][0]
                if not loads:
                    return
                sl, Bsl = wsl[steps[i][2] % NSLOT]
                for off, src, nk, c, Bsrc in loads:
                    kb.dma(sp, sl[:, off:off + nk * c], src, [Bsrc], [Bsl], Bsl)

            widx = 0
            for i, st in enumerate(steps):
                if st[0]:
                    steps[i] = (st[0], st[1], widx)
                    widx += 1
                else:
                    steps[i] = (st[0], st[1], -1)
            n = len(steps)
            nxt = 0
            for i in range(n):
                ahead = 0
                j = i
                while j < n and ahead < NSLOT - 1:
                    if steps[j][0]:
                        ahead += 1
                        if j >= nxt:
                            emit_load(j)
                            nxt = j + 1
                    j += 1
                nxt = max(nxt, i + 1) if not steps[i][0] else nxt
                if steps[i][0]:
                    sl, Bsl = wsl[steps[i][2] % NSLOT]
                    steps[i][1](sl, Bsl)
                else:
                    steps[i][1](None, None)

        def group(gi, NQ, x_src, row0, halo_kind, mkT, B_mk, mv, B_mv, kt_scr, B_kt, v_scr, B_v, blocks, conv_out):
            ntile = (NQ + 127) // 128
            tl = [(t, min(128, NQ - t * 128)) for t in range(ntile)]
            st = {}

            def s_load(_, __):
                for t, nt in tl:
                    kb.dma(sp, xg[:nt, t, :], x_src[t * 128:t * 128 + nt, :], [], [B_xg], B_xg)
                for t, nt in tl:
                    rmsnorm_T(xg[:nt, t, :], B_xg, nt, G_MIX, hT, B_hT, t * 128, xn, B_xn, sq, B_sq, 16)
            steps.append(([], s_load))

            for which, c0, outd in ((0, C_K, k_o), (1, C_V, v_o)):
                for cb in range(4):
                    def s_kvout(sl, Bsl, cb=cb, outd=outd):
                        w = sl[:, 0:KC * 512].rearrange("p (k c) -> p k c", c=512)
                        for t, nt in tl:
                            b = next_ps()
                            mm_acc(b, ps[b][:nt, :], [(hT[:, k, t * 128:t * 128 + nt], w[:, k, :]) for k in range(KC)],
                                   [Bsl, B_hT])
                            evac(t, ostage[:nt, t % 2, 0:512], ps[b][:nt, :], [psb[b]], [B_ost])
                            kb.dma(pool, outd[row0 + t * 128:row0 + t * 128 + nt, cb * 512:(cb + 1) * 512],
                                   ostage[:nt, t % 2, 0:512], [B_ost], [], B_ost)
                    steps.append(([(0, s_win[c0 // 512 + cb], KC, 512, B_win["kv"])], s_kvout))

            if STAGE < 2:
                return
            is_s = (halo_kind == "cache")
            sc = {}

            def open_scope():
                kb.barrier()
                sc["es"] = ExitStack()
                sc["old"] = kb.es
                kb.es = sc["es"]

            def close_scope(_=None, __=None):
                kb.barrier()
                kb.es = sc["old"]
                sc["es"].close()

            def wstep(src, nk, Bsrc, fn, c=512):
                steps.append(([(0, src, nk, c, Bsrc)], fn))

            def branch_out(bidx, gname, wsrc_fn, wk_n, Bw, rhs_key):
                for cb in range(4):
                    def s_gate(sl, Bsl, cb=cb):
                        w = sl[:, 0:KC * 512].rearrange("p (k c) -> p k c", c=512)
                        for j in range(4):
                            b = next_ps()
                            mm_acc(b, ps[b][:, :NQ], [(w[:, k, j * 128:(j + 1) * 128], hT[:, k, :NQ]) for k in range(KC)],
                                   [Bsl, B_hT])
                            kb.op(act, lambda e, j=j, b=b: e.activation(out=sig[:, j, :NQ], in_=ps[b][:, :NQ],
                                                                        func=ACT.Sigmoid), [psb[b]], [B_sig])
                    wstep(s_win[(C_G + bidx * D) // 512 + cb], KC, B_win[gname], s_gate)

                    def s_y(sl, Bsl, cb=cb):
                        rhsT, BrhsT = sc[rhs_key]
                        w = sl[:, 0:wk_n * 512].rearrange("p (k c) -> p k c", c=512)
                        for j in range(4):
                            c = cb * 4 + j
                            b = next_ps()
                            mm_acc(b, ps[b][:, :NQ], [(w[:, k, j * 128:(j + 1) * 128], rhsT[:, k, :NQ]) for k in range(wk_n)],
                                   [Bsl, BrhsT])
                            if bidx == 0:
                                kb.op(dve, lambda e, j=j, b=b, c=c: e.tensor_tensor(out=merged[:, c, :NQ], in0=ps[b][:, :NQ],
                                                                                  in1=sig[:, j, :NQ], op=ALU.mult),
                                      [psb[b], B_sig], [B_mg])
                            else:
                                kb.op(dve, lambda e, j=j, b=b: e.tensor_tensor(out=tmpf[:, j % 2, :NQ], in0=ps[b][:, :NQ],
                                                                               in1=sig[:, j, :NQ], op=ALU.mult),
                                      [psb[b], B_sig], [B_tmpf])
                                kb.op(dve, lambda e, j=j, c=c: e.tensor_tensor(out=merged[:, c, :NQ], in0=merged[:, c, :NQ],
                                                                               in1=tmpf[:, j % 2, :NQ], op=ALU.add),
                                      [B_tmpf, B_mg], [B_mg])
                    wstep(wsrc_fn(cb), wk_n, Bw, s_y)

            def s_conv_open(_, __):
                open_scope()
                sc["uext"] = kb.sb("uext", [128, 8, 32 + GT], F32)
                sc["cacc"] = kb.sb("cacc", [128, 8, GT], F32)
                sc["ub"] = kb.sb("ub", [128, 8, 32 + GT], BF16)
                sc["zT"] = kb.sb("zT", [128, 8, GT], BF16)
                sc["hh"] = kb.sb("hh", [128, KC, 32], BF16)
                sc["st"] = kb.sb("cst", [128, 3, GT], F32)
                sc["identf"] = kb.sb("identf", [128, 128], F32)
                uext, B_u = sc["uext"]
                hh, B_hh = sc["hh"]
                cacc_, B_xh = sc["cacc"]
                xh = cacc_[:, 0:4, :].rearrange("p a c -> p (a c)")
                identf, B_if = sc["identf"]
                kb.op(dve, lambda e: e.tensor_copy(out=identf[:, :], in_=ident[:, :]), [B_c], [B_if])
                if not is_s:
                    kb.dma(sp, xh[:32, :], xhalo[gi], [], [B_xh], B_xh)
                    rmsnorm_T(xh[:32, :], B_xh, 32, G_MIX, hh, B_hh, 0, xn, B_xn, sq, B_sq, 16)
                else:
                    kb.op(dve, lambda e: e.memset(xh[:32, 0:DCONV], 0.0), [], [B_xh])
                    kb.dma(sp, xh[2:32, 0:DCONV], cconv[:, :], [], [B_xh], B_xh)
                    kb.op(act, lambda e: e.activation(out=xn[:32, 0:DCONV], in_=xh[:32, 0:DCONV], func=ACT.Copy),
                          [B_xh], [B_xn])
                    b = next_ps()
                    pv = ps[b][:].bitcast(BF16).rearrange("p (j c) -> p j c", c=128)
                    for cc in range(8):
                        kb.op(pe, lambda e, cc=cc, pv=pv: e.transpose(pv[:, cc, :32], xn[:32, cc * 128:(cc + 1) * 128],
                                                                      ident[:32, :32]), [B_xn, B_c], [psb[b]])
                    kb.op(dve, lambda e, pv=pv: e.tensor_copy(out=uext[:, :, 0:32], in_=pv[:, :, :32]), [psb[b]], [B_u])
            steps.append(([], s_conv_open))

            for half_i, cbase in ((0, C_AGT), (1, C_AGT + DCONV)):
                for cb in range(2):
                    def s_agt(sl, Bsl, half_i=half_i, cb=cb):
                        uext, B_u = sc["uext"]
                        hh, B_hh = sc["hh"]
                        w = sl[:, 0:KC * 512].rearrange("p (k c) -> p k c", c=512)
                        for j in range(4):
                            cc = cb * 4 + j
                            parts = [(32, NQ, hT, B_hT)]
                            if not is_s:
                                parts.append((0, 32, hh, B_hh))
                            for (o0, n, src, Bs) in parts:
                                b = next_ps()
                                mm_acc(b, ps[b][:, :n], [(w[:, k, j * 128:(j + 1) * 128], src[:, k, :n]) for k in range(KC)],
                                       [Bsl, Bs])
                                if half_i == 0:
                                    evac(j, uext[:, cc, o0:o0 + n], ps[b][:, :n], [psb[b]], [B_u])
                                else:
                                    kb.op(act, lambda e, b=b, n=n: e.activation(out=tmpf[:, 0, :n], in_=ps[b][:, :n],
                                                                                func=ACT.Sigmoid), [psb[b]], [B_tmpf])
                                    kb.op(dve, lambda e, cc=cc, o0=o0, n=n: e.tensor_tensor(
                                        out=uext[:, cc, o0:o0 + n], in0=uext[:, cc, o0:o0 + n], in1=tmpf[:, 0, :n],
                                        op=ALU.mult), [B_tmpf, B_u], [B_u])
                    wstep(s_win[cbase // 512 + cb], KC, B_win["agt"], s_agt)

            def s_conv_pre(_, __):
                uext, B_u = sc["uext"]
                identf, B_if = sc["identf"]
                ub, B_ub = sc["ub"]
                xh, B_xh = ostage[:, :, :].rearrange("p a c -> p (a c)"), B_ost
                kb.op(act, lambda e: e.activation(out=ub[:, :, 0:32 + NQ], in_=uext[:, :, 0:32 + NQ], func=ACT.Copy),
                      [B_u], [B_ub])
                if conv_out is not None:
                    for half in range(2):
                        b = next_ps()
                        for j in range(4):
                            cc = half * 4 + j
                            kb.op(pe, lambda e, cc=cc, b=b, j=j: e.transpose(ps[b][:32, j * 128:(j + 1) * 128],
                                                                            uext[:, cc, NQ:NQ + 32], identf[:, :]),
                                  [B_u, B_if], [psb[b]])
                        evac(half, xh[:32, half * 512:(half + 1) * 512], ps[b][:32, :], [psb[b]], [B_xh])
                    kb.dma(pool, conv_out[:, :], xh[2:32, 0:DCONV], [B_xh], [], B_xh)
            steps.append(([], s_conv_pre))
            for cc in range(8):
                def s_dw(sl, Bsl, cc=cc):
                    ub, B_ub = sc["ub"]
                    cacc, B_ca = sc["cacc"]
                    b = next_ps()
                    mm_acc(b, ps[b][:, :NQ], [(sl[:, k * 128:(k + 1) * 128], ub[:, cc, 2 + k:2 + k + NQ]) for k in range(31)],
                           [Bsl, B_ub])
                    kb.op(act, lambda e, b=b: e.activation(out=cacc[:, cc, :NQ], in_=ps[b][:, :NQ], func=ACT.Identity,
                                                           bias=pcv[:, 31, cc:cc + 1]), [psb[b], B_pcv], [B_ca])
                steps.append(([(0, s_dg[cc], 1, 31 * 128, dgB)], s_dw))

            def s_conv(_, __):
                uext, B_u = sc["uext"]
                cacc, B_ca = sc["cacc"]
                zT, B_z = sc["zT"]
                cst, B_st = sc["st"]
                ysq, B_ysq = sc["ub"]
                kb.op(act, lambda e: e.activation(out=zT[:, :, :NQ], in_=cacc[:, :, :NQ], func=ACT.Copy), [B_ca], [B_z])
                kb.op(act, lambda e: e.activation(out=ysq[:, :, :NQ], in_=cacc[:, :, :NQ], func=ACT.Square), [B_ca], [B_ysq])
                b1 = next_ps()
                mm_acc(b1, ps[b1][:, :NQ], [(ones[:, :], zT[:, k, :NQ]) for k in range(8)], [B_z, B_c])
                b2 = next_ps()
                mm_acc(b2, ps[b2][:, :NQ], [(ones[:, :], ysq[:, k, :NQ]) for k in range(8)], [B_ysq, B_c])
                mean, msq, var = cst[:, 0, :NQ], cst[:, 1, :NQ], cst[:, 2, :NQ]
                kb.op(dve, lambda e: e.tensor_scalar(out=mean, in0=ps[b1][:, :NQ], scalar1=1.0 / DCONV, scalar2=None,
                                                     op0=ALU.mult), [psb[b1]], [B_st])
                kb.op(dve, lambda e: e.tensor_tensor(out=msq, in0=mean, in1=mean, op=ALU.mult), [B_st], [B_st])
                kb.op(dve, lambda e: e.scalar_tensor_tensor(out=var, in0=ps[b2][:, :NQ], scalar=1.0 / DCONV, in1=msq,
                                                            op0=ALU.mult, op1=ALU.subtract), [psb[b2], B_st], [B_st])
                kb.op(act, lambda e: e.activation(out=var, in_=var, func=ACT.Sqrt, bias=EPS), [B_st], [B_st])
                kb.op(dve, lambda e: e.reciprocal(out=var, in_=var), [B_st], [B_st])
                for cc in range(8):
                    kb.op(dve, lambda e, cc=cc: e.tensor_tensor(out=cacc[:, cc, :NQ], in0=cacc[:, cc, :NQ], in1=mean,
                                                                op=ALU.subtract), [B_st, B_ca], [B_ca])
                    kb.op(dve, lambda e, cc=cc: e.tensor_tensor(out=cacc[:, cc, :NQ], in0=cacc[:, cc, :NQ], in1=var,
                                                                op=ALU.mult), [B_st, B_ca], [B_ca])
                    kb.op(act, lambda e, cc=cc: e.activation(out=zT[:, cc, :NQ], in_=cacc[:, cc, :NQ], func=ACT.Silu,
                                                             scale=pcv[:, 32, cc:cc + 1], bias=pcv[:, 33, cc:cc + 1]),
                          [B_ca, B_pcv, B_z], [B_z])
            steps.append(([], s_conv))
            branch_out(0, "g0", lambda cb: s_wco[cb], 8, B_wco, "zT")
            steps.append(([], close_scope))

            def s_mem_open(_, __):
                open_scope()
                sc["mqT"] = kb.sb("mqT", [128, 8, GT], BF16)
                sc["moT"] = kb.sb("moT", [128, 8, GT], BF16)
                sc["mP"] = kb.sb("mP", [128, 2, GT], BF16)
                sc["mr"] = kb.sb("mr", [128, GT], F32)
            steps.append(([], s_mem_open))
            for cb in range(2):
                def s_mq(sl, Bsl, cb=cb):
                    mqT, B_mq = sc["mqT"]
                    w = sl[:, 0:KC * 512].rearrange("p (k c) -> p k c", c=512)
                    for j in range(4):
                        b = next_ps()
                        mm_acc(b, ps[b][:, :NQ], [(w[:, k, j * 128:(j + 1) * 128], hT[:, k, :NQ]) for k in range(KC)],
                               [Bsl, B_hT])
                        evac(j, mqT[:, cb * 4 + j, :NQ], ps[b][:, :NQ], [psb[b]], [B_mq], scale=1.0 / 16.0)
                wstep(s_win[C_MQ // 512 + cb], KC, B_win["mq"], s_mq)

            def s_mem(_, __):
                mqT, B_mq = sc["mqT"]
                moT, B_mo = sc["moT"]
                mP, B_mP = sc["mP"]
                mr, B_mr = sc["mr"]
                for hd in range(4):
                    for mt in range(2):
                        b = next_ps()
                        mm_acc(b, ps[b][:, :NQ], [(mkT[:, hd * 2 + hf, mt * 128:(mt + 1) * 128], mqT[:, hd * 2 + hf, :NQ])
                                                  for hf in range(2)], [B_mk, B_mq])
                        kb.op(act, lambda e, b=b, mt=mt: e.activation(out=mP[:, mt, :NQ], in_=ps[b][:, :NQ], func=ACT.Exp),
                              [psb[b]], [B_mP])
                    bl = next_ps()
                    mm_acc(bl, ps[bl][:, :NQ], [(ones[:, :], mP[:, mt, :NQ]) for mt in range(2)], [B_mP, B_c])
                    kb.op(dve, lambda e, bl=bl: e.reciprocal(out=mr[:, :NQ], in_=ps[bl][:, :NQ]), [psb[bl]], [B_mr])
                    for dh in range(2):
                        b = next_ps()
                        mm_acc(b, ps[b][:, :NQ], [(mv[:, mt, hd * 256 + dh * 128:hd * 256 + (dh + 1) * 128], mP[:, mt, :NQ])
                                                  for mt in range(2)], [B_mv, B_mP])
                        kb.op(dve, lambda e, b=b, hd=hd, dh=dh: e.tensor_tensor(out=moT[:, hd * 2 + dh, :NQ], in0=ps[b][:, :NQ],
                                                                               in1=mr[:, :NQ], op=ALU.mult),
                              [psb[b], B_mr], [B_mo])
            steps.append(([], s_mem))
            branch_out(2, "g2", lambda cb: s_wmo[cb], 8, B_wmo, "moT")
            steps.append(([], close_scope))

            def s_att_open(_, __):
                open_scope()
                sc["qT"] = kb.sb("qT", [128, 16, GT], BF16)
                sc["onT"] = kb.sb("onT", [128, 16, GT], BF16)
                sc["kt"] = [kb.sb("akt%d" % i, [128, 2048], BF16) for i in range(2)]
                sc["vv"] = [kb.sb("avv%d" % i, [128, 16, 128], BF16) for i in range(2)]
                sc["P"] = [kb.sb("aP%d" % i, [128, GT], BF16) for i in range(4)]
                sc["fin"] = kb.sb("afin", [128, 4, GT], F32)
                sc["osq"] = kb.sb("aosq", [128, GT], BF16)
            steps.append(([], s_att_open))
            for cb in range(4):
                def s_q(sl, Bsl, cb=cb):
                    qT, B_q = sc["qT"]
                    w = sl[:, 0:KC * 512].rearrange("p (k c) -> p k c", c=512)
                    for j in range(4):
                        b = next_ps()
                        mm_acc(b, ps[b][:, :NQ], [(w[:, k, j * 128:(j + 1) * 128], hT[:, k, :NQ]) for k in range(KC)],
                               [Bsl, B_hT])
                        evac(j, qT[:, cb * 4 + j, :NQ], ps[b][:, :NQ], [psb[b]], [B_q], scale=0.125)
                wstep(s_win[C_Q // 512 + cb], KC, B_win["q"], s_q)

            segs = []
            if not is_s:
                for s_ in range(gi):
                    segs.append((s_ * 2048, [(128, "full", None)] * 16))
                dblk = [(128, "diag", kb_) for kb_ in range(4)] + [(128, "bias", 1 + (bk - 4) // 4) for bk in range(4, 16)]
                segs.append((gi * 2048, dblk))
            else:
                nb_tot = PAST // 128
                k0 = 0
                while nb_tot > 0:
                    n = min(16, nb_tot)
                    segs.append((k0, [(128, "full", None)] * n))
                    k0 += n * 128
                    nb_tot -= n
                if len(segs[-1][1]) < 16:
                    segs[-1][1].append((NSAMP, "full", None))
                else:
                    segs.append((k0, [(NSAMP, "full", None)]))

            def s_att(_, __):
                qT, B_q = sc["qT"]
                onT, B_on = sc["onT"]
                fin, B_fin = sc["fin"]
                osq, B_osq = sc["osq"]
                ktv = kt_scr
                vvv = v_scr
                li = [0]
                pi = [0]
                nblk_all = sum(len(s_[1]) for s_ in segs)
                bO = [4, 5, 6, 7]
                r1, r2, t1, o = fin[:, 0, :NQ], fin[:, 1, :NQ], fin[:, 2, :NQ], fin[:, 3, :NQ]

                items = []
                for h in range(16):
                    bi = 0
                    for (k0, blks) in segs:
                        for bl, (kn, kind, arg) in enumerate(blks):
                            items.append(dict(h=h, k0=k0, blks=blks, bl=bl, kn=kn, kind=kind, arg=arg,
                                              first=(bi == 0), last=(bi == nblk_all - 1), bi=bi))
                            bi += 1

                def emit_S(it):
                    h = it["h"]
                    if it["bl"] == 0:
                        ktt, B_ktt = sc["kt"][li[0] % 2]
                        vvt, B_vvt = sc["vv"][li[0] % 2]
                        li[0] += 1
                        blks, k0 = it["blks"], it["k0"]
                        nk = sum(b_[0] for b_ in blks)
                        nfull = sum(1 for b_ in blks if b_[0] == 128)
                        kb.dma(sp, ktt[:, :nk], ktv[h, :, k0:k0 + nk], [B_kt], [B_ktt], B_ktt)
                        if nfull:
                            kb.dma(sp, vvt[:, :nfull, :], vvv[h, :, k0 // 128:k0 // 128 + nfull, :], [B_v], [B_vvt], B_vvt)
                        if nfull < len(blks):
                            kn_ = blks[-1][0]
                            kb.dma(sp, vvt[:kn_, nfull, :], vvv[h, :kn_, k0 // 128 + nfull, :], [B_v], [B_vvt], B_vvt)
                        cur["kt"] = (ktt, B_ktt)
                        cur["vv"] = (vvt, B_vvt)
                    ktt, B_ktt = cur["kt"]
                    it["vv"] = cur["vv"]
                    kn, kind, arg, bl = it["kn"], it["kind"], it["arg"], it["bl"]
                    c0 = 128 * arg if kind == "diag" else 0
                    it["c0"] = c0
                    it["P"] = []
                    for mp in range(2):
                        bS = next_ps("s")
                        p0 = mp * 64
                        kb.op(pe, lambda e, bS=bS, p0=p0: e.matmul(
                            ps[bS][:kn, c0:NQ], ktt[p0:p0 + 64, bl * 128:bl * 128 + kn], qT[p0:p0 + 64, h, c0:NQ],
                            start=True, stop=True), [B_ktt, B_q], [psb[bS]])
                        Pt, B_P = sc["P"][pi[0] % 4]
                        pi[0] += 1
                        it["P"].append((Pt, B_P))
                        if kind == "bias":
                            kb.op(act, lambda e, bS=bS, Pt=Pt: e.activation(
                                out=Pt[:kn, c0:NQ], in_=ps[bS][:kn, c0:NQ], func=ACT.Exp,
                                bias=cols[:kn, 8 + arg:9 + arg]), [psb[bS], B_cols], [B_P])
                        else:
                            kb.op(act, lambda e, bS=bS, Pt=Pt: e.activation(
                                out=Pt[:kn, c0:NQ], in_=ps[bS][:kn, c0:NQ], func=ACT.Exp), [psb[bS]], [B_P])
                        if kind == "diag":
                            kb.op(pool, lambda e, Pt=Pt: e.memset(Pt[64:128, c0:c0 + 64], 0.0), [], [B_P])

                def emit_PV(it):
                    kn, bl, c0, first, last = it["kn"], it["bl"], it["c0"], it["first"], it["last"]
                    vvt, B_vvt = it["vv"]
                    for mp in range(2):
                        Pt, B_P = it["P"][mp]
                        kb.op(pe, lambda e, mp=mp, Pt=Pt: e.matmul(
                            ps[bO[2 * mp]][:, c0:NQ], vvt[:kn, bl, :], Pt[:kn, c0:NQ], start=first, stop=last),
                            [B_vvt, B_P], [psb[bO[2 * mp]]])
                        kb.op(pe, lambda e, mp=mp, Pt=Pt: e.matmul(
                            ps[bO[2 * mp + 1]][:, c0:NQ], ones[:kn, :], Pt[:kn, c0:NQ], start=first, stop=last),
                            [B_c, B_P], [psb[bO[2 * mp + 1]]])

                def fin1():
                    kb.op(dve, lambda e: e.reciprocal(out=r1, in_=ps[5][:, :NQ]), [psb[5]], [B_fin])
                    kb.op(dve, lambda e: e.reciprocal(out=r2, in_=ps[7][:, :NQ]), [psb[7]], [B_fin])
                    kb.op(dve, lambda e: e.tensor_tensor(out=t1, in0=ps[4][:, :NQ], in1=r1, op=ALU.mult), [psb[4], B_fin], [B_fin])
                    kb.op(dve, lambda e: e.tensor_tensor(out=r2, in0=ps[6][:, :NQ], in1=r2, op=ALU.mult), [psb[6], B_fin], [B_fin])
                    kb.op(dve, lambda e: e.scalar_tensor_tensor(out=o, in0=r2, scalar=cols[:, 1:2], in1=t1, op0=ALU.mult,
                                                                op1=ALU.add), [B_fin, B_cols], [B_fin])
                    kb.op(act, lambda e: e.activation(out=osq[:, :NQ], in_=o, func=ACT.Square), [B_fin], [B_osq])

                def fin2(h):
                    bs_ = next_ps("s")
                    kb.op(pe, lambda e, bs_=bs_: e.matmul(ps[bs_][:, :NQ], ones[:, :], osq[:, :NQ], start=True, stop=True),
                          [B_osq, B_c], [psb[bs_]])
                    kb.op(act, lambda e, bs_=bs_: e.activation(out=r1, in_=ps[bs_][:, :NQ], func=ACT.Sqrt, scale=1.0 / 128,
                                                               bias=1e-5), [psb[bs_]], [B_fin])
                    kb.op(dve, lambda e: e.reciprocal(out=r1, in_=r1), [B_fin], [B_fin])
                    kb.op(dve, lambda e, h=h: e.scalar_tensor_tensor(out=onT[:, h, :NQ], in0=o, scalar=cols[:, 2:3], in1=r1,
                                                                     op0=ALU.mult, op1=ALU.mult), [B_fin, B_cols], [B_on])

                cur = {}
                n_it = len(items)
                pend_fin2 = None
                emit_S(items[0])
                for i in range(n_it):
                    it = items[i]
                    if i + 1 < n_it:
                        emit_S(items[i + 1])
                    emit_PV(it)
                    if pend_fin2 is not None and (it["bi"] >= min(1, nblk_all - 1)):
                        fin2(pend_fin2)
                        pend_fin2 = None
                    if it["last"]:
                        fin1()
                        pend_fin2 = it["h"]
                if pend_fin2 is not None:
                    fin2(pend_fin2)
            steps.append(([], s_att))
            branch_out(1, "g1", lambda cb: s_wao[cb], KC, B_wao, "onT")
            steps.append(([], close_scope))

            sc["merged"] = (merged, B_mg)
            for cb in range(4):
                def s_wout(sl, Bsl, cb=cb):
                    w = sl[:, 0:KC * 512].rearrange("p (k c) -> p k c", c=512)
                    for t, nt in tl:
                        b = next_ps()
                        mm_acc(b, ps[b][:nt, :], [(merged[:, k, t * 128:t * 128 + nt], w[:, k, :]) for k in range(KC)],
                               [Bsl, B_mg])
                        kb.op(dve, lambda e, b=b, t=t, nt=nt: e.tensor_tensor(
                            out=xg[:nt, t, cb * 512:(cb + 1) * 512], in0=xg[:nt, t, cb * 512:(cb + 1) * 512],
                            in1=ps[b][:nt, :], op=ALU.add), [psb[b], B_xg], [B_xg])
                wstep(s_wo[cb], KC, B_wo, s_wout)

            def s_moe_open(_, __):
                open_scope()
                sc["acc"] = kb.sb("macc", [128, 4, D], F32)
                sc["aT"] = [kb.sb("maT%d" % i, [128, 2, GT], BF16) for i in range(2)]
                sc["sg"] = [kb.sb("msg%d" % i, [128, GT], F32) for i in range(2)]
                sc["comb"] = kb.sb("mcomb", [128, 4, 32], F32)
                sc["mtmp"] = [kb.sb("mtmp%d" % i, [128, 512], F32) for i in range(3)]
                sc["rt"] = kb.sb("mrt", [128, 96], F32)
                sc["gfin"] = kb.sb("gfin", [128, D], F32)
                acc, B_acc = sc["acc"]
                sc["accp"] = Buf("accp")
                comb, B_comb = sc["comb"]
                rt, B_rt = sc["rt"]
                gfin, B_gfin = sc["gfin"]
                kb.dma(sp, gfin[:, :], norm_final.partition_broadcast(128), [], [B_gfin], B_gfin)
                kb.op(pool, lambda e: e.memset(acc[:, :, :], 0.0), [], [B_acc, sc["accp"]])
                for t, nt in tl:
                    rmsnorm_T(xg[:nt, t, :], B_xg, nt, G_FFN, hT, B_hT, t * 128, xn, B_xn, sq, B_sq, 16)
                for t, nt in tl:
                    b = next_ps()
                    mm_acc(b, ps[b][:nt, :36], [(hT[:, k, t * 128:t * 128 + nt], wr_sb[:, k, :]) for k in range(KC)],
                           [B_hT, B_wrs])
                    lg = rt[:nt, 0:36]
                    gmax, ngmax, gsum, emax, nemax, m2, den = (rt[:nt, 36 + i:37 + i] for i in range(7))
                    gmask = rt[:nt, 44:48]
                    pen = rt[:nt, 48:52]
                    ge = rt[:nt, 52:56]
                    ee = rt[:nt, 56:88]
                    kb.op(dve, lambda e, b=b, nt=nt, lg=lg: e.tensor_tensor(out=lg, in0=ps[b][:nt, :36], in1=rbias[:nt, :],
                                                                         op=ALU.add), [psb[b], B_rb], [B_rt])
                    kb.op(dve, lambda e, lg=lg, gmax=gmax: e.reduce_max(out=gmax, in_=lg[:, 0:4], axis=AX.X), [B_rt], [B_rt])
                    kb.op(dve, lambda e, gmax=gmax, ngmax=ngmax: e.tensor_scalar(out=ngmax, in0=gmax, scalar1=-1.0, scalar2=None,
                                                                                 op0=ALU.mult), [B_rt], [B_rt])
                    kb.op(dve, lambda e, lg=lg, gmax=gmax, gmask=gmask: e.tensor_scalar(out=gmask, in0=lg[:, 0:4], scalar1=gmax,
                                                                                      scalar2=None, op0=ALU.is_ge),
                          [B_rt], [B_rt])
                    kb.op(dve, lambda e, gsum=gsum: e.memset(gsum, 0.0), [], [B_rt])
                    kb.op(act, lambda e, lg=lg, ge=ge, ngmax=ngmax, gsum=gsum: e.activation(
                        out=ge, in_=lg[:, 0:4], func=ACT.Exp, bias=ngmax, accum_out=gsum), [B_rt], [B_rt])
                    kb.op(dve, lambda e, gmask=gmask, pen=pen: e.tensor_scalar(out=pen, in0=gmask, scalar1=-1.0, scalar2=1e30,
                                                                              op0=ALU.add, op1=ALU.mult), [B_rt], [B_rt])
                    for g in range(4):
                        kb.op(dve, lambda e, g=g, lg=lg, pen=pen, ee=ee: e.tensor_scalar(
                            out=ee[:, g * 8:(g + 1) * 8], in0=lg[:, 4 + g * 8:12 + g * 8], scalar1=pen[:, g:g + 1],
                            scalar2=None, op0=ALU.add), [B_rt], [B_rt])
                    kb.op(dve, lambda e, ee=ee, emax=emax: e.reduce_max(out=emax, in_=ee, axis=AX.X), [B_rt], [B_rt])
                    kb.op(dve, lambda e, emax=emax, nemax=nemax: e.tensor_scalar(out=nemax, in0=emax, scalar1=-1.0, scalar2=None,
                                                                                 op0=ALU.mult), [B_rt], [B_rt])
                    kb.op(act, lambda e, ee=ee, nemax=nemax: e.activation(out=ee, in_=ee, func=ACT.Exp, bias=nemax),
                          [B_rt], [B_rt])
                    e2 = rt[:nt, 0:32]
                    kb.op(dve, lambda e, ee=ee, e2=e2: e.scalar_tensor_tensor(out=e2, in0=ee, scalar=1.0, in1=ee, op0=ALU.is_lt,
                                                                             op1=ALU.mult), [B_rt], [B_rt])
                    kb.op(dve, lambda e, e2=e2, m2=m2: e.reduce_max(out=m2, in_=e2, axis=AX.X), [B_rt], [B_rt])
                    kb.op(dve, lambda e, m2=m2, den=den, gsum=gsum: e.scalar_tensor_tensor(
                        out=den, in0=m2, scalar=1.0, in1=gsum, op0=ALU.add, op1=ALU.mult), [B_rt], [B_rt])
                    kb.op(dve, lambda e, den=den: e.reciprocal(out=den, in_=den), [B_rt], [B_rt])
                    kb.op(dve, lambda e, ee=ee, e2=e2, m2=m2: e.scalar_tensor_tensor(out=e2, in0=ee, scalar=m2, in1=ee,
                                                                                   op0=ALU.is_ge, op1=ALU.mult), [B_rt], [B_rt])
                    kb.op(dve, lambda e, e2=e2, den=den, t=t, nt=nt: e.tensor_scalar(out=comb[:nt, t, :], in0=e2, scalar1=den,
                                                                                    scalar2=None, op0=ALU.mult),
                          [B_rt], [B_comb])
            steps.append(([], s_moe_open))

            mtc = [0]
            for ex in range(NEXP):
                def s_gu(sl, Bsl, ex=ex):
                    aT, B_aT = sc["aT"][ex % 2]
                    wg = sl[:, 0:KC * 256].rearrange("p (k c) -> p k c", c=256)
                    wu = sl[:, KC * 256:2 * KC * 256].rearrange("p (k c) -> p k c", c=256)
                    for hc in range(2):
                        sg, B_sg = sc["sg"][hc]
                        bg = next_ps()
                        mm_acc(bg, ps[bg][:, :NQ], [(wg[:, k, hc * 128:(hc + 1) * 128], hT[:, k, :NQ]) for k in range(KC)],
                               [Bsl, B_hT])
                        bu = next_ps()
                        mm_acc(bu, ps[bu][:, :NQ], [(wu[:, k, hc * 128:(hc + 1) * 128], hT[:, k, :NQ]) for k in range(KC)],
                               [Bsl, B_hT])
                        kb.op(act, lambda e, bg=bg, sg=sg: e.activation(out=sg[:, :NQ], in_=ps[bg][:, :NQ], func=ACT.Silu),
                              [psb[bg]], [B_sg])
                        kb.op(dve, lambda e, bu=bu, sg=sg, hc=hc, aT=aT: e.tensor_tensor(out=aT[:, hc, :NQ], in0=ps[bu][:, :NQ],
                                                                                        in1=sg[:, :NQ], op=ALU.mult),
                              [psb[bu], B_sg], [B_aT])
                steps.append(([(0, s_egu[ex], 2 * KC, 256, B_exp[ex // 4])], s_gu))

                def s_dn(sl, Bsl, ex=ex):
                    aT, B_aT = sc["aT"][ex % 2]
                    acc, B_acc = sc["acc"]
                    B_accp = sc["accp"]
                    comb, B_comb = sc["comb"]
                    wd = sl[:, 0:2 * D].rearrange("p (k c) -> p k c", c=D)
                    for t, nt in tl:
                        for cb in range(4):
                            b = next_ps()
                            mm_acc(b, ps[b][:nt, :], [(aT[:, hc, t * 128:t * 128 + nt], wd[:, hc, cb * 512:(cb + 1) * 512])
                                                      for hc in range(2)], [Bsl, B_aT])
                            if MOE_SPLIT and (t * 4 + cb) % 3 == 2:
                                mt_, B_mt = sc["mtmp"][mtc[0] % 3]
                                mtc[0] += 1
                                kb.op(act, lambda e, b=b, t=t, nt=nt, mt_=mt_: e.activation(
                                    out=mt_[:nt, :], in_=ps[b][:nt, :], func=ACT.Copy, scale=comb[:nt, t, ex:ex + 1]),
                                    [psb[b], B_comb], [B_mt])
                                kb.op(pool, lambda e, t=t, nt=nt, cb=cb, mt_=mt_: e.tensor_tensor(
                                    out=acc[:nt, t, cb * 512:(cb + 1) * 512], in0=acc[:nt, t, cb * 512:(cb + 1) * 512],
                                    in1=mt_[:nt, :], op=ALU.add), [B_mt, B_accp], [B_accp])
                            else:
                                kb.op(dve, lambda e, b=b, t=t, nt=nt, cb=cb: e.scalar_tensor_tensor(
                                    out=acc[:nt, t, cb * 512:(cb + 1) * 512], in0=ps[b][:nt, :], scalar=comb[:nt, t, ex:ex + 1],
                                    in1=acc[:nt, t, cb * 512:(cb + 1) * 512], op0=ALU.mult, op1=ALU.add),
                                    [psb[b], B_comb, B_acc], [B_acc])
                steps.append(([(0, s_ed[ex], 2, D, B_exp[ex // 4])], s_dn))

            def s_final(_, __):
                acc, B_acc = sc["acc"]
                B_accp = sc["accp"]
                gfin, B_gfin = sc["gfin"]
                for t, nt in tl:
                    c_ss = cols[:nt, 20:21]
                    c_r = cols[:nt, 21:22]
                    kb.op(dve, lambda e, t=t, nt=nt: e.tensor_tensor(out=acc[:nt, t, :], in0=acc[:nt, t, :], in1=xg[:nt, t, :],
                                                                    op=ALU.add), [B_xg, B_acc, B_accp], [B_acc, B_accp])
                    kb.op(dve, lambda e, c_ss=c_ss: e.memset(c_ss, 0.0), [], [B_cols])
                    kb.op(act, lambda e, t=t, nt=nt, c_ss=c_ss: e.activation(out=sq[:nt, :], in_=acc[:nt, t, :], func=ACT.Square,
                                                                            accum_out=c_ss), [B_acc, B_cols], [B_sq, B_cols])
                    kb.op(act, lambda e, c_ss=c_ss, c_r=c_r: e.activation(out=c_r, in_=c_ss, func=ACT.Sqrt, scale=1.0 / D,
                                                                          bias=EPS), [B_cols], [B_cols])
                    kb.op(dve, lambda e, c_r=c_r: e.reciprocal(out=c_r, in_=c_r), [B_cols], [B_cols])
                    kb.op(dve, lambda e, t=t, nt=nt, c_r=c_r: e.scalar_tensor_tensor(
                        out=acc[:nt, t, :], in0=acc[:nt, t, :], scalar=c_r, in1=gfin[:nt, :], op0=ALU.mult, op1=ALU.mult),
                        [B_acc, B_cols, B_gfin], [B_acc])
                    kb.dma(pool, y_o[row0 + t * 128:row0 + t * 128 + nt, :], acc[:nt, t, :], [B_acc], [], B_acc)
            steps.append(([], s_final))
            steps.append(([], close_scope))

        for gi in range(NGRP_):
            blocks = None
            group(gi, GT, xfull[gi * 4 * GT:(gi * 4 + 1) * GT, :], gi * GT, "x", mkT_p, B_mkp, mv_p, B_mvp,
                  s_ktp, B_ktp, s_vp, B_vp, blocks, convp_o if gi == NGRP_ - 1 else None)
        def s_mem_sample(_, __):
            cmb = xn[:, :].rearrange("p (t c) -> p t c", c=1024)
            for t in range(2):
                kb.dma(pool, cmb[:, t, :], cmk[t * 128:(t + 1) * 128, :], [], [B_xn], B_xn)
                kb.dma(pool, mv_p[:, t, :], cmv[t * 128:(t + 1) * 128, :], [], [B_mvp], B_mvp)
            for t in range(2):
                b = next_ps()
                pv = ps[b][:].bitcast(BF16).rearrange("p (j c) -> p j c", c=128)
                for cc in range(8):
                    kb.op(pe, lambda e, cc=cc, pv=pv, t=t: e.transpose(pv[:, cc, :], cmb[:, t, cc * 128:(cc + 1) * 128],
                                                                      ident[:, :]), [B_xn, B_c], [psb[b]])
                evac(t, mkT_p[:, :, t * 128:(t + 1) * 128], pv[:, :, :], [psb[b]], [B_mkp])
        steps.append(([], s_mem_sample))
        group(NGRP_, NSAMP, xsamp, NGRP_ * GT, "cache", mkT_p, B_mkp, mv_p, B_mvp, s_kts, B_kts, s_vs, B_vs, None, convs_o)
        run_steps()

        kb.barrier()
    return nc


_NC_CACHE = {}


def kernel(**inp):
    inp = {k: np.asarray(v) for k, v in inp.items()}
    if "nc" not in _NC_CACHE:
        _NC_CACHE["nc"] = build_program()
    nc = _NC_CACHE["nc"]
    xp = inp["x_prompt"]
    in_maps = []
    wnames = ["norm_mix", "w_in", "w_dw", "b_dw", "conv_ln_g", "conv_ln_b", "w_conv_out", "lambda_q1", "lambda_k1",
              "lambda_q2", "lambda_k2", "subln_g", "w_attn_out", "norm_mem", "w_mem_k", "w_mem_v", "w_mem_out",
              "w_out", "norm_ffn", "w_router_grp", "b_router_grp", "w_router_exp", "b_router_exp", "w_exp_gate",
              "w_exp_up", "w_exp_down"]
    wts = {n: np.ascontiguousarray(inp[n][0]) for n in wnames}
    wts["norm_final"] = np.ascontiguousarray(inp["norm_final"])
    for c in range(8):
        b, j = c // 4, c % 4
        xb = xp[b].reshape(16, GT, D)
        order = []
        for i in range(4):
            order.append(4 * i + j)
            order += [4 * i + m for m in range(4) if m != j]
        xfull = np.ascontiguousarray(xb[order].reshape(SEQ, D))
        xhalo = np.zeros((NGRP, 32, D), np.float32)
        for i in range(4):
            g = 4 * i + j
            if g > 0:
                xhalo[i] = xp[b, g * GT - 32:g * GT]
        gp = np.array([j] + [m for m in range(4) if m != j], np.float32)
        m = dict(wts)
        m.update({
            "xfull": xfull, "xhalo": xhalo, "gpos": np.ascontiguousarray(np.broadcast_to(gp, (128, 4))),
            "xsamp": np.ascontiguousarray(inp["x_sample"][c]),
            "cconv": np.ascontiguousarray(inp["cache_conv"][0, c]),
            "ck": np.ascontiguousarray(inp["cache_diff_k"][0, c].reshape(PAST, D)),
            "cv": np.ascontiguousarray(inp["cache_diff_v"][0, c].reshape(PAST, D)),
            "cmk": np.ascontiguousarray(inp["cache_mem_k"][0, c].reshape(256, 1024)),
            "cmv": np.ascontiguousarray(inp["cache_mem_v"][0, c].reshape(256, 1024)),
            "memx": np.ascontiguousarray(inp["mem_prompt"][b]),
        })
        in_maps.append(m)
    res = run_bass_kernel_spmd(nc, in_maps, core_ids=list(range(8)))
    R = res.results
    y_p = np.zeros((2, SEQ, D), np.float32)
    k_p = np.zeros((1, 2, SEQ, 16, 128), np.float32)
    v_p = np.zeros((1, 2, SEQ, 16, 128), np.float32)
    y_s = np.zeros((8, NSAMP, D), np.float32)
    k_s = np.zeros((1, 8, NSAMP, 16, 128), np.float32)
    v_s = np.zeros((1, 8, NSAMP, 16, 128), np.float32)
    conv_p = np.zeros((1, 2, 30, DCONV), np.float32)
    conv_s = np.zeros((1, 8, 30, DCONV), np.float32)
    mk_p = np.zeros((1, 2, 256, 4, 256), np.float32)
    mv_p = np.zeros((1, 2, 256, 4, 256), np.float32)
    for c in range(8):
        b, j = c // 4, c % 4
        r = R[c]
        for i in range(4):
            g = 4 * i + j
            y_p[b, g * GT:(g + 1) * GT] = r["y"][i * GT:(i + 1) * GT]
            k_p[0, b, g * GT:(g + 1) * GT] = r["kout"][i * GT:(i + 1) * GT].reshape(GT, 16, 128)
            v_p[0, b, g * GT:(g + 1) * GT] = r["vout"][i * GT:(i + 1) * GT].reshape(GT, 16, 128)
        y_s[c] = r["y"][NGRP * GT:]
        k_s[0, c] = r["kout"][NGRP * GT:].reshape(NSAMP, 16, 128)
        v_s[0, c] = r["vout"][NGRP * GT:].reshape(NSAMP, 16, 128)
        conv_s[0, c] = r["convs"]
        if j == 3:
            conv_p[0, b] = r["convp"]
        if j == 0:
            mk_p[0, b] = r["memk"].reshape(256, 4, 256)
            mv_p[0, b] = r["memv"].reshape(256, 4, 256)
    return (y_p, y_s, conv_p, k_p, v_p, mk_p, mv_p, conv_s, k_s, v_s)
```

```python
import math
from contextlib import ExitStack

import numpy as np
import concourse.bass as bass
import concourse.mybir as mybir
from concourse.bass_utils import run_bass_kernel_spmd

F32 = mybir.dt.float32
BF16 = mybir.dt.bfloat16
ACT = mybir.ActivationFunctionType
ALU = mybir.AluOpType
AX = mybir.AxisListType

D = 2048
KC = 16
SEQ = 8192
NGRP = 4
DBG_STOP = None
GT = 512
DCONV = 1024
NSAMP = 64
PAST = 4096
NIN = 15360
NEXP = 32
DEXP = 256
EPS = 1e-6
NEG = -30000.0
C_AGT, C_Q, C_K, C_V, C_MQ, C_G = 0, 2048, 4096, 6144, 8192, 9216
STAGE = 2
MOE_SPLIT = True
NPT = 6


class Buf:
    def __init__(self, name):
        self.name = name
        self.w = {}
        self.r = {}
        self.dsem = None
        self.dcount = 0


class Eng:
    def __init__(self, kb, e, name, is_pe=False):
        self.e = e
        self.name = name
        self.sem = kb.newsem("e_" + name)
        self.n = 0
        self.seen = {}
        self.is_pe = is_pe

    def wait(self, sem, val):
        if val <= 0:
            return
        if self.is_pe and sem is self.sem:
            return
        if self.seen.get(sem, 0) >= val:
            return
        self.e.wait_ge(sem, val)
        self.seen[sem] = val


class KB:
    def __init__(self, nc, es):
        self.nc = nc
        self.es = es
        self.sem_es = es
        self.nsem = 0
        self.pe = Eng(self, nc.tensor, "pe", True)
        self.act = Eng(self, nc.scalar, "act")
        self.dve = Eng(self, nc.vector, "dve")
        self.pool = Eng(self, nc.gpsimd, "pool")
        self.sp = Eng(self, nc.sync, "sp")
        self.engs = [self.pe, self.act, self.dve, self.pool, self.sp]
        self.dma_bufs = []

    def newsem(self, name):
        self.nsem += 1
        return self.sem_es.enter_context(self.nc.semaphore(name + "_%d" % self.nsem))

    def sb(self, name, shape, dt):
        self.nsb = getattr(self, "nsb", 0) + 1
        name = "%s_%d" % (name, self.nsb)
        t = self.es.enter_context(self.nc.sbuf_tensor(name, shape, dt))
        return t, Buf(name)

    def _deps(self, eng, reads, writes):
        for b in reads:
            for s, v in b.w.items():
                eng.wait(s, v)
        for b in writes:
            for s, v in b.w.items():
                eng.wait(s, v)
            for s, v in b.r.items():
                eng.wait(s, v)

    def _mark(self, sem, val, reads, writes):
        for b in reads:
            if b.r.get(sem, 0) < val:
                b.r[sem] = val
        for b in writes:
            if b.w.get(sem, 0) < val:
                b.w[sem] = val

    def op(self, eng, fn, reads=(), writes=()):
        self._deps(eng, reads, writes)
        ins = fn(eng.e)
        eng.n += 1
        ins.then_inc(eng.sem, 1)
        self._mark(eng.sem, eng.n, reads, writes)

    def dma(self, q, out, in_, reads, writes, owner, throttle=None):
        self._deps(q, reads, writes)
        if throttle is not None:
            hist, depth = throttle
            if len(hist) >= depth:
                ps_, pv_ = hist[len(hist) - depth]
                q.wait(ps_, pv_)
        if owner.dsem is None:
            owner.dsem = self.newsem("d_" + owner.name)
            self.dma_bufs.append(owner)
        ins = q.e.dma_start(out=out, in_=in_)
        owner.dcount += 16
        ins.then_inc(owner.dsem, 16)
        if throttle is not None:
            throttle[0].append((owner.dsem, owner.dcount))
        self._mark(owner.dsem, owner.dcount, reads, writes)

    def sync_bufs(self, bufs):
        for e in self.engs:
            for b in bufs:
                for sm, v in list(b.w.items()) + list(b.r.items()):
                    e.wait(sm, v)

    def barrier(self):
        for e in self.engs:
            for f in self.engs:
                if f is not e:
                    e.wait(f.sem, f.n)
            for b in self.dma_bufs:
                e.wait(b.dsem, b.dcount)


def build_program():
    nc = bass.Bass("TRN2", target_bir_lowering=False)

    def din(name, shape):
        return nc.dram_tensor(name, list(shape), F32, kind="ExternalInput").ap()

    def dout(name, shape):
        return nc.dram_tensor(name, list(shape), F32, kind="ExternalOutput").ap()

    def dscr(name, shape, dt=BF16):
        return nc.dram_tensor(name, list(shape), dt, kind="Internal").ap()

    xfull = din("xfull", [SEQ, D])
    xhalo = din("xhalo", [NGRP, 32, D])
    gpos = din("gpos", [128, 4])
    xsamp = din("xsamp", [NSAMP, D])
    cconv = din("cconv", [30, DCONV])
    ck = din("ck", [PAST, D])
    cv = din("cv", [PAST, D])
    cmk = din("cmk", [256, 1024])
    cmv = din("cmv", [256, 1024])
    memx = din("memx", [256, D])
    norm_mix = din("norm_mix", [D])
    w_in = din("w_in", [D, NIN])
    w_dw = din("w_dw", [31, DCONV])
    b_dw = din("b_dw", [DCONV])
    ln_g = din("conv_ln_g", [DCONV])
    ln_b = din("conv_ln_b", [DCONV])
    w_co = din("w_conv_out", [DCONV, D])
    lq1 = din("lambda_q1", [64])
    lk1 = din("lambda_k1", [64])
    lq2 = din("lambda_q2", [64])
    lk2 = din("lambda_k2", [64])
    subg = din("subln_g", [128])
    w_ao = din("w_attn_out", [D, D])
    norm_mem = din("norm_mem", [D])
    w_mk = din("w_mem_k", [D, 1024])
    w_mv = din("w_mem_v", [D, 1024])
    w_mo = din("w_mem_out", [1024, D])
    w_o = din("w_out", [D, D])
    norm_ffn = din("norm_ffn", [D])
    w_rg = din("w_router_grp", [D, 4])
    b_rg = din("b_router_grp", [4])
    w_re = din("w_router_exp", [D, 32])
    b_re = din("b_router_exp", [32])
    w_eg = din("w_exp_gate", [NEXP, D, DEXP])
    w_eu = din("w_exp_up", [NEXP, D, DEXP])
    w_ed = din("w_exp_down", [NEXP, DEXP, D])
    norm_final = din("norm_final", [D])

    NTOK = (SEQ // 2048) * GT + NSAMP
    y_o = dout("y", [NTOK, D])
    k_o = dout("kout", [NTOK, D])
    v_o = dout("vout", [NTOK, D])
    convp_o = dout("convp", [30, DCONV])
    convs_o = dout("convs", [30, DCONV])
    memk_o = dout("memk", [256, 1024])
    memv_o = dout("memv", [256, 1024])

    s_win = dscr("s_win", [NIN // 512, 128, KC * 512])
    s_wco = dscr("s_wco", [4, 128, 8 * 512])
    s_wao = dscr("s_wao", [4, 128, KC * 512])
    s_wmk = dscr("s_wmk", [D, 1024])
    s_wmv = dscr("s_wmv", [D, 1024])
    s_wmo = dscr("s_wmo", [4, 128, 8 * 512])
    s_wo = dscr("s_wo", [4, 128, KC * 512])
    s_wr = dscr("s_wr", [D, 36])
    s_egu = dscr("s_egu", [NEXP, 128, 2 * KC * DEXP])
    s_ed = dscr("s_ed", [NEXP, 128, 2 * D])
    NBP = SEQ // 128
    NBS = PAST // 128 + 1
    NGRP_ = SEQ // 2048
    s_ktp = dscr("s_ktp", [16, 128, SEQ])
    s_vp = dscr("s_vp", [16, 128, NBP, 128])
    s_dg = dscr("s_dg", [8, 128, 31 * 128])
    s_kts = dscr("s_kts", [16, 128, NBS * 128])
    s_vs = dscr("s_vs", [16, 128, NBS, 128])

    with ExitStack() as es:
        es.enter_context(nc.allow_non_contiguous_dma(reason="small parameter loads"))
        kb = KB(nc, es)
        pe, act, dve, pool, sp = kb.pe, kb.act, kb.dve, kb.pool, kb.sp

        ps = []
        psb = []
        for i in range(8):
            t = es.enter_context(nc.psum_tensor("ps%d" % i, [128, 512], F32))
            ps.append(t)
            psb.append(Buf("ps%d" % i))
        rr = {"a": 0, "s": 0}

        def next_ps(pool_name="a"):
            if pool_name == "a":
                i = rr["a"] % 8
                rr["a"] += 1
            else:
                i = rr["s"] % 4
                rr["s"] += 1
            return i

        if DBG_STOP == "pm1":
            kb.dma(sp, y_o[0:128, :], xfull[0:128, :], [], [], Buf("t"))
            kb.barrier()
            return nc
        if DBG_STOP == "p0a":
            kb.barrier()
            return nc
        ident, B_c = kb.sb("ident", [128, 128], BF16)
        ones, _ = kb.sb("ones", [128, 128], BF16)
        kb.op(pool, lambda e: e.memset(ident[:], 0.0), [], [B_c])
        kb.op(pool, lambda e: e.affine_select(out=ident[:], in_=ident[:], pattern=[[-1, 128]],
                                              compare_op=ALU.not_equal, fill=1.0, base=0,
                                              channel_multiplier=1), [B_c], [B_c])
        kb.op(pool, lambda e: e.memset(ones[:], 1.0), [], [B_c])

        cols, B_cols = kb.sb("cols", [128, 64], F32)
        kb.op(dve, lambda e: e.memset(cols[:], 0.0), [], [B_cols])
        pfm, B_pfm = kb.sb("pfm", [128, 3, 16], F32)
        for i, src in enumerate((norm_mix, norm_mem, norm_ffn)):
            kb.dma(sp, pfm[:, i, :], src.rearrange("(k p) -> p k", p=128), [], [B_pfm], B_pfm)
        pcv, B_pcv = kb.sb("pcv", [128, 34 + 3, 8], F32)
        kb.dma(sp, pcv[:, 0:31, :], w_dw.rearrange("t (k p) -> p t k", p=128), [], [B_pcv], B_pcv)
        kb.dma(sp, pcv[:, 31, :], b_dw.rearrange("(k p) -> p k", p=128), [], [B_pcv], B_pcv)
        kb.dma(sp, pcv[:, 32, :], ln_g.rearrange("(k p) -> p k", p=128), [], [B_pcv], B_pcv)
        kb.dma(sp, pcv[:, 33, :], ln_b.rearrange("(k p) -> p k", p=128), [], [B_pcv], B_pcv)
        lamt, B_lam = kb.sb("lamt", [128, 4, 64], F32)
        for i, src in enumerate((lq1, lk1, lq2, lk2)):
            kb.dma(sp, lamt[:, i, :], src.partition_broadcast(128), [], [B_lam], B_lam)
        subgc, B_subg = kb.sb("subgc", [128, 1], F32)
        kb.dma(sp, subgc[:, :], subg.rearrange("(p o) -> p o", o=1), [], [B_subg], B_subg)
        gpt, B_gp = kb.sb("gpt", [128, 4], F32)
        kb.dma(sp, gpt[:, :], gpos[:, :], [], [B_gp], B_gp)
        rbias, B_rb = kb.sb("rbias", [128, 36], F32)
        kb.dma(sp, rbias[:, 0:4], b_rg.partition_broadcast(128), [], [B_rb], B_rb)
        kb.dma(sp, rbias[:, 4:36], b_re.partition_broadcast(128), [], [B_rb], B_rb)
        wr_f, B_wrf = kb.sb("wr_f", [128, KC, 36], F32)
        wr_sb, B_wrs = kb.sb("wr_sb", [128, KC, 36], BF16)
        kb.dma(sp, wr_f[:, :, 0:4], w_rg.rearrange("(k p) c -> p k c", p=128), [], [B_wrf], B_wrf)
        kb.dma(sp, wr_f[:, :, 4:36], w_re.rearrange("(k p) c -> p k c", p=128), [], [B_wrf], B_wrf)
        kb.op(dve, lambda e: e.tensor_copy(out=wr_sb[:, :, :], in_=wr_f[:, :, :]), [B_wrf], [B_wrs])

        gbc, B_gbc = kb.sb("gbc", [128, 2, KC, 128], BF16)

        def fill_gbc(dst, src_i):
            for k in range(KC):
                kb.op(dve, lambda e, k=k: e.tensor_copy(out=dst[:, k, :], in_=pfm[:, src_i, k:k + 1].to_broadcast([128, 128])),
                      [B_pfm], [B_gbc])
        fill_gbc(gbc[:, 0, :, :], 0)
        fill_gbc(gbc[:, 1, :, :], 2)
        G_MIX, G_FFN = gbc[:, 0, :, :], gbc[:, 1, :, :]
        lam_init = 0.8 - 0.6 * math.exp(-0.3 * 0)
        ltmp, B_lt = kb.sb("ltmp", [128, 2, 64], F32)
        kb.op(dve, lambda e: e.tensor_tensor(out=ltmp[:, 0, :], in0=lamt[:, 0, :], in1=lamt[:, 1, :], op=ALU.mult),
              [B_lam], [B_lt])
        kb.op(dve, lambda e: e.tensor_tensor(out=ltmp[:, 1, :], in0=lamt[:, 2, :], in1=lamt[:, 3, :], op=ALU.mult),
              [B_lam], [B_lt])
        kb.op(dve, lambda e: e.reduce_sum(out=cols[:, 4:6], in_=ltmp[:, :, :], axis=AX.X), [B_lt], [B_cols])
        kb.op(act, lambda e: e.activation(out=cols[:, 6:8], in_=cols[:, 4:6], func=ACT.Exp), [B_cols], [B_cols])
        kb.op(dve, lambda e: e.tensor_tensor(out=cols[:, 0:1], in0=cols[:, 6:7], in1=cols[:, 7:8], op=ALU.subtract),
              [B_cols], [B_cols])
        kb.op(dve, lambda e: e.tensor_scalar(out=cols[:, 0:1], in0=cols[:, 0:1], scalar1=lam_init, scalar2=None,
                                             op0=ALU.add), [B_cols], [B_cols])
        kb.op(dve, lambda e: e.tensor_scalar(out=cols[:, 1:2], in0=cols[:, 0:1], scalar1=-1.0, scalar2=None,
                                             op0=ALU.mult), [B_cols], [B_cols])
        kb.op(dve, lambda e: e.tensor_scalar(out=cols[:, 2:3], in0=subgc[:, 0:1], scalar1=(1.0 - lam_init),
                                             scalar2=None, op0=ALU.mult), [B_subg, B_cols], [B_cols])
        for m in range(1, 4):
            kb.op(dve, lambda e, m=m: e.tensor_scalar(out=cols[:, 8 + m:9 + m], in0=gpt[:, m:m + 1],
                                                      scalar1=gpt[:, 0:1], scalar2=NEG, op0=ALU.is_gt,
                                                      op1=ALU.mult), [B_gp, B_cols], [B_cols])

        B_win = {}
        cth = ([], 2)
        def blk_cast(dst2d, src_cols, nk, c, b):
            kb.dma(pool, dst2d.rearrange("p (k c) -> p k c", c=c), src_cols.rearrange("(k p) c -> p k c", p=128),
                   [], [b], b, cth)

        for nm, c0, c1 in (("kv", C_K, C_MQ), ("agt", C_AGT, C_Q), ("g0", C_G, C_G + D), ("mq", C_MQ, C_G),
                           ("g2", C_G + 2 * D, NIN), ("q", C_Q, C_K), ("g1", C_G + D, C_G + 2 * D)):
            b = Buf("cw_" + nm)
            B_win[nm] = b
            for cc0 in range(c0, c1, 512):
                blk_cast(s_win[cc0 // 512], w_in[:, cc0:cc0 + 512], KC, 512, b)

        def cast_simple(name, dst, src, rows):
            b = Buf("cw_" + name)
            step = 512
            for r in range(0, rows, step):
                kb.dma(pool, dst[r:r + step, :], src[r:r + step, :], [], [b], b, cth)
            return b

        def cast_blocks(name, dst, src, rows):
            b = Buf("cw_" + name)
            for cb in range(4):
                blk_cast(dst[cb], src[:, cb * 512:(cb + 1) * 512], rows // 128, 512, b)
            return b

        B_wmk = cast_simple("wmk", s_wmk, w_mk, D)
        B_wmv = cast_simple("wmv", s_wmv, w_mv, D)
        B_wco = cast_blocks("wco", s_wco, w_co, DCONV)
        B_wao = cast_blocks("wao", s_wao, w_ao, D)
        B_wmo = cast_blocks("wmo", s_wmo, w_mo, 1024)
        B_wo = cast_blocks("wo", s_wo, w_o, D)
        B_exp = []
        for g8 in range(NEXP // 4):
            b = Buf("cw_e%d" % g8)
            B_exp.append(b)
            for e in range(g8 * 4, g8 * 4 + 4):
                blk_cast(s_egu[e][:, 0:KC * DEXP], w_eg[e], KC, DEXP, b)
                blk_cast(s_egu[e][:, KC * DEXP:2 * KC * DEXP], w_eu[e], KC, DEXP, b)
                blk_cast(s_ed[e], w_ed[e], 2, D, b)

        if DBG_STOP == "p0":
            kb.barrier()
            return nc
        nsts = [kb.sb("nst%d" % i, [128, 2], F32) for i in range(2)]
        nsti = [0]

        def norm_prep(xt, Bx, nt, xn, Bxn):
            nst, B_nst = nsts[nsti[0] % 2]
            nsti[0] += 1
            c_ss = nst[:nt, 0:1]
            c_r = nst[:nt, 1:2]
            kb.op(dve, lambda e: e.memset(c_ss, 0.0), [], [B_nst])
            kb.op(act, lambda e: e.activation(out=xn[:nt, :], in_=xt, func=ACT.Square, accum_out=c_ss),
                  [Bx, B_nst], [Bxn, B_nst])
            kb.op(act, lambda e: e.activation(out=c_r, in_=c_ss, func=ACT.Sqrt, scale=1.0 / D, bias=EPS),
                  [B_nst], [B_nst])
            kb.op(dve, lambda e: e.reciprocal(out=c_r, in_=c_r), [B_nst], [B_nst])
            kb.op(act, lambda e: e.activation(out=xn[:nt, :], in_=xt, func=ACT.Copy, scale=c_r),
                  [Bx, B_nst], [Bxn])

        def norm_tr(nt, gi, hT, BhT, t0, xn, Bxn):
            for half in range(2):
                b = next_ps()
                pv = ps[b][:].bitcast(BF16).rearrange("p (j c) -> p j c", c=128)
                for j in range(8):
                    k = half * 8 + j
                    kb.op(pe, lambda e, j=j, k=k: e.transpose(pv[:, j, :nt], xn[:nt, k * 128:(k + 1) * 128],
                                                              ident[:nt, :nt]), [Bxn, B_c], [psb[b]])
                kb.op(dve, lambda e, half=half, pv=pv: e.tensor_tensor(
                    out=hT[:, half * 8:half * 8 + 8, t0:t0 + nt], in0=pv[:, :, :nt],
                    in1=gi[:, half * 8:half * 8 + 8, :nt], op=ALU.mult), [psb[b], B_gbc], [BhT])

        def rmsnorm_T(xt, Bx, nt, gi, hT, BhT, t0, xn, Bxn, sq, Bsq, col0):
            norm_prep(xt, Bx, nt, xn, Bxn)
            norm_tr(nt, gi, hT, BhT, t0, xn, Bxn)

        def evac(i, out_ap, in_ap, reads, writes, scale=None):
            if i % 2 == 0:
                if scale is None:
                    kb.op(act, lambda e: e.activation(out=out_ap, in_=in_ap, func=ACT.Copy), reads, writes)
                else:
                    kb.op(act, lambda e: e.activation(out=out_ap, in_=in_ap, func=ACT.Copy, scale=scale), reads, writes)
            else:
                if scale is None:
                    kb.op(dve, lambda e: e.tensor_copy(out=out_ap, in_=in_ap), reads, writes)
                else:
                    kb.op(dve, lambda e: e.tensor_scalar(out=out_ap, in0=in_ap, scalar1=scale, scalar2=None,
                                                         op0=ALU.mult), reads, writes)

        def mm_acc(b, out_ap, pairs, reads):
            n = len(pairs)
            for i, (l, r) in enumerate(pairs):
                kb.op(pe, lambda e, l=l, r=r, i=i: e.matmul(out_ap, l, r, start=(i == 0), stop=(i == n - 1)),
                      reads, [psb[b]])

        B_ktp, B_vp, B_kts, B_vs = Buf("ktp"), Buf("vp"), Buf("kts"), Buf("vs")
        dgB = Buf("s_dg")
        with ExitStack() as es0:
            kb_es = kb.es
            kb.es = es0
            B_dg = dgB
            dgs = [kb.sb("dgs%d" % i, [128, 31, 128], BF16) for i in range(2)]
            for cc in range(8):
                dg_, Bdg_ = dgs[cc % 2]
                for k in range(31):
                    kb.op(dve, lambda e, cc=cc, k=k, dg_=dg_: e.tensor_scalar(out=dg_[:, k, :], in0=ident[:, :],
                                                                            scalar1=pcv[:, k, cc:cc + 1], scalar2=None,
                                                                            op0=ALU.mult), [B_c, B_pcv], [Bdg_])
                kb.dma(sp, s_dg[cc], dg_[:, :, :].rearrange("p k c -> p (k c)"), [Bdg_], [B_dg], Bdg_)
            kb.sync_bufs([d_[1] for d_ in dgs])
            kb.es = kb_es
        with ExitStack() as es1:
            kb_es = kb.es
            kb.es = es1
            p1w = [kb.sb("p1w%d" % i, [128, KC, 512], BF16) for i in range(3)]
            xs = [kb.sb("p1x%d" % i, [128, D], F32) for i in range(2)]
            xn1s = [kb.sb("p1xn%d" % i, [128, D], BF16) for i in range(2)]
            xn1, B_xn1 = xn1s[0]
            sq1, B_sq1 = xn1, B_xn1
            CH = 1024
            hTs = [kb.sb("p1hT%d" % i, [128, KC, CH], BF16) for i in range(2)]
            kts = [kb.sb("p1kt%d" % i, [128, 4, GT], BF16) for i in range(2)]
            vss = [kb.sb("p1v%d" % i, [128, 4, 512], BF16) for i in range(2)]
            ckb = [kb.sb("p1ck%d" % i, [128, D], BF16) for i in range(2)]
            ckf = [kb.sb("p1ckf%d" % i, [128, D], F32) for i in range(2)]
            ktc = [kb.sb("p1ktc%d" % i, [128, 16, 256], BF16) for i in range(2)]
            xi = [0]
            ki = [0]
            wq = {"n": 0, "loaded": 0}
            NCHK = SEQ // CH
            NCH = NCHK + 1
            vp_v = s_vp.rearrange("h p nb d -> p h nb d")
            vs_v = s_vs.rearrange("h p nb d -> p h nb d")
            ktp_v = s_ktp.rearrange("h p n -> p h n")
            kts_v = s_kts.rearrange("h p n -> p h n")

            def p1_wload(upto):
                while wq["loaded"] < min(upto, NCH * 8):
                    i = wq["loaded"]
                    blk = i % 8
                    w, Bw = p1w[i % 3]
                    kb.dma(sp, w[:, :, :].rearrange("p k c -> p (k c)"), s_win[C_K // 512 + blk], [B_win["kv"]], [Bw], Bw)
                    wq["loaded"] += 1

            def kv_norm(ci, src_rows, ntok):
                hT, BhT = hTs[ci % 2]
                ntile = (ntok + 127) // 128
                for t in range(ntile):
                    nt = min(128, ntok - t * 128)
                    xt, Bx = xs[xi[0] % 2]
                    xi[0] += 1
                    kb.dma(sp, xt[:nt, :], src_rows[t * 128:t * 128 + nt, :], [], [Bx], Bx)
                    rmsnorm_T(xt[:nt, :], Bx, nt, G_MIX, hT, BhT, t * 128, xn1, B_xn1, sq1, B_sq1, 16)

            cache_nb = [0]
            cache_ld = [0]

            def cache_load():
                nb = cache_ld[0]
                if nb >= PAST // 128:
                    return
                cache_ld[0] += 1
                ct, Bct = ckb[nb % 2]
                cf, Bcf = ckf[nb % 2]
                kb.dma(sp, cf[:, :], ck[nb * 128:(nb + 1) * 128, :], [], [Bcf], Bcf)
                kb.op(act, lambda e, ct=ct, cf=cf: e.activation(out=ct[:, :], in_=cf[:, :], func=ACT.Copy), [Bcf], [Bct])

            def cache_blocks(n):
                for _ in range(n):
                    nb = cache_nb[0]
                    if nb >= PAST // 128:
                        return
                    cache_nb[0] += 1
                    while cache_ld[0] <= nb:
                        cache_load()
                    ct, Bct = ckb[nb % 2]
                    kt, Bk = ktc[(nb // 2) % 2]
                    for half in range(2):
                        b = next_ps()
                        pv = ps[b][:].bitcast(BF16).rearrange("p (j c) -> p j c", c=128)
                        for j in range(8):
                            h = half * 8 + j
                            kb.op(pe, lambda e, j=j, h=h, pv=pv, ct=ct: e.transpose(pv[:, j, :], ct[:, h * 128:(h + 1) * 128],
                                                                                  ident[:, :]), [Bct, B_c], [psb[b]])
                        evac(half, kt[:, half * 8:half * 8 + 8, (nb % 2) * 128:(nb % 2 + 1) * 128], pv[:, :, :],
                             [psb[b]], [Bk])
                    if nb % 2 == 1:
                        kb.dma(sp, kts_v[:, :, (nb - 1) * 128:(nb + 1) * 128], kt[:, :, :], [Bk], [B_kts], Bk)
                    cache_load()

            pend = []

            def norm_sched(ci_next, src_rows, ntok):
                hTn, BhTn = hTs[ci_next % 2]
                ntile = (ntok + 127) // 128
                jobs = []
                for t in range(ntile):
                    nt = min(128, ntok - t * 128)
                    jobs.append((t, nt))

                def prep(t, nt):
                    xt, Bx = xs[xi[0] % 2]
                    xnb, Bxnb = xn1s[xi[0] % 2]
                    xi[0] += 1
                    kb.dma(sp, xt[:nt, :], src_rows[t * 128:t * 128 + nt, :], [], [Bx], Bx)
                    norm_prep(xt[:nt, :], Bx, nt, xnb, Bxnb)
                    return (nt, hTn, BhTn, t * 128, xnb, Bxnb)
                return jobs, prep

            def kv_chunk(ci, ntok, kt_dst_fn, v_dst_fn, Bkt, Bv, nxt=None, ncache=0):
                hT, BhT = hTs[ci % 2]
                jobs, prep = nxt if nxt is not None else ([], None)
                per_blk = (len(jobs) + 7) // 8 if jobs else 0
                ji = 0
                for blk in range(8):
                    while pend:
                        a_ = pend.pop(0)
                        norm_tr(a_[0], G_MIX, a_[1], a_[2], a_[3], a_[4], a_[5])
                    for _ in range(per_blk):
                        if ji < len(jobs):
                            pend.append(prep(*jobs[ji]))
                            ji += 1
                    if ncache and blk % 2 == 1:
                        cache_blocks(ncache)
                    i = wq["n"]
                    wq["n"] += 1
                    p1_wload(i + 2)
                    w, Bw = p1w[i % 3]
                    for c0 in range(0, ntok, GT):
                        nq = min(GT, ntok - c0)
                        ntile = (nq + 127) // 128
                        if blk < 4:
                            kt, Bk = kts[ki[0] % 2]
                            ki[0] += 1
                            for hh in range(4):
                                b = next_ps()
                                mm_acc(b, ps[b][:, :nq], [(w[:, k, hh * 128:(hh + 1) * 128], hT[:, k, c0:c0 + nq])
                                                          for k in range(KC)], [Bw, BhT])
                                evac(hh, kt[:, hh, :nq], ps[b][:, :nq], [psb[b]], [Bk])
                            kb.dma(sp, kt_dst_fn(blk, c0, nq), kt[:, :, :nq], [Bk], [Bkt], Bk)
                        else:
                            cb = blk - 4
                            vs_, Bvs = vss[ki[0] % 2]
                            ki[0] += 1
                            vs4 = vs_[:, :, :].rearrange("p a c -> p (a c)").rearrange("p (h t d) -> p h t d", h=4, t=4)
                            for t in range(ntile):
                                nt = min(128, nq - t * 128)
                                b = next_ps()
                                mm_acc(b, ps[b][:nt, :], [(hT[:, k, c0 + t * 128:c0 + t * 128 + nt], w[:, k, :])
                                                          for k in range(KC)], [Bw, BhT])
                                evac(t, vs4[:nt, :, t, :], ps[b][:nt, :].rearrange("p (h d) -> p h d", d=128), [psb[b]], [Bvs])
                            nt0 = min(128, nq)
                            kb.dma(sp, v_dst_fn(c0 // 128, ntile, nt0, cb).rearrange("p h n d -> p h (n d)"),
                                   vs4[:nt0, :, 0:ntile, :].rearrange("p h t d -> p h (t d)"), [Bvs], [Bv], Bvs)
                while pend:
                    a_ = pend.pop(0)
                    norm_tr(a_[0], G_MIX, a_[1], a_[2], a_[3], a_[4], a_[5])

            kv_norm(0, xfull[0:CH, :], CH)
            cache_load()
            for ci in range(NCHK):
                if ci + 1 < NCHK:
                    mid = norm_sched(ci + 1, xfull[(ci + 1) * CH:(ci + 2) * CH, :], CH)
                else:
                    mid = norm_sched(NCHK, xsamp, NSAMP)
                kv_chunk(ci, CH,
                         lambda blk, c0, nq, ci=ci: ktp_v[:, blk * 4:blk * 4 + 4, ci * CH + c0:ci * CH + c0 + nq],
                         lambda nb0, ntl, nt, cb, ci=ci: vp_v[:nt, cb * 4:cb * 4 + 4, ci * (CH // 128) + nb0:ci * (CH // 128) + nb0 + ntl, :], B_ktp, B_vp, mid,
                         ncache=1)
            kv_chunk(NCHK, NSAMP, lambda blk, c0, nq: kts_v[:, blk * 4:blk * 4 + 4, PAST:PAST + NSAMP],
                     lambda nb0, ntl, nt, cb: vs_v[:nt, cb * 4:cb * 4 + 4, NBS - 1:NBS, :], B_kts, B_vs)
            for nb0 in range(PAST // 128):
                kb.dma(pool, vs_v[:, :, nb0, :],
                       cv[nb0 * 128:(nb0 + 1) * 128, :].rearrange("p (h d) -> p h d", d=128),
                       [], [B_vs], B_vs)
            cache_blocks(PAST // 128)
            kb.sync_bufs([x_[1] for x_ in (p1w + xs + xn1s + hTs + kts + vss + ckb + ckf + ktc)])
            kb.es = kb_es

        if DBG_STOP == "p1":
            return nc
        mkT_p, B_mkp = kb.sb("mkT_p", [128, 8, 256], BF16)
        mv_p, B_mvp = kb.sb("mv_p", [128, 2, 1024], BF16)

        xn, B_xn = kb.sb("xn", [128, D], BF16)
        sq, B_sq = xn, B_xn
        ostage, B_ost = kb.sb("ostage", [128, 2, 512], F32)
        with ExitStack() as es2:
            kb_es = kb.es
            kb.es = es2
            mx, B_mx = kb.sb("mx", [128, 2, D], F32)
            gmem, _ = kb.sb("gmem", [128, KC, 128], BF16)
            fill_gbc(gmem, 1)
            hmT, B_hmT = kb.sb("hmT", [128, KC, 256], BF16)
            wm, B_wm = kb.sb("wm", [128, KC, 1024], BF16)
            for t in range(2):
                kb.dma(sp, mx[:, t, :], memx[t * 128:(t + 1) * 128, :], [], [B_mx], B_mx)
            for t in range(2):
                rmsnorm_T(mx[:, t, :], B_mx, 128, gmem[:, :, :], hmT, B_hmT, t * 128, xn, B_xn, sq, B_sq, 16)
            if DBG_STOP == "p2a":
                kb.barrier()
                return nc
            for which, (sw, Bsw, outd) in enumerate(((s_wmk, B_wmk, memk_o), (s_wmv, B_wmv, memv_o))):
                if DBG_STOP == "p2b" and which == 1:
                    kb.barrier()
                    return nc
                kb.dma(sp, wm[:, :, :], sw.rearrange("(k p) c -> p k c", p=128), [Bsw], [B_wm], B_wm)
                if which == 0:
                    for cc in range(8):
                        b = next_ps()
                        mm_acc(b, ps[b][:, :256], [(wm[:, k, cc * 128:(cc + 1) * 128], hmT[:, k, :]) for k in range(KC)],
                               [B_wm, B_hmT])
                        evac(cc, mkT_p[:, cc, :], ps[b][:, :256], [psb[b]], [B_mkp])
                for t in range(2):
                    for cb in range(2):
                        b = next_ps()
                        mm_acc(b, ps[b][:, :], [(hmT[:, k, t * 128:(t + 1) * 128], wm[:, k, cb * 512:(cb + 1) * 512])
                                                for k in range(KC)], [B_wm, B_hmT])
                        evac(0, ostage[:, cb, :], ps[b][:, :], [psb[b]], [B_ost])
                        if which == 1:
                            kb.op(dve, lambda e, t=t, cb=cb: e.tensor_copy(out=mv_p[:, t, cb * 512:(cb + 1) * 512],
                                                                           in_=ostage[:, cb, :]), [B_ost], [B_mvp])
                        kb.dma(pool, outd[t * 128:(t + 1) * 128, cb * 512:(cb + 1) * 512], ostage[:, cb, :], [B_ost], [], B_ost)
            if DBG_STOP == "p2c":
                kb.barrier()
                return nc
            kb.sync_bufs([B_mx, B_hmT, B_wm, B_gbc])
            kb.es = kb_es

        xg, B_xg = kb.sb("xg", [128, 4, D], F32)
        hT, B_hT = kb.sb("hT", [128, KC, GT], BF16)
        NSLOT = 2
        wsl = [kb.sb("ws%d" % i, [128, 8192], BF16) for i in range(NSLOT)]
        merged, B_mg = kb.sb("merged", [128, KC, GT], BF16)
        sig, B_sig = kb.sb("sig", [128, 4, GT], F32)
        tmpf, B_tmpf = kb.sb("tmpf", [128, 2, GT], F32)


        if DBG_STOP == "p2":
            return nc
        steps = []

        def run_steps():
            emitted = [0]

            def emit_load(i):
                loads = steps[i][0]
                if not loads:
                    return
                sl, Bsl = wsl[steps[i][2] % NSLOT]
                for off, src, nk, c, Bsrc in loads:
                    kb.dma(sp, sl[:, off:off + nk * c], src, [Bsrc], [Bsl], Bsl)

            widx = 0
            for i, st in enumerate(steps):
                if st[0]:
                    steps[i] = (st[0], st[1], widx)
                    widx += 1
                else:
                    steps[i] = (st[0], st[1], -1)
            n = len(steps)
            nxt = 0
            for i in range(n):
                ahead = 0
                j = i
                while j < n and ahead < NSLOT - 1:
                    if steps[j][0]:
                        ahead += 1
                        if j >= nxt:
                            emit_load(j)
                            nxt = j + 1
                    j += 1
                nxt = max(nxt, i + 1) if not steps[i][0] else nxt
                if steps[i][0]:
                    sl, Bsl = wsl[steps[i][2] % NSLOT]
                    steps[i][1](sl, Bsl)
                else:
                    steps[i][1](None, None)

        def group(gi, NQ, x_src, row0, halo_kind, mkT, B_mk, mv, B_mv, kt_scr, B_kt, v_scr, B_v, blocks, conv_out):
            ntile = (NQ + 127) // 128
            tl = [(t, min(128, NQ - t * 128)) for t in range(ntile)]
            st = {}

            def s_load(_, __):
                for t, nt in tl:
                    kb.dma(sp, xg[:nt, t, :], x_src[t * 128:t * 128 + nt, :], [], [B_xg], B_xg)
                for t, nt in tl:
                    rmsnorm_T(xg[:nt, t, :], B_xg, nt, G_MIX, hT, B_hT, t * 128, xn, B_xn, sq, B_sq, 16)
            steps.append(([], s_load))

            for which, c0, outd in ((0, C_K, k_o), (1, C_V, v_o)):
                for cb in range(4):
                    def s_kvout(sl, Bsl, cb=cb, outd=outd):
                        w = sl[:, 0:KC * 512].rearrange("p (k c) -> p k c", c=512)
                        for t, nt in tl:
                            b = next_ps()
                            mm_acc(b, ps[b][:nt, :], [(hT[:, k, t * 128:t * 128 + nt], w[:, k, :]) for k in range(KC)],
                                   [Bsl, B_hT])
                            evac(t, ostage[:nt, t % 2, 0:512], ps[b][:nt, :], [psb[b]], [B_ost])
                            kb.dma(pool, outd[row0 + t * 128:row0 + t * 128 + nt, cb * 512:(cb + 1) * 512],
                                   ostage[:nt, t % 2, 0:512], [B_ost], [], B_ost)
                    steps.append(([(0, s_win[c0 // 512 + cb], KC, 512, B_win["kv"])], s_kvout))

            if STAGE < 2:
                return
            is_s = (halo_kind == "cache")
            sc = {}

            def open_scope():
                kb.barrier()
                sc["es"] = ExitStack()
                sc["old"] = kb.es
                kb.es = sc["es"]

            def close_scope(_=None, __=None):
                kb.barrier()
                kb.es = sc["old"]
                sc["es"].close()

            def wstep(src, nk, Bsrc, fn, c=512):
                steps.append(([(0, src, nk, c, Bsrc)], fn))

            def branch_out(bidx, gname, wsrc_fn, wk_n, Bw, rhs_key):
                for cb in range(4):
                    def s_gate(sl, Bsl, cb=cb):
                        w = sl[:, 0:KC * 512].rearrange("p (k c) -> p k c", c=512)
                        for j in range(4):
                            b = next_ps()
                            mm_acc(b, ps[b][:, :NQ], [(w[:, k, j * 128:(j + 1) * 128], hT[:, k, :NQ]) for k in range(KC)],
                                   [Bsl, B_hT])
                            kb.op(act, lambda e, j=j, b=b: e.activation(out=sig[:, j, :NQ], in_=ps[b][:, :NQ],
                                                                        func=ACT.Sigmoid), [psb[b]], [B_sig])
                    wstep(s_win[(C_G + bidx * D) // 512 + cb], KC, B_win[gname], s_gate)

                    def s_y(sl, Bsl, cb=cb):
                        rhsT, BrhsT = sc[rhs_key]
                        w = sl[:, 0:wk_n * 512].rearrange("p (k c) -> p k c", c=512)
                        for j in range(4):
                            c = cb * 4 + j
                            b = next_ps()
                            mm_acc(b, ps[b][:, :NQ], [(w[:, k, j * 128:(j + 1) * 128], rhsT[:, k, :NQ]) for k in range(wk_n)],
                                   [Bsl, BrhsT])
                            if bidx == 0:
                                kb.op(dve, lambda e, j=j, b=b, c=c: e.tensor_tensor(out=merged[:, c, :NQ], in0=ps[b][:, :NQ],
                                                                                  in1=sig[:, j, :NQ], op=ALU.mult),
                                      [psb[b], B_sig], [B_mg])
                            else:
                                kb.op(dve, lambda e, j=j, b=b: e.tensor_tensor(out=tmpf[:, j % 2, :NQ], in0=ps[b][:, :NQ],
                                                                               in1=sig[:, j, :NQ], op=ALU.mult),
                                      [psb[b], B_sig], [B_tmpf])
                                kb.op(dve, lambda e, j=j, c=c: e.tensor_tensor(out=merged[:, c, :NQ], in0=merged[:, c, :NQ],
                                                                               in1=tmpf[:, j % 2, :NQ], op=ALU.add),
                                      [B_tmpf, B_mg], [B_mg])
                    wstep(wsrc_fn(cb), wk_n, Bw, s_y)

            def s_conv_open(_, __):
                open_scope()
                sc["uext"] = kb.sb("uext", [128, 8, 32 + GT], F32)
                sc["cacc"] = kb.sb("cacc", [128, 8, GT], F32)
                sc["ub"] = kb.sb("ub", [128, 8, 32 + GT], BF16)
                sc["zT"] = kb.sb("zT", [128, 8, GT], BF16)
                sc["hh"] = kb.sb("hh", [128, KC, 32], BF16)
                sc["st"] = kb.sb("cst", [128, 3, GT], F32)
                sc["identf"] = kb.sb("identf", [128, 128], F32)
                uext, B_u = sc["uext"]
                hh, B_hh = sc["hh"]
                cacc_, B_xh = sc["cacc"]
                xh = cacc_[:, 0:4, :].rearrange("p a c -> p (a c)")
                identf, B_if = sc["identf"]
                kb.op(dve, lambda e: e.tensor_copy(out=identf[:, :], in_=ident[:, :]), [B_c], [B_if])
                if not is_s:
                    kb.dma(sp, xh[:32, :], xhalo[gi], [], [B_xh], B_xh)
                    rmsnorm_T(xh[:32, :], B_xh, 32, G_MIX, hh, B_hh, 0, xn, B_xn, sq, B_sq, 16)
                else:
                    kb.op(dve, lambda e: e.memset(xh[:32, 0:DCONV], 0.0), [], [B_xh])
                    kb.dma(sp, xh[2:32, 0:DCONV], cconv[:, :], [], [B_xh], B_xh)
                    kb.op(act, lambda e: e.activation(out=xn[:32, 0:DCONV], in_=xh[:32, 0:DCONV], func=ACT.Copy),
                          [B_xh], [B_xn])
                    b = next_ps()
                    pv = ps[b][:].bitcast(BF16).rearrange("p (j c) -> p j c", c=128)
                    for cc in range(8):
                        kb.op(pe, lambda e, cc=cc, pv=pv: e.transpose(pv[:, cc, :32], xn[:32, cc * 128:(cc + 1) * 128],
                                                                      ident[:32, :32]), [B_xn, B_c], [psb[b]])
                    kb.op(dve, lambda e, pv=pv: e.tensor_copy(out=uext[:, :, 0:32], in_=pv[:, :, :32]), [psb[b]], [B_u])
            steps.append(([], s_conv_open))

            for half_i, cbase in ((0, C_AGT), (1, C_AGT + DCONV)):
                for cb in range(2):
                    def s_agt(sl, Bsl, half_i=half_i, cb=cb):
                        uext, B_u = sc["uext"]
                        hh, B_hh = sc["hh"]
                        w = sl[:, 0:KC * 512].rearrange("p (k c) -> p k c", c=512)
                        for j in range(4):
                            cc = cb * 4 + j
                            parts = [(32, NQ, hT, B_hT)]
                            if not is_s:
                                parts.append((0, 32, hh, B_hh))
                            for (o0, n, src, Bs) in parts:
                                b = next_ps()
                                mm_acc(b, ps[b][:, :n], [(w[:, k, j * 128:(j + 1) * 128], src[:, k, :n]) for k in range(KC)],
                                       [Bsl, Bs])
                                if half_i == 0:
                                    evac(j, uext[:, cc, o0:o0 + n], ps[b][:, :n], [psb[b]], [B_u])
                                else:
                                    kb.op(act, lambda e, b=b, n=n: e.activation(out=tmpf[:, 0, :n], in_=ps[b][:, :n],
                                                                                func=ACT.Sigmoid), [psb[b]], [B_tmpf])
                                    kb.op(dve, lambda e, cc=cc, o0=o0, n=n: e.tensor_tensor(
                                        out=uext[:, cc, o0:o0 + n], in0=uext[:, cc, o0:o0 + n], in1=tmpf[:, 0, :n],
                                        op=ALU.mult), [B_tmpf, B_u], [B_u])
                    wstep(s_win[cbase // 512 + cb], KC, B_win["agt"], s_agt)

            def s_conv_pre(_, __):
                uext, B_u = sc["uext"]
                identf, B_if = sc["identf"]
                ub, B_ub = sc["ub"]
                xh, B_xh = ostage[:, :, :].rearrange("p a c -> p (a c)"), B_ost
                kb.op(act, lambda e: e.activation(out=ub[:, :, 0:32 + NQ], in_=uext[:, :, 0:32 + NQ], func=ACT.Copy),
                      [B_u], [B_ub])
                if conv_out is not None:
                    for half in range(2):
                        b = next_ps()
                        for j in range(4):
                            cc = half * 4 + j
                            kb.op(pe, lambda e, cc=cc, b=b, j=j: e.transpose(ps[b][:32, j * 128:(j + 1) * 128],
                                                                            uext[:, cc, NQ:NQ + 32], identf[:, :]),
                                  [B_u, B_if], [psb[b]])
                        evac(half, xh[:32, half * 512:(half + 1) * 512], ps[b][:32, :], [psb[b]], [B_xh])
                    kb.dma(pool, conv_out[:, :], xh[2:32, 0:DCONV], [B_xh], [], B_xh)
            steps.append(([], s_conv_pre))
            for cc in range(8):
                def s_dw(sl, Bsl, cc=cc):
                    ub, B_ub = sc["ub"]
                    cacc, B_ca = sc["cacc"]
                    b = next_ps()
                    mm_acc(b, ps[b][:, :NQ], [(sl[:, k * 128:(k + 1) * 128], ub[:, cc, 2 + k:2 + k + NQ]) for k in range(31)],
                           [Bsl, B_ub])
                    kb.op(act, lambda e, b=b: e.activation(out=cacc[:, cc, :NQ], in_=ps[b][:, :NQ], func=ACT.Identity,
                                                           bias=pcv[:, 31, cc:cc + 1]), [psb[b], B_pcv], [B_ca])
                steps.append(([(0, s_dg[cc], 1, 31 * 128, dgB)], s_dw))

            def s_conv(_, __):
                uext, B_u = sc["uext"]
                cacc, B_ca = sc["cacc"]
                zT, B_z = sc["zT"]
                cst, B_st = sc["st"]
                ysq, B_ysq = sc["ub"]
                kb.op(act, lambda e: e.activation(out=zT[:, :, :NQ], in_=cacc[:, :, :NQ], func=ACT.Copy), [B_ca], [B_z])
                kb.op(act, lambda e: e.activation(out=ysq[:, :, :NQ], in_=cacc[:, :, :NQ], func=ACT.Square), [B_ca], [B_ysq])
                b1 = next_ps()
                mm_acc(b1, ps[b1][:, :NQ], [(ones[:, :], zT[:, k, :NQ]) for k in range(8)], [B_z, B_c])
                b2 = next_ps()
                mm_acc(b2, ps[b2][:, :NQ], [(ones[:, :], ysq[:, k, :NQ]) for k in range(8)], [B_ysq, B_c])
                mean, msq, var = cst[:, 0, :NQ], cst[:, 1, :NQ], cst[:, 2, :NQ]
                kb.op(dve, lambda e: e.tensor_scalar(out=mean, in0=ps[b1][:, :NQ], scalar1=1.0 / DCONV, scalar2=None,
                                                     op0=ALU.mult), [psb[b1]], [B_st])
                kb.op(dve, lambda e: e.tensor_tensor(out=msq, in0=mean, in1=mean, op=ALU.mult), [B_st], [B_st])
                kb.op(dve, lambda e: e.scalar_tensor_tensor(out=var, in0=ps[b2][:, :NQ], scalar=1.0 / DCONV, in1=msq,
                                                            op0=ALU.mult, op1=ALU.subtract), [psb[b2], B_st], [B_st])
                kb.op(act, lambda e: e.activation(out=var, in_=var, func=ACT.Sqrt, bias=EPS), [B_st], [B_st])
                kb.op(dve, lambda e: e.reciprocal(out=var, in_=var), [B_st], [B_st])
                for cc in range(8):
                    kb.op(dve, lambda e, cc=cc: e.tensor_tensor(out=cacc[:, cc, :NQ], in0=cacc[:, cc, :NQ], in1=mean,
                                                                op=ALU.subtract), [B_st, B_ca], [B_ca])
                    kb.op(dve, lambda e, cc=cc: e.tensor_tensor(out=cacc[:, cc, :NQ], in0=cacc[:, cc, :NQ], in1=var,
                                                                op=ALU.mult), [B_st, B_ca], [B_ca])
                    kb.op(act, lambda e, cc=cc: e.activation(out=zT[:, cc, :NQ], in_=cacc[:, cc, :NQ], func=ACT.Silu,
                                                             scale=pcv[:, 32, cc:cc + 1], bias=pcv[:, 33, cc:cc + 1]),
                          [B_ca, B_pcv, B_z], [B_z])
            steps.append(([], s_conv))
            branch_out(0, "g0", lambda cb: s_wco[cb], 8, B_wco, "zT")
            steps.append(([], close_scope))

            def s_mem_open(_, __):
                open_scope()
                sc["mqT"] = kb.sb("mqT", [128, 8, GT], BF16)
                sc["moT"] = kb.sb("moT", [128, 8, GT], BF16)
                sc["mP"] = kb.sb("mP", [128, 2, GT], BF16)
                sc["mr"] = kb.sb("mr", [128, GT], F32)
            steps.append(([], s_mem_open))
            for cb in range(2):
                def s_mq(sl, Bsl, cb=cb):
                    mqT, B_mq = sc["mqT"]
                    w = sl[:, 0:KC * 512].rearrange("p (k c) -> p k c", c=512)
                    for j in range(4):
                        b = next_ps()
                        mm_acc(b, ps[b][:, :NQ], [(w[:, k, j * 128:(j + 1) * 128], hT[:, k, :NQ]) for k in range(KC)],
                               [Bsl, B_hT])
                        evac(j, mqT[:, cb * 4 + j, :NQ], ps[b][:, :NQ], [psb[b]], [B_mq], scale=1.0 / 16.0)
                wstep(s_win[C_MQ // 512 + cb], KC, B_win["mq"], s_mq)

            def s_mem(_, __):
                mqT, B_mq = sc["mqT"]
                moT, B_mo = sc["moT"]
                mP, B_mP = sc["mP"]
                mr, B_mr = sc["mr"]
                for hd in range(4):
                    for mt in range(2):
                        b = next_ps()
                        mm_acc(b, ps[b][:, :NQ], [(mkT[:, hd * 2 + hf, mt * 128:(mt + 1) * 128], mqT[:, hd * 2 + hf, :NQ])
                                                  for hf in range(2)], [B_mk, B_mq])
                        kb.op(act, lambda e, b=b, mt=mt: e.activation(out=mP[:, mt, :NQ], in_=ps[b][:, :NQ], func=ACT.Exp),
                              [psb[b]], [B_mP])
                    bl = next_ps()
                    mm_acc(bl, ps[bl][:, :NQ], [(ones[:, :], mP[:, mt, :NQ]) for mt in range(2)], [B_mP, B_c])
                    kb.op(dve, lambda e, bl=bl: e.reciprocal(out=mr[:, :NQ], in_=ps[bl][:, :NQ]), [psb[bl]], [B_mr])
                    for dh in range(2):
                        b = next_ps()
                        mm_acc(b, ps[b][:, :NQ], [(mv[:, mt, hd * 256 + dh * 128:hd * 256 + (dh + 1) * 128], mP[:, mt, :NQ])
                                                  for mt in range(2)], [B_mv, B_mP])
                        kb.op(dve, lambda e, b=b, hd=hd, dh=dh: e.tensor_tensor(out=moT[:, hd * 2 + dh, :NQ], in0=ps[b][:, :NQ],
                                                                               in1=mr[:, :NQ], op=ALU.mult),
                              [psb[b], B_mr], [B_mo])
            steps.append(([], s_mem))
            branch_out(2, "g2", lambda cb: s_wmo[cb], 8, B_wmo, "moT")
            steps.append(([], close_scope))

            def s_att_open(_, __):
                open_scope()
                sc["qT"] = kb.sb("qT", [128, 16, GT], BF16)
                sc["onT"] = kb.sb("onT", [128, 16, GT], BF16)
                sc["kt"] = [kb.sb("akt%d" % i, [128, 2048], BF16) for i in range(2)]
                sc["vv"] = [kb.sb("avv%d" % i, [128, 16, 128], BF16) for i in range(2)]
                sc["P"] = [kb.sb("aP%d" % i, [128, GT], BF16) for i in range(NPT)]
                sc["fin"] = kb.sb("afin", [128, 4, GT], F32)
                sc["osq"] = kb.sb("aosq", [128, GT], BF16)
                sc["accP"] = kb.sb("accP", [128, GT], F32)
                sc["aphl"] = kb.sb("aphl", [128, 2, GT], BF16)
            steps.append(([], s_att_open))
            for cb in range(4):
                def s_q(sl, Bsl, cb=cb):
                    qT, B_q = sc["qT"]
                    w = sl[:, 0:KC * 512].rearrange("p (k c) -> p k c", c=512)
                    for j in range(4):
                        b = next_ps()
                        mm_acc(b, ps[b][:, :NQ], [(w[:, k, j * 128:(j + 1) * 128], hT[:, k, :NQ]) for k in range(KC)],
                               [Bsl, B_hT])
                        evac(j, qT[:, cb * 4 + j, :NQ], ps[b][:, :NQ], [psb[b]], [B_q], scale=0.125)
                wstep(s_win[C_Q // 512 + cb], KC, B_win["q"], s_q)

            segs = []
            if not is_s:
                for s_ in range(gi):
                    segs.append((s_ * 2048, [(128, "full", None)] * 16))
                dblk = [(128, "diag", kb_) for kb_ in range(4)] + [(128, "bias", 1 + (bk - 4) // 4) for bk in range(4, 16)]
                segs.append((gi * 2048, dblk))
            else:
                nb_tot = PAST // 128
                k0 = 0
                while nb_tot > 0:
                    n = min(16, nb_tot)
                    segs.append((k0, [(128, "full", None)] * n))
                    k0 += n * 128
                    nb_tot -= n
                if len(segs[-1][1]) < 16:
                    segs[-1][1].append((NSAMP, "full", None))
                else:
                    segs.append((k0, [(NSAMP, "full", None)]))

            def s_att(_, __):
                qT, B_q = sc["qT"]
                onT, B_on = sc["onT"]
                fin, B_fin = sc["fin"]
                osq, B_osq = sc["osq"]
                ktv = kt_scr
                vvv = v_scr
                li = [0]
                pi = [0]
                nblk_all = sum(len(s_[1]) for s_ in segs)
                bO = [4, 5, 6, 7]
                r1, r2, t1, o = fin[:, 0, :NQ], fin[:, 1, :NQ], fin[:, 2, :NQ], fin[:, 3, :NQ]

                items = []
                for h in range(16):
                    bi = 0
                    for (k0, blks) in segs:
                        for bl, (kn, kind, arg) in enumerate(blks):
                            items.append(dict(h=h, k0=k0, blks=blks, bl=bl, kn=kn, kind=kind, arg=arg,
                                              first=(bi == 0), last=(bi == nblk_all - 1), bi=bi))
                            bi += 1

                def emit_S(it):
                    h = it["h"]
                    if it["bl"] == 0:
                        ktt, B_ktt = sc["kt"][li[0] % 2]
                        vvt, B_vvt = sc["vv"][li[0] % 2]
                        li[0] += 1
                        blks, k0 = it["blks"], it["k0"]
                        nk = sum(b_[0] for b_ in blks)
                        nfull = sum(1 for b_ in blks if b_[0] == 128)
                        kb.dma(sp, ktt[:, :nk], ktv[h, :, k0:k0 + nk], [B_kt], [B_ktt], B_ktt)
                        if nfull:
                            kb.dma(sp, vvt[:, :nfull, :], vvv[h, :, k0 // 128:k0 // 128 + nfull, :], [B_v], [B_vvt], B_vvt)
                        if nfull < len(blks):
                            kn_ = blks[-1][0]
                            kb.dma(sp, vvt[:kn_, nfull, :], vvv[h, :kn_, k0 // 128 + nfull, :], [B_v], [B_vvt], B_vvt)
                        cur["kt"] = (ktt, B_ktt)
                        cur["vv"] = (vvt, B_vvt)
                    ktt, B_ktt = cur["kt"]
                    it["vv"] = cur["vv"]
                    kn, kind, arg, bl = it["kn"], it["kind"], it["arg"], it["bl"]
                    c0 = 128 * arg if kind == "diag" else 0
                    it["c0"] = c0
                    it["P"] = []
                    for mp in range(2):
                        bS = next_ps("s")
                        p0 = mp * 64
                        kb.op(pe, lambda e, bS=bS, p0=p0: e.matmul(
                            ps[bS][:kn, c0:NQ], ktt[p0:p0 + 64, bl * 128:bl * 128 + kn], qT[p0:p0 + 64, h, c0:NQ],
                            start=True, stop=True), [B_ktt, B_q], [psb[bS]])
                        Pt, B_P = sc["P"][pi[0] % NPT]
                        pi[0] += 1
                        it["P"].append((Pt, B_P))
                        if kind == "bias":
                            kb.op(act, lambda e, bS=bS, Pt=Pt: e.activation(
                                out=Pt[:kn, c0:NQ], in_=ps[bS][:kn, c0:NQ], func=ACT.Exp,
                                bias=cols[:kn, 8 + arg:9 + arg]), [psb[bS], B_cols], [B_P])
                        else:
                            kb.op(act, lambda e, bS=bS, Pt=Pt: e.activation(
                                out=Pt[:kn, c0:NQ], in_=ps[bS][:kn, c0:NQ], func=ACT.Exp), [psb[bS]], [B_P])
                        if kind == "diag":
                            kb.op(pool, lambda e, Pt=Pt: e.memset(Pt[64:128, c0:c0 + 64], 0.0), [], [B_P])

                def emit_PV(it):
                    kn, bl, c0, first, last = it["kn"], it["bl"], it["c0"], it["first"], it["last"]
                    vvt, B_vvt = it["vv"]
                    accP, B_aP = sc["accP"]
                    for mp in range(2):
                        Pt, B_P = it["P"][mp]
                        kb.op(pe, lambda e, mp=mp, Pt=Pt: e.matmul(
                            ps[bO[2 * mp]][:, c0:NQ], vvt[:kn, bl, :], Pt[:kn, c0:NQ], start=first, stop=last),
                            [B_vvt, B_P], [psb[bO[2 * mp]]])
                        if mp == 0:
                            if first:
                                kb.op(dve, lambda e: e.memset(accP[:, :NQ], 0.0), [], [B_aP])
                            kb.op(dve, lambda e, Pt=Pt: e.tensor_tensor(out=accP[:kn, c0:NQ], in0=accP[:kn, c0:NQ],
                                                                        in1=Pt[:kn, c0:NQ], op=ALU.add), [B_P, B_aP], [B_aP])
                        else:
                            kb.op(pe, lambda e, mp=mp, Pt=Pt: e.matmul(
                                ps[bO[2 * mp + 1]][:, c0:NQ], ones[:kn, :], Pt[:kn, c0:NQ], start=first, stop=last),
                                [B_c, B_P], [psb[bO[2 * mp + 1]]])

                def fin1():
                    accP, B_aP = sc["accP"]
                    aphl, B_hl = sc["aphl"]
                    kb.op(dve, lambda e: e.reciprocal(out=r2, in_=ps[7][:, :NQ]), [psb[7]], [B_fin])
                    kb.op(dve, lambda e: e.tensor_tensor(out=r2, in0=ps[6][:, :NQ], in1=r2, op=ALU.mult), [psb[6], B_fin], [B_fin])
                    kb.op(dve, lambda e: e.tensor_copy(out=t1, in_=ps[4][:, :NQ]), [psb[4]], [B_fin])
                    kb.op(act, lambda e: e.activation(out=aphl[:, 0, :NQ], in_=accP[:, :NQ], func=ACT.Copy), [B_aP], [B_hl])
                    kb.op(dve, lambda e: e.tensor_tensor(out=aphl[:, 1, :NQ], in0=accP[:, :NQ], in1=aphl[:, 0, :NQ],
                                                         op=ALU.subtract), [B_aP, B_hl], [B_hl])

                def fin1b():
                    aphl, B_hl = sc["aphl"]
                    kb.op(pe, lambda e: e.matmul(ps[5][:, :NQ], ones[:, :], aphl[:, 0, :NQ], start=True, stop=False),
                          [B_c, B_hl], [psb[5]])
                    kb.op(pe, lambda e: e.matmul(ps[5][:, :NQ], ones[:, :], aphl[:, 1, :NQ], start=False, stop=True),
                          [B_c, B_hl], [psb[5]])
                    kb.op(dve, lambda e: e.reciprocal(out=r1, in_=ps[5][:, :NQ]), [psb[5]], [B_fin])
                    kb.op(dve, lambda e: e.tensor_tensor(out=t1, in0=t1, in1=r1, op=ALU.mult), [B_fin], [B_fin])
                    kb.op(dve, lambda e: e.scalar_tensor_tensor(out=o, in0=r2, scalar=cols[:, 1:2], in1=t1, op0=ALU.mult,
                                                                op1=ALU.add), [B_fin, B_cols], [B_fin])
                    kb.op(act, lambda e: e.activation(out=osq[:, :NQ], in_=o, func=ACT.Square), [B_fin], [B_osq])

                def fin2(h):
                    bs_ = next_ps("s")
                    kb.op(pe, lambda e, bs_=bs_: e.matmul(ps[bs_][:, :NQ], ones[:, :], osq[:, :NQ], start=True, stop=True),
                          [B_osq, B_c], [psb[bs_]])
                    kb.op(act, lambda e, bs_=bs_: e.activation(out=r1, in_=ps[bs_][:, :NQ], func=ACT.Sqrt, scale=1.0 / 128,
                                                               bias=1e-5), [psb[bs_]], [B_fin])
                    kb.op(dve, lambda e: e.reciprocal(out=r1, in_=r1), [B_fin], [B_fin])
                    kb.op(dve, lambda e, h=h: e.scalar_tensor_tensor(out=onT[:, h, :NQ], in0=o, scalar=cols[:, 2:3], in1=r1,
                                                                     op0=ALU.mult, op1=ALU.mult), [B_fin, B_cols], [B_on])

                cur = {}
                n_it = len(items)
                pend_fin2 = None
                emit_S(items[0])
                for i in range(n_it):
                    it = items[i]
                    if i + 1 < n_it:
                        emit_S(items[i + 1])
                    emit_PV(it)
                    if pend_fin2 is not None and (it["bi"] >= min(1, nblk_all - 1)):
                        fin1b()
                        fin2(pend_fin2)
                        pend_fin2 = None
                    if it["last"]:
                        fin1()
                        pend_fin2 = it["h"]
                if pend_fin2 is not None:
                    fin1b()
                    fin2(pend_fin2)
            steps.append(([], s_att))
            branch_out(1, "g1", lambda cb: s_wao[cb], KC, B_wao, "onT")
            steps.append(([], close_scope))

            sc["merged"] = (merged, B_mg)
            for cb in range(4):
                def s_wout(sl, Bsl, cb=cb):
                    w = sl[:, 0:KC * 512].rearrange("p (k c) -> p k c", c=512)
                    for t, nt in tl:
                        b = next_ps()
                        mm_acc(b, ps[b][:nt, :], [(merged[:, k, t * 128:t * 128 + nt], w[:, k, :]) for k in range(KC)],
                               [Bsl, B_mg])
                        kb.op(dve, lambda e, b=b, t=t, nt=nt: e.tensor_tensor(
                            out=xg[:nt, t, cb * 512:(cb + 1) * 512], in0=xg[:nt, t, cb * 512:(cb + 1) * 512],
                            in1=ps[b][:nt, :], op=ALU.add), [psb[b], B_xg], [B_xg])
                wstep(s_wo[cb], KC, B_wo, s_wout)

            def s_moe_open(_, __):
                open_scope()
                sc["acc"] = kb.sb("macc", [128, 4, D], F32)
                sc["aT"] = [kb.sb("maT%d" % i, [128, 2, GT], BF16) for i in range(2)]
                sc["sg"] = [kb.sb("msg%d" % i, [128, GT], F32) for i in range(2)]
                sc["comb"] = kb.sb("mcomb", [128, 4, 32], F32)
                sc["mtmp"] = [kb.sb("mtmp%d" % i, [128, 512], F32) for i in range(3)]
                sc["rt"] = [kb.sb("mrt%d" % i, [128, 96], F32) for i in range(4)]
                sc["gfin"] = kb.sb("gfin", [128, D], F32)
                acc, B_acc = sc["acc"]
                sc["accp"] = Buf("accp")
                comb, B_comb = sc["comb"]
                gfin, B_gfin = sc["gfin"]
                kb.dma(sp, gfin[:, :], norm_final.partition_broadcast(128), [], [B_gfin], B_gfin)
                kb.op(pool, lambda e: e.memset(acc[:, :, :], 0.0), [], [B_acc, sc["accp"]])
                for t, nt in tl:
                    rmsnorm_T(xg[:nt, t, :], B_xg, nt, G_FFN, hT, B_hT, t * 128, xn, B_xn, sq, B_sq, 16)
                rec_lists = []
                real_op = kb.op
                for t, nt in tl:
                    rec = []
                    rec_lists.append(rec)
                    kb.op = lambda eng, fn, reads=(), writes=(), rec=rec: rec.append((eng, fn, reads, writes))
                    rt, B_rt = sc["rt"][t]
                    b = next_ps()
                    mm_acc(b, ps[b][:nt, :36], [(hT[:, k, t * 128:t * 128 + nt], wr_sb[:, k, :]) for k in range(KC)],
                           [B_hT, B_wrs])
                    lg = rt[:nt, 0:36]
                    gmax, ngmax, gsum, emax, nemax, m2, den = (rt[:nt, 36 + i:37 + i] for i in range(7))
                    gmask = rt[:nt, 44:48]
                    pen = rt[:nt, 48:52]
                    ge = rt[:nt, 52:56]
                    ee = rt[:nt, 56:88]
                    kb.op(dve, lambda e, b=b, nt=nt, lg=lg: e.tensor_tensor(out=lg, in0=ps[b][:nt, :36], in1=rbias[:nt, :],
                                                                         op=ALU.add), [psb[b], B_rb], [B_rt])
                    kb.op(dve, lambda e, lg=lg, gmax=gmax: e.reduce_max(out=gmax, in_=lg[:, 0:4], axis=AX.X), [B_rt], [B_rt])
                    kb.op(dve, lambda e, gmax=gmax, ngmax=ngmax: e.tensor_scalar(out=ngmax, in0=gmax, scalar1=-1.0, scalar2=None,
                                                                                 op0=ALU.mult), [B_rt], [B_rt])
                    kb.op(dve, lambda e, lg=lg, gmax=gmax, gmask=gmask: e.tensor_scalar(out=gmask, in0=lg[:, 0:4], scalar1=gmax,
                                                                                      scalar2=None, op0=ALU.is_ge),
                          [B_rt], [B_rt])
                    kb.op(dve, lambda e, gsum=gsum: e.memset(gsum, 0.0), [], [B_rt])
                    kb.op(act, lambda e, lg=lg, ge=ge, ngmax=ngmax, gsum=gsum: e.activation(
                        out=ge, in_=lg[:, 0:4], func=ACT.Exp, bias=ngmax, accum_out=gsum), [B_rt], [B_rt])
                    kb.op(dve, lambda e, gmask=gmask, pen=pen: e.tensor_scalar(out=pen, in0=gmask, scalar1=-1.0, scalar2=1e30,
                                                                              op0=ALU.add, op1=ALU.mult), [B_rt], [B_rt])
                    for g in range(4):
                        kb.op(dve, lambda e, g=g, lg=lg, pen=pen, ee=ee: e.tensor_scalar(
                            out=ee[:, g * 8:(g + 1) * 8], in0=lg[:, 4 + g * 8:12 + g * 8], scalar1=pen[:, g:g + 1],
                            scalar2=None, op0=ALU.add), [B_rt], [B_rt])
                    kb.op(dve, lambda e, ee=ee, emax=emax: e.reduce_max(out=emax, in_=ee, axis=AX.X), [B_rt], [B_rt])
                    kb.op(dve, lambda e, emax=emax, nemax=nemax: e.tensor_scalar(out=nemax, in0=emax, scalar1=-1.0, scalar2=None,
                                                                                 op0=ALU.mult), [B_rt], [B_rt])
                    kb.op(act, lambda e, ee=ee, nemax=nemax: e.activation(out=ee, in_=ee, func=ACT.Exp, bias=nemax),
                          [B_rt], [B_rt])
                    e2 = rt[:nt, 0:32]
                    kb.op(dve, lambda e, ee=ee, e2=e2: e.scalar_tensor_tensor(out=e2, in0=ee, scalar=1.0, in1=ee, op0=ALU.is_lt,
                                                                             op1=ALU.mult), [B_rt], [B_rt])
                    kb.op(dve, lambda e, e2=e2, m2=m2: e.reduce_max(out=m2, in_=e2, axis=AX.X), [B_rt], [B_rt])
                    kb.op(dve, lambda e, m2=m2, den=den, gsum=gsum: e.scalar_tensor_tensor(
                        out=den, in0=m2, scalar=1.0, in1=gsum, op0=ALU.add, op1=ALU.mult), [B_rt], [B_rt])
                    kb.op(dve, lambda e, den=den: e.reciprocal(out=den, in_=den), [B_rt], [B_rt])
                    kb.op(dve, lambda e, ee=ee, e2=e2, m2=m2: e.scalar_tensor_tensor(out=e2, in0=ee, scalar=m2, in1=ee,
                                                                                   op0=ALU.is_ge, op1=ALU.mult), [B_rt], [B_rt])
                    kb.op(dve, lambda e, e2=e2, den=den, t=t, nt=nt: e.tensor_scalar(out=comb[:nt, t, :], in0=e2, scalar1=den,
                                                                                    scalar2=None, op0=ALU.mult),
                          [B_rt], [B_comb])
                kb.op = real_op
                del kb.__dict__["op"]
                for i_ in range(max(len(r_) for r_ in rec_lists)):
                    for r_ in rec_lists:
                        if i_ < len(r_):
                            kb.op(*r_[i_])
            steps.append(([], s_moe_open))

            mtc = [0]
            for ex in range(NEXP):
                def s_gu(sl, Bsl, ex=ex):
                    aT, B_aT = sc["aT"][ex % 2]
                    wg = sl[:, 0:KC * 256].rearrange("p (k c) -> p k c", c=256)
                    wu = sl[:, KC * 256:2 * KC * 256].rearrange("p (k c) -> p k c", c=256)
                    for hc in range(2):
                        sg, B_sg = sc["sg"][hc]
                        bg = next_ps()
                        mm_acc(bg, ps[bg][:, :NQ], [(wg[:, k, hc * 128:(hc + 1) * 128], hT[:, k, :NQ]) for k in range(KC)],
                               [Bsl, B_hT])
                        bu = next_ps()
                        mm_acc(bu, ps[bu][:, :NQ], [(wu[:, k, hc * 128:(hc + 1) * 128], hT[:, k, :NQ]) for k in range(KC)],
                               [Bsl, B_hT])
                        kb.op(act, lambda e, bg=bg, sg=sg: e.activation(out=sg[:, :NQ], in_=ps[bg][:, :NQ], func=ACT.Silu),
                              [psb[bg]], [B_sg])
                        kb.op(dve, lambda e, bu=bu, sg=sg, hc=hc, aT=aT: e.tensor_tensor(out=aT[:, hc, :NQ], in0=ps[bu][:, :NQ],
                                                                                        in1=sg[:, :NQ], op=ALU.mult),
                              [psb[bu], B_sg], [B_aT])
                steps.append(([(0, s_egu[ex], 2 * KC, 256, B_exp[ex // 4])], s_gu))

                def s_dn(sl, Bsl, ex=ex):
                    aT, B_aT = sc["aT"][ex % 2]
                    acc, B_acc = sc["acc"]
                    B_accp = sc["accp"]
                    comb, B_comb = sc["comb"]
                    wd = sl[:, 0:2 * D].rearrange("p (k c) -> p k c", c=D)
                    for t, nt in tl:
                        for cb in range(4):
                            b = next_ps()
                            mm_acc(b, ps[b][:nt, :], [(aT[:, hc, t * 128:t * 128 + nt], wd[:, hc, cb * 512:(cb + 1) * 512])
                                                      for hc in range(2)], [Bsl, B_aT])
                            if MOE_SPLIT and (t * 4 + cb) % 3 == 2:
                                mt_, B_mt = sc["mtmp"][mtc[0] % 3]
                                mtc[0] += 1
                                kb.op(act, lambda e, b=b, t=t, nt=nt, mt_=mt_: e.activation(
                                    out=mt_[:nt, :], in_=ps[b][:nt, :], func=ACT.Copy, scale=comb[:nt, t, ex:ex + 1]),
                                    [psb[b], B_comb], [B_mt])
                                kb.op(pool, lambda e, t=t, nt=nt, cb=cb, mt_=mt_: e.tensor_tensor(
                                    out=acc[:nt, t, cb * 512:(cb + 1) * 512], in0=acc[:nt, t, cb * 512:(cb + 1) * 512],
                                    in1=mt_[:nt, :], op=ALU.add), [B_mt, B_accp], [B_accp])
                            else:
                                kb.op(dve, lambda e, b=b, t=t, nt=nt, cb=cb: e.scalar_tensor_tensor(
                                    out=acc[:nt, t, cb * 512:(cb + 1) * 512], in0=ps[b][:nt, :], scalar=comb[:nt, t, ex:ex + 1],
                                    in1=acc[:nt, t, cb * 512:(cb + 1) * 512], op0=ALU.mult, op1=ALU.add),
                                    [psb[b], B_comb, B_acc], [B_acc])
                steps.append(([(0, s_ed[ex], 2, D, B_exp[ex // 4])], s_dn))

            def s_final(_, __):
                acc, B_acc = sc["acc"]
                B_accp = sc["accp"]
                gfin, B_gfin = sc["gfin"]
                for t, nt in tl:
                    c_ss = cols[:nt, 20:21]
                    c_r = cols[:nt, 21:22]
                    kb.op(dve, lambda e, t=t, nt=nt: e.tensor_tensor(out=acc[:nt, t, :], in0=acc[:nt, t, :], in1=xg[:nt, t, :],
                                                                    op=ALU.add), [B_xg, B_acc, B_accp], [B_acc, B_accp])
                    kb.op(dve, lambda e, c_ss=c_ss: e.memset(c_ss, 0.0), [], [B_cols])
                    kb.op(act, lambda e, t=t, nt=nt, c_ss=c_ss: e.activation(out=sq[:nt, :], in_=acc[:nt, t, :], func=ACT.Square,
                                                                            accum_out=c_ss), [B_acc, B_cols], [B_sq, B_cols])
                    kb.op(act, lambda e, c_ss=c_ss, c_r=c_r: e.activation(out=c_r, in_=c_ss, func=ACT.Sqrt, scale=1.0 / D,
                                                                          bias=EPS), [B_cols], [B_cols])
                    kb.op(dve, lambda e, c_r=c_r: e.reciprocal(out=c_r, in_=c_r), [B_cols], [B_cols])
                    kb.op(dve, lambda e, t=t, nt=nt, c_r=c_r: e.scalar_tensor_tensor(
                        out=acc[:nt, t, :], in0=acc[:nt, t, :], scalar=c_r, in1=gfin[:nt, :], op0=ALU.mult, op1=ALU.mult),
                        [B_acc, B_cols, B_gfin], [B_acc])
                    kb.dma(pool, y_o[row0 + t * 128:row0 + t * 128 + nt, :], acc[:nt, t, :], [B_acc], [], B_acc)
            steps.append(([], s_final))
            steps.append(([], close_scope))

        for gi in range(NGRP_):
            blocks = None
            group(gi, GT, xfull[gi * 4 * GT:(gi * 4 + 1) * GT, :], gi * GT, "x", mkT_p, B_mkp, mv_p, B_mvp,
                  s_ktp, B_ktp, s_vp, B_vp, blocks, convp_o if gi == NGRP_ - 1 else None)
        def s_mem_sample(_, __):
            cmb = xn[:, :].rearrange("p (t c) -> p t c", c=1024)
            for t in range(2):
                kb.dma(pool, cmb[:, t, :], cmk[t * 128:(t + 1) * 128, :], [], [B_xn], B_xn)
                kb.dma(pool, mv_p[:, t, :], cmv[t * 128:(t + 1) * 128, :], [], [B_mvp], B_mvp)
            for t in range(2):
                b = next_ps()
                pv = ps[b][:].bitcast(BF16).rearrange("p (j c) -> p j c", c=128)
                for cc in range(8):
                    kb.op(pe, lambda e, cc=cc, pv=pv, t=t: e.transpose(pv[:, cc, :], cmb[:, t, cc * 128:(cc + 1) * 128],
                                                                      ident[:, :]), [B_xn, B_c], [psb[b]])
                evac(t, mkT_p[:, :, t * 128:(t + 1) * 128], pv[:, :, :], [psb[b]], [B_mkp])
        steps.append(([], s_mem_sample))
        group(NGRP_, NSAMP, xsamp, NGRP_ * GT, "cache", mkT_p, B_mkp, mv_p, B_mvp, s_kts, B_kts, s_vs, B_vs, None, convs_o)
        run_steps()

        kb.barrier()
    return nc


_NC_CACHE = {}


def kernel(**inp):
    inp = {k: np.asarray(v) for k, v in inp.items()}
    if "nc" not in _NC_CACHE:
        _NC_CACHE["nc"] = build_program()
    nc = _NC_CACHE["nc"]
    xp = inp["x_prompt"]
    in_maps = []
    wnames = ["norm_mix", "w_in", "w_dw", "b_dw", "conv_ln_g", "conv_ln_b", "w_conv_out", "lambda_q1", "lambda_k1",
              "lambda_q2", "lambda_k2", "subln_g", "w_attn_out", "norm_mem", "w_mem_k", "w_mem_v", "w_mem_out",
              "w_out", "norm_ffn", "w_router_grp", "b_router_grp", "w_router_exp", "b_router_exp", "w_exp_gate",
              "w_exp_up", "w_exp_down"]
    wts = {n: np.ascontiguousarray(inp[n][0]) for n in wnames}
    wts["norm_final"] = np.ascontiguousarray(inp["norm_final"])
    for c in range(8):
        b, j = c // 4, c % 4
        xb = xp[b].reshape(16, GT, D)
        order = []
        for i in range(4):
            order.append(4 * i + j)
            order += [4 * i + m for m in range(4) if m != j]
        xfull = np.ascontiguousarray(xb[order].reshape(SEQ, D))
        xhalo = np.zeros((NGRP, 32, D), np.float32)
        for i in range(4):
            g = 4 * i + j
            if g > 0:
                xhalo[i] = xp[b, g * GT - 32:g * GT]
        gp = np.array([j] + [m for m in range(4) if m != j], np.float32)
        m = dict(wts)
        m.update({
            "xfull": xfull, "xhalo": xhalo, "gpos": np.ascontiguousarray(np.broadcast_to(gp, (128, 4))),
            "xsamp": np.ascontiguousarray(inp["x_sample"][c]),
            "cconv": np.ascontiguousarray(inp["cache_conv"][0, c]),
            "ck": np.ascontiguousarray(inp["cache_diff_k"][0, c].reshape(PAST, D)),
            "cv": np.ascontiguousarray(inp["cache_diff_v"][0, c].reshape(PAST, D)),
            "cmk": np.ascontiguousarray(inp["cache_mem_k"][0, c].reshape(256, 1024)),
            "cmv": np.ascontiguousarray(inp["cache_mem_v"][0, c].reshape(256, 1024)),
            "memx": np.ascontiguousarray(inp["mem_prompt"][b]),
        })
        in_maps.append(m)
    res = run_bass_kernel_spmd(nc, in_maps, core_ids=list(range(8)))
    R = res.results
    y_p = np.zeros((2, SEQ, D), np.float32)
    k_p = np.zeros((1, 2, SEQ, 16, 128), np.float32)
    v_p = np.zeros((1, 2, SEQ, 16, 128), np.float32)
    y_s = np.zeros((8, NSAMP, D), np.float32)
    k_s = np.zeros((1, 8, NSAMP, 16, 128), np.float32)
    v_s = np.zeros((1, 8, NSAMP, 16, 128), np.float32)
    conv_p = np.zeros((1, 2, 30, DCONV), np.float32)
    conv_s = np.zeros((1, 8, 30, DCONV), np.float32)
    mk_p = np.zeros((1, 2, 256, 4, 256), np.float32)
    mv_p = np.zeros((1, 2, 256, 4, 256), np.float32)
    for c in range(8):
        b, j = c // 4, c % 4
        r = R[c]
        for i in range(4):
            g = 4 * i + j
            y_p[b, g * GT:(g + 1) * GT] = r["y"][i * GT:(i + 1) * GT]
            k_p[0, b, g * GT:(g + 1) * GT] = r["kout"][i * GT:(i + 1) * GT].reshape(GT, 16, 128)
            v_p[0, b, g * GT:(g + 1) * GT] = r["vout"][i * GT:(i + 1) * GT].reshape(GT, 16, 128)
        y_s[c] = r["y"][NGRP * GT:]
        k_s[0, c] = r["kout"][NGRP * GT:].reshape(NSAMP, 16, 128)
        v_s[0, c] = r["vout"][NGRP * GT:].reshape(NSAMP, 16, 128)
        conv_s[0, c] = r["convs"]
        if j == 3:
            conv_p[0, b] = r["convp"]
        if j == 0:
            mk_p[0, b] = r["memk"].reshape(256, 4, 256)
            mv_p[0, b] = r["memv"].reshape(256, 4, 256)
    return (y_p, y_s, conv_p, k_p, v_p, mk_p, mv_p, conv_s, k_s, v_s)
```
